# Optimizing a Trainium2 kernel written in Bass

```python
import jax, jax.numpy as jnp
from jax import lax
import numpy as np

D_MODEL = 2048
BATCH = 4
SEQ = 2048
DEPTH = 2

D_FF = 5632
SSD_HEADS = 32
SSD_HEAD_DIM = 64
SSD_D = SSD_HEADS * SSD_HEAD_DIM
SSD_GROUPS = 8
SSD_STATE = 128
SSD_CONV = 4
SSD_CHUNK = 128
SSD_XBC = SSD_D + 2 * SSD_GROUPS * SSD_STATE
RET_HEADS = 8
RET_QK_DIM = 256
RET_V_DIM = 256
RET_D = RET_HEADS * RET_V_DIM
RET_CHUNK = 128
RET_THETA = 10000.0
ATT_HEADS = 16
ATT_HEAD_DIM = 128
IDX_HEADS = 16
IDX_DIM = 64
TOPK_MAX = 256
Q_BLOCK = 128
ROPE_THETA = 500000.0
ROPE_FRACTION = 4
EPS = 1e-6

IN0_WIDTHS = (SSD_D, SSD_XBC, SSD_HEADS, RET_HEADS * RET_QK_DIM, RET_HEADS * RET_QK_DIM, RET_D, RET_D)
IN0 = sum(IN0_WIDTHS)
IN1_WIDTHS = (ATT_HEADS * ATT_HEAD_DIM, ATT_HEAD_DIM, ATT_HEAD_DIM, IDX_HEADS * IDX_DIM, IDX_DIM, IDX_HEADS)
IN1 = sum(IN1_WIDTHS)
N_EVEN = (DEPTH + 1) // 2
N_ODD = DEPTH // 2

kernel_name = "hybrid_ssd_retention_dsa_macaron_adaln"


def split_cols(a, widths):
    return jnp.split(a, [int(o) for o in np.cumsum(widths)[:-1]], axis=-1)


def rms_norm(x, w):
    xf = x.astype(jnp.float32)
    y = xf * lax.rsqrt(jnp.mean(xf * xf, axis=-1, keepdims=True) + EPS)
    return (y * w.astype(jnp.float32)).astype(x.dtype)


def rope(x, positions, rot_dim, theta):
    half = rot_dim // 2
    inv = theta ** (-jnp.arange(half, dtype=jnp.float32) / half)
    ang = positions.astype(jnp.float32)[..., None] * inv
    cos, sin = jnp.cos(ang)[:, :, None, :], jnp.sin(ang)[:, :, None, :]
    xr = x[..., :rot_dim].astype(jnp.float32)
    x1, x2 = xr[..., :half], xr[..., half:]
    rot = jnp.concatenate([x1 * cos - x2 * sin, x1 * sin + x2 * cos], axis=-1).astype(x.dtype)
    return jnp.concatenate([rot, x[..., rot_dim:]], axis=-1)


def swiglu(h, w_gate, w_up, w_down):
    return (jax.nn.silu(h @ w_gate) * (h @ w_up)) @ w_down


def causal_depthwise_conv(u, w, b):
    ch = u.shape[-1]
    out = lax.conv_general_dilated(u, w[:, None, :], (1,), [(w.shape[0] - 1, 0)],
                                   dimension_numbers=('NWC', 'WIO', 'NWC'), feature_group_count=ch)
    return out + b


def ssd_mixer(z, xbc, dt_raw, conv_w, conv_b, A_log, dt_bias, D_skip, norm_w):
    Bsz, L, _ = xbc.shape
    Q, G, R, P, N = SSD_CHUNK, SSD_GROUPS, SSD_HEADS // SSD_GROUPS, SSD_HEAD_DIM, SSD_STATE
    NC = L // Q
    xbc = jax.nn.silu(causal_depthwise_conv(xbc, conv_w, conv_b))
    xs, Bm, Cm = split_cols(xbc, (SSD_D, G * N, G * N))
    xs = xs.reshape(Bsz, NC, Q, G, R, P)
    Bm = Bm.reshape(Bsz, NC, Q, G, N)
    Cm = Cm.reshape(Bsz, NC, Q, G, N)
    dt = jax.nn.softplus(dt_raw.astype(jnp.float32) + dt_bias.astype(jnp.float32))
    A = -jnp.exp(A_log.astype(jnp.float32))
    dA = (dt * A).reshape(Bsz, NC, Q, G, R)
    dt = dt.reshape(Bsz, NC, Q, G, R)
    cum = jnp.cumsum(dA, axis=2)
    xdt = xs * dt[..., None]
    seg = cum[:, :, :, None] - cum[:, :, None, :]
    causal = jnp.tril(jnp.ones((Q, Q), dtype=bool))[:, :, None, None]
    decay = jnp.exp(jnp.where(causal, seg, -jnp.inf))
    CB = jnp.einsum('bclgn,bcsgn->bclsg', Cm, Bm)
    y_diag = jnp.einsum('bclsg,bclsgr,bcsgrp->bclgrp', CB, decay, xdt)
    decay_s = jnp.exp(cum[:, :, -1:] - cum)
    states = jnp.einsum('bcsgn,bcsgr,bcsgrp->bcgrpn', Bm, decay_s, xdt)
    chunk_decay = jnp.exp(cum[:, :, -1])

    def step(h, inp):
        s_c, a_c = inp
        return h * a_c[..., None, None] + s_c, h

    h0 = jnp.zeros((Bsz, G, R, P, N), jnp.float32)
    _, h_prev = lax.scan(step, h0, (jnp.moveaxis(states, 1, 0), jnp.moveaxis(chunk_decay, 1, 0)))
    h_prev = jnp.moveaxis(h_prev, 0, 1)
    y_off = jnp.einsum('bclgn,bcgrpn,bclgr->bclgrp', Cm, h_prev, jnp.exp(cum))
    y = y_diag + y_off + xs * D_skip.reshape(G, R)[:, :, None].astype(jnp.float32)
    y = y.reshape(Bsz, L, SSD_D)
    y = rms_norm(y * jax.nn.silu(z.astype(jnp.float32)), norm_w)
    return y.astype(z.dtype)


def retention_mixer(q, k, v, g, positions, norm_w):
    Bsz, L, _ = q.shape
    H, Q = RET_HEADS, RET_CHUNK
    NC = L // Q
    q = rope(q.reshape(Bsz, L, H, RET_QK_DIM), positions, RET_QK_DIM, RET_THETA)
    k = rope(k.reshape(Bsz, L, H, RET_QK_DIM), positions, RET_QK_DIM, RET_THETA) * (RET_QK_DIM ** -0.5)
    v = v.reshape(Bsz, L, H, RET_V_DIM)
    q = q.reshape(Bsz, NC, Q, H, RET_QK_DIM)
    k = k.reshape(Bsz, NC, Q, H, RET_QK_DIM)
    v = v.reshape(Bsz, NC, Q, H, RET_V_DIM)
    log_gamma = jnp.log(1.0 - 2.0 ** (-5.0 - jnp.arange(H, dtype=jnp.float32)))
    idx = jnp.arange(Q, dtype=jnp.float32)
    dist = idx[:, None] - idx[None, :]
    intra = jnp.exp(jnp.where(dist[..., None] >= 0, dist[..., None] * log_gamma, -jnp.inf))
    scores = jnp.einsum('bclhd,bcshd->bchls', q, k) * jnp.transpose(intra, (2, 0, 1))
    y_intra = jnp.einsum('bchls,bcshv->bclhv', scores, v)
    k_dec = jnp.exp((Q - 1 - idx)[:, None] * log_gamma)
    states = jnp.einsum('bcshd,sh,bcshv->bchdv', k, k_dec, v)
    chunk_decay = jnp.exp(Q * log_gamma)

    def step(r, s_c):
        return r * chunk_decay[:, None, None] + s_c, r

    r0 = jnp.zeros((Bsz, H, RET_QK_DIM, RET_V_DIM), jnp.float32)
    _, r_prev = lax.scan(step, r0, jnp.moveaxis(states, 1, 0))
    r_prev = jnp.moveaxis(r_prev, 0, 1)
    q_dec = jnp.exp((idx + 1)[:, None] * log_gamma)
    y_inter = jnp.einsum('bclhd,lh,bchdv->bclhv', q, q_dec, r_prev)
    y = (y_intra + y_inter).reshape(Bsz, L, H, RET_V_DIM)
    y = rms_norm(y, norm_w.reshape(H, RET_V_DIM)).reshape(Bsz, L, RET_D)
    return (jax.nn.silu(g.astype(jnp.float32)) * y).astype(g.dtype)


def dsa_mixer(proj, positions, idx_k_norm_w):
    Bsz, L, _ = proj.shape
    q, k, v, qi, ki, wi = split_cols(proj, IN1_WIDTHS)
    rot = ATT_HEAD_DIM // ROPE_FRACTION
    rot_i = IDX_DIM // ROPE_FRACTION
    q = rope(q.reshape(Bsz, L, ATT_HEADS, ATT_HEAD_DIM), positions, rot, ROPE_THETA)
    k = rope(k[:, :, None, :], positions, rot, ROPE_THETA)[:, :, 0]
    qi = rope(qi.reshape(Bsz, L, IDX_HEADS, IDX_DIM), positions, rot_i, ROPE_THETA)
    ki = rope(rms_norm(ki, idx_k_norm_w)[:, :, None, :], positions, rot_i, ROPE_THETA)[:, :, 0]
    wi = wi * (IDX_HEADS ** -0.5) * (IDX_DIM ** -0.5)
    kv = jnp.concatenate([k, v], axis=-1)
    topk = min(TOPK_MAX, L // 4)
    nb = L // Q_BLOCK
    key_pos = jnp.arange(L)

    def to_blocks(a):
        return jnp.swapaxes(a.reshape(Bsz, nb, Q_BLOCK, *a.shape[2:]), 0, 1)

    def block(args):
        qb, qib, wib, start = args
        q_pos = start + jnp.arange(Q_BLOCK)
        s = jax.nn.relu(jnp.einsum('bqhd,bsd->bqhs', qib, ki).astype(jnp.float32))
        score = jnp.einsum('bqhs,bqh->bqs', s, wib.astype(jnp.float32))
        visible = key_pos[None, :] <= q_pos[:, None]
        score = jnp.where(visible[None], score, -jnp.inf)
        _, sel = lax.top_k(score, topk)
        kv_sel = jax.vmap(lambda kvb, ib: kvb[ib])(kv, sel)
        k_sel, v_sel = kv_sel[..., :ATT_HEAD_DIM], kv_sel[..., ATT_HEAD_DIM:]
        logits = jnp.einsum('bqhd,bqkd->bqhk', qb, k_sel).astype(jnp.float32) * (ATT_HEAD_DIM ** -0.5)
        valid = sel <= q_pos[None, :, None]
        logits = jnp.where(valid[:, :, None, :], logits, -jnp.inf)
        p = jax.nn.softmax(logits, axis=-1).astype(v_sel.dtype)
        return jnp.einsum('bqhk,bqkd->bqhd', p, v_sel)

    starts = jnp.arange(nb) * Q_BLOCK
    out = lax.map(block, (to_blocks(q), to_blocks(qi), to_blocks(wi), starts))
    return jnp.swapaxes(out, 0, 1).reshape(Bsz, L, ATT_HEADS * ATT_HEAD_DIM)


def modulated_sublayer(x, mods, j, pre_w, post_w, fn, res_weight):
    shift, scale, gate = (mods[..., (3 * j + m) * D_MODEL:(3 * j + m + 1) * D_MODEL] for m in range(3))
    h = rms_norm(x, pre_w) * (1 + scale) + shift
    y = rms_norm(fn(h), post_w)
    return x + res_weight * gate * y


def setup_inputs(seed: int = 0) -> dict:
    key = jax.random.key(seed)
    ks = jax.random.split(key, 24)
    f32 = jnp.float32

    def nrm(k, shape, fan_in, gain=1.0):
        return jax.random.normal(k, shape, f32) * (gain * fan_in ** -0.5)

    def gain(k, shape):
        return 1.0 + 0.05 * jax.random.normal(k, shape, f32)

    x = jax.random.normal(ks[0], (BATCH, SEQ, D_MODEL), f32)
    c = jax.random.normal(ks[1], (BATCH, D_MODEL), f32)
    offset = jax.random.randint(ks[2], (BATCH, 1), 0, 4096, dtype=jnp.int32)
    positions = (jnp.arange(SEQ, dtype=jnp.int32)[None, :] + offset).astype(jnp.int32)
    mod_w = nrm(ks[3], (DEPTH, D_MODEL, 9 * D_MODEL), D_MODEL, 0.5)
    mod_b = 0.01 * jax.random.normal(ks[4], (DEPTH, 9 * D_MODEL), f32)
    norm_w = gain(ks[5], (DEPTH, 6, D_MODEL))
    ffn_w_gate = nrm(ks[6], (DEPTH, 2, D_MODEL, D_FF), D_MODEL)
    ffn_w_up = nrm(ks[7], (DEPTH, 2, D_MODEL, D_FF), D_MODEL)
    ffn_w_down = nrm(ks[8], (DEPTH, 2, D_FF, D_MODEL), D_FF)
    hy_w_in = nrm(ks[9], (N_EVEN, D_MODEL, IN0), D_MODEL)
    hy_conv_w = nrm(ks[10], (N_EVEN, SSD_CONV, SSD_XBC), SSD_CONV)
    hy_conv_b = 0.05 * jax.random.normal(ks[11], (N_EVEN, SSD_XBC), f32)
    ssd_A_log = jnp.log(jax.random.uniform(ks[12], (N_EVEN, SSD_HEADS), f32, 1.0, 16.0))
    dt0 = jnp.exp(jax.random.uniform(ks[13], (N_EVEN, SSD_HEADS), f32, np.log(1e-3), np.log(1e-1)))
    ssd_dt_bias = dt0 + jnp.log(-jnp.expm1(-dt0))
    ssd_D = 1.0 + 0.1 * jax.random.normal(ks[14], (N_EVEN, SSD_HEADS), f32)
    ssd_norm_w = gain(ks[15], (N_EVEN, SSD_D))
    ret_norm_w = gain(ks[16], (N_EVEN, RET_D))
    hy_w_out = nrm(ks[17], (N_EVEN, SSD_D + RET_D, D_MODEL), SSD_D + RET_D)
    dsa_w_in = nrm(ks[18], (N_ODD, D_MODEL, IN1), D_MODEL)
    idx_k_norm_w = gain(ks[19], (N_ODD, IDX_DIM))
    dsa_w_out = nrm(ks[20], (N_ODD, ATT_HEADS * ATT_HEAD_DIM, D_MODEL), ATT_HEADS * ATT_HEAD_DIM)
    return {"x": x, "c": c, "positions": positions, "mod_w": mod_w, "mod_b": mod_b, "norm_w": norm_w,
            "ffn_w_gate": ffn_w_gate, "ffn_w_up": ffn_w_up, "ffn_w_down": ffn_w_down,
            "hy_w_in": hy_w_in, "hy_conv_w": hy_conv_w, "hy_conv_b": hy_conv_b, "ssd_A_log": ssd_A_log,
            "ssd_dt_bias": ssd_dt_bias, "ssd_D": ssd_D, "ssd_norm_w": ssd_norm_w, "ret_norm_w": ret_norm_w,
            "hy_w_out": hy_w_out, "dsa_w_in": dsa_w_in, "idx_k_norm_w": idx_k_norm_w, "dsa_w_out": dsa_w_out}


def reference(x, c, positions, mod_w, mod_b, norm_w, ffn_w_gate, ffn_w_up, ffn_w_down,
              hy_w_in, hy_conv_w, hy_conv_b, ssd_A_log, ssd_dt_bias, ssd_D, ssd_norm_w, ret_norm_w,
              hy_w_out, dsa_w_in, idx_k_norm_w, dsa_w_out):
    cond = jax.nn.silu(c)
    for i in range(DEPTH):
        mods = (cond @ mod_w[i] + mod_b[i])[:, None, :]

        def ffn(h, f, i=i):
            return swiglu(h, ffn_w_gate[i, f], ffn_w_up[i, f], ffn_w_down[i, f])

        if i % 2 == 0:
            e = i // 2

            def mixer(h, e=e):
                proj = h @ hy_w_in[e]
                z, xbc, dt_raw, rq, rk, rv, rg = split_cols(proj, IN0_WIDTHS)
                ya = ssd_mixer(z, xbc, dt_raw, hy_conv_w[e], hy_conv_b[e], ssd_A_log[e],
                               ssd_dt_bias[e], ssd_D[e], ssd_norm_w[e])
                yb = retention_mixer(rq, rk, rv, rg, positions, ret_norm_w[e])
                return jnp.concatenate([ya.astype(h.dtype), yb.astype(h.dtype)], axis=-1) @ hy_w_out[e]
        else:
            o = i // 2

            def mixer(h, o=o):
                return dsa_mixer(h @ dsa_w_in[o], positions, idx_k_norm_w[o]) @ dsa_w_out[o]

        x = modulated_sublayer(x, mods, 0, norm_w[i, 0], norm_w[i, 1], lambda h: ffn(h, 0), 0.5)
        x = modulated_sublayer(x, mods, 1, norm_w[i, 2], norm_w[i, 3], mixer, 1.0)
        x = modulated_sublayer(x, mods, 2, norm_w[i, 4], norm_w[i, 5], lambda h: ffn(h, 1), 0.5)
    return x
```

```python
import numpy as np
import concourse.bass as bass
import concourse.mybir as mybir
from concourse.bass_utils import run_bass_kernel_spmd

F32 = mybir.dt.float32
BF16 = mybir.dt.bfloat16
I32 = mybir.dt.int32
AF = mybir.ActivationFunctionType
ALU = mybir.AluOpType

D = 2048
T = 2048
DFF = 5632
KC = 16
NEG = -1.0e30
EPS = 1e-6
IN0_OFF = dict(z=0, xbc=2048, dt=6144, rq=6176, rk=8224, rv=10272, rg=12320)
IN1_OFF = dict(q=0, k=2048, v=2176, qi=2304, ki=3328, wi=3392)


class Buf:
    __slots__ = ("w", "r", "const", "excl")

    def __init__(self, const=False, excl=False):
        self.w = None
        self.r = []
        self.const = const
        self.excl = excl


class Prog:
    def __init__(self, nc):
        self.nc = nc
        self.eng = {"pe": nc.tensor, "dve": nc.vector, "act": nc.scalar, "pool": nc.gpsimd, "sp": nc.sync}
        self.sem = {k: nc.alloc_semaphore("s_" + k) for k in self.eng}
        self.cnt = {k: 0 for k in self.eng}
        self.pend = {k: 0 for k in self.eng}
        self.seen = {k: {} for k in self.eng}
        self.nslots = 10
        self.slots = {}
        self.rr = {}
        for q in ("sp", "pool", "act"):
            self.rr[q] = 0
            for i in range(self.nslots):
                key = "d_%s%d" % (q, i)
                self.slots[key] = [nc.alloc_semaphore(key), 0]
        self.sb_off = 16384
        self.sb_max = 16384 + 212000
        self.uid = 0
        self.psum = []
        for i in range(8):
            self.psum.append((nc.alloc_psum_tensor("psb%d" % i, [128, 512], F32), Buf(excl=True)))
        self.prr = 0
        self.reserved = set()
        self.log = {k: [] for k in self.eng}

    def sb(self, shape, dtype, name=None):
        self.uid += 1
        esz = 2 if dtype == BF16 else 4
        n = 1
        for s in shape[1:]:
            n *= s
        nbytes = (n * esz + 63) // 64 * 64
        off = self.sb_off
        assert off + nbytes <= self.sb_max, ("SBUF overflow", name, off, nbytes)
        self.sb_off += nbytes
        t = self.nc.alloc_sbuf_tensor_at("%s_%d" % (name or "t", self.uid), list(shape), dtype, offset=off)
        return t

    def mark(self):
        return self.sb_off

    def release(self, m):
        self.barrier()
        self.sb_off = m

    def ps(self):
        while True:
            i = self.prr % 8
            self.prr += 1
            if i not in self.reserved:
                return self.psum[i]

    def ps_reserve(self):
        for i in range(7, -1, -1):
            if i not in self.reserved:
                self.reserved.add(i)
                return i, self.psum[i]
        raise RuntimeError("no psum")

    def ps_unreserve(self, i):
        self.reserved.discard(i)

    def _semof(self, key):
        if key in self.sem:
            return self.sem[key]
        return self.slots[key][0]

    def _wait(self, e, deps):
        for o, v in deps.items():
            if self.seen[e].get(o, 0) < v:
                self.eng[e].wait_ge(self._semof(o), v)
                self.seen[e][o] = v
                self.log[e].append(("w", o, v))

    def _collect(self, e, reads, writes):
        deps = {}

        def add(x):
            if x is None:
                return
            o, v = x
            if o == e and e == "pe":
                return
            if deps.get(o, 0) < v:
                deps[o] = v

        for b in reads:
            add(b.w)
            if b.excl:
                for r in b.r:
                    if r[0] != e:
                        add(r)
        for b in writes:
            add(b.w)
            for r in b.r:
                if r[0] != e:
                    add(r)
        return deps

    def op(self, e, fn, reads=(), writes=(), inc=True):
        deps = self._collect(e, reads, writes)
        self._wait(e, deps)
        ins = fn(self.eng[e])
        val = self.cnt[e] + 1
        if inc:
            ins.then_inc(self.sem[e], 1)
            self.cnt[e] = val
            self.pend[e] = 0
            self.log[e].append(("i", e, 1))
        else:
            self.pend[e] += 1
        for b in reads:
            if not b.const:
                b.r.append((e, val))
        for b in writes:
            b.w = (e, val)
            b.r = []
        return ins

    def dma(self, q, out, in_, reads=(), writes=()):
        key = "d_%s%d" % (q, self.rr[q] % self.nslots)
        self.rr[q] += 1
        slot = self.slots[key]
        deps = self._collect(key, reads, writes)
        if slot[1] > 0:
            deps[key] = max(deps.get(key, 0), slot[1])
        self._wait(q, deps)
        slot[1] += 16
        self.eng[q].dma_start(out=out, in_=in_).then_inc(slot[0], 16)
        self.log[q].append(("i", key, 16))
        for b in reads:
            if not b.const:
                b.r.append((key, slot[1]))
        for b in writes:
            b.w = (key, slot[1])
            b.r = []

    def barrier(self):
        for e in self.eng:
            assert self.pend[e] == 0, ("pending non-inc ops on", e)
        tgt = dict(self.cnt)
        for k, s in self.slots.items():
            if s[1] > 0:
                tgt[k] = s[1]
        for e in self.eng:
            deps = {o: v for o, v in tgt.items() if o != e and v > 0}
            self._wait(e, deps)

    def mm(self, out, lhsT, rhs, start, stop, reads=(), writes=(), inc=None):
        if inc is None:
            inc = stop
        return self.op("pe", lambda e: e.matmul(out, lhsT=lhsT, rhs=rhs, start=start, stop=stop),
                       reads, writes, inc=inc)

    def tr(self, out, in_, ident, reads=(), writes=(), inc=True):
        return self.op("pe", lambda e: e.matmul(out, lhsT=in_, rhs=ident, start=True, stop=True), reads, writes,
                       inc=inc)

    def act(self, out, in_, func, reads=(), writes=(), bias=None, scale=None, e="act"):
        kw = {}
        if bias is not None:
            kw["bias"] = bias
        if scale is not None:
            kw["scale"] = scale
        return self.op("act", lambda en: en.activation(out=out, in_=in_, func=func, **kw), reads, writes)

    def tt(self, e, out, in0, in1, op, reads=(), writes=()):
        return self.op(e, lambda en: en.tensor_tensor(out=out, in0=in0, in1=in1, op=op), reads, writes)

    def ts(self, e, out, in0, s1, op0, s2=None, op1=None, reads=(), writes=()):
        if op1 is None:
            return self.op(e, lambda en: en.tensor_scalar(out=out, in0=in0, scalar1=s1, scalar2=None, op0=op0),
                           reads, writes)
        return self.op(e, lambda en: en.tensor_scalar(out=out, in0=in0, scalar1=s1, scalar2=s2, op0=op0, op1=op1),
                       reads, writes)

    def stt(self, out, in0, scalar, in1, op0, op1, reads=(), writes=()):
        return self.op("dve", lambda en: en.scalar_tensor_tensor(out=out, in0=in0, scalar=scalar, in1=in1,
                                                                  op0=op0, op1=op1), reads, writes)

    def copy(self, e, out, in_, reads=(), writes=()):
        if e == "act":
            return self.op("act", lambda en: en.activation(out=out, in_=in_, func=AF.Copy), reads, writes)
        return self.op(e, lambda en: en.tensor_copy(out=out, in_=in_), reads, writes)

    def memset(self, e, ap, val, writes=()):
        return self.op(e, lambda en: en.memset(ap, val), (), writes)


def host_consts():
    c = {}
    c["ident"] = np.eye(128, dtype=np.float32)
    c["ones"] = np.ones((128, 128), np.float32)
    bo = np.zeros((128, 128), np.float32)
    bo[:64, :64] = 1
    bo[64:, 64:] = 1
    c["bones"] = bo
    s = np.arange(128)[:, None]
    l = np.arange(128)[None, :]
    c["triT"] = (l >= s).astype(np.float32)
    sel = np.zeros((32, 32, 128), np.float32)
    for h in range(32):
        sel[h, h, :] = 1
    c["selh"] = np.concatenate([sel.reshape(32, 32 * 128), np.zeros((96, 4096), np.float32)])
    ex = np.zeros((32, 32, 64), np.float32)
    for h in range(32):
        ex[h, h, :] = 1
    c["expand"] = np.concatenate([ex.reshape(32, 2048), np.zeros((96, 2048), np.float32)])
    lg = np.log(1.0 - 2.0 ** (-5.0 - np.arange(8, dtype=np.float64)))
    sc = 256.0 ** -0.5
    dm = np.zeros((128, 8, 128), np.float64)
    qd = np.zeros((128, 8, 128), np.float64)
    kd = np.zeros((128, 8), np.float64)
    for h in range(8):
        dm[:, h, :] = np.where(l >= s, np.exp((l - s) * lg[h]), 0.0) * sc
        qd[:, h, :] = np.exp((l + 1) * lg[h])
        kd[:, h] = np.exp((127 - np.arange(128)) * lg[h]) * sc
    c["ret_dm"] = dm.reshape(128, 1024).astype(np.float32)
    c["ret_qd"] = qd.reshape(128, 1024).astype(np.float32)
    c["ret_kd"] = kd.astype(np.float32)
    c["_ret_cd"] = [float(np.exp(128 * lg[h])) for h in range(8)]
    inv = np.zeros((128, 4), np.float32)
    inv[:, 0] = (10000.0 ** (-np.arange(128, dtype=np.float32) / 128)).astype(np.float32)
    th = 500000.0
    for p in range(128):
        if p < 32:
            inv[p, 1] = np.float32(th) ** np.float32(-(p % 16) / 16.0)
        if p % 64 < 16:
            inv[p, 2] = np.float32(th) ** np.float32(-((p % 64) % 8) / 8.0)
    c["inv"] = inv
    pq = np.zeros((128, 128), np.float32)
    for m in range(16):
        pq[m + 16, m] = -1.0
        pq[m, m + 16] = 1.0
    c["pm_q"] = pq
    pi = np.zeros((128, 128), np.float32)
    for b in (0, 64):
        for m in range(8):
            pi[b + m + 8, b + m] = -1.0
            pi[b + m, b + m + 8] = 1.0
    c["pm_i"] = pi
    c["cbias"] = np.where(l <= s, 0.0, NEG).astype(np.float32)
    return c


CONST_SHAPES = dict(ident=(128, 128), ones=(128, 128), bones=(128, 128), triT=(128, 128), selh=(128, 4096),
                    expand=(128, 2048), ret_dm=(128, 1024), ret_qd=(128, 1024), ret_kd=(128, 8), inv=(128, 4),
                    pm_q=(128, 128), pm_i=(128, 128), cbias=(128, 128))


class Builder:
    def __init__(self, n_sub=6):
        self.n_sub = n_sub
        self.overlap_mods = (n_sub == 6)
        nc = bass.Bass("TRN2", target_bir_lowering=False)
        self.nc = nc
        self.P = Prog(nc)
        self.inp = {}
        self.build()

    def din(self, name, shape, dtype=F32):
        t = self.nc.dram_tensor(name, list(shape), dtype, kind="ExternalInput").ap()
        self.inp[name] = t
        return t

    def dscr(self, name, shape, dtype):
        return self.nc.dram_tensor(name, list(shape), dtype, kind="Internal").ap()

    def build(self):
        nc, P = self.nc, self.P
        self.xin = self.din("xT", [D, T])
        self.cin = self.din("c_col", [128, KC])
        self.posin = self.din("pos", [T], I32)
        self.mod_w = self.din("mod_w", [2, D, 9 * D])
        self.mod_b = self.din("mod_b", [2, 128, 144])
        self.norm_w = self.din("norm_w", [2, 128, 6 * KC])
        self.wg = self.din("ffn_w_gate", [2, 2, D, DFF])
        self.wu = self.din("ffn_w_up", [2, 2, D, DFF])
        self.wd = self.din("ffn_w_down", [2, 2, DFF, D])
        self.hy_in = self.din("hy_w_in", [D, 14368])
        self.conv_w = self.din("conv_w", [128, 32 * 4])
        self.conv_b = self.din("conv_b", [128, 32])
        self.ssd_small = self.din("ssd_small", [128, 4])
        self.ssd_Dc = self.din("ssd_Dc", [128, 16])
        self.ssd_nw = self.din("ssd_nw", [128, 16])
        self.ret_nw = self.din("ret_nw", [128, 16])
        self.hy_out = self.din("hy_w_out", [4096, D])
        self.dsa_in = self.din("dsa_w_in", [D, 3408])
        self.idx_nw = self.din("idx_nw", [128, 1])
        self.dsa_out = self.din("dsa_w_out", [D, D])
        self.cst_d = {k: self.din("c_" + k, shp) for k, shp in CONST_SHAPES.items()}
        self.yout = nc.dram_tensor("yT", [D, T], F32, kind="ExternalOutput").ap()
        self.xr = self.dscr("xr", [D, T], F32)
        self.xr_buf = Buf()
        self.xin_buf = Buf()
        self.yout_buf = Buf()
        self.hid_d = self.dscr("hid_d", [DFF, T], BF16)
        self.hid_buf = Buf()

        self.cs = {}
        self.cbuf = Buf(const=True)
        for k in ("ident", "ones", "bones", "triT", "inv", "pm_q", "pm_i", "cbias", "ret_kd"):
            shp = CONST_SHAPES[k]
            t = P.sb(list(shp), F32, "c_" + k)
            P.dma("sp", t[:], self.cst_d[k], writes=[self.cbuf])
            self.cs[k] = t
        self.eps_t = P.sb([128, 1], F32, "eps")
        P.memset("dve", self.eps_t[:], EPS, writes=[self.cbuf])
        self.ident_bf = P.sb([128, 128], BF16, "identbf")
        self.ones_bf = P.sb([128, 128], BF16, "onesbf")
        P.copy("dve", self.ident_bf[:], self.cs["ident"][:], reads=[self.cbuf], writes=[self.cbuf])
        P.copy("dve", self.ones_bf[:], self.cs["ones"][:], reads=[self.cbuf], writes=[self.cbuf])
        self.normw_sb = P.sb([128, 2, 6 * KC], F32, "normw")
        for i in range(2):
            P.dma("sp", self.normw_sb[:, i, :], self.norm_w[i], writes=[self.cbuf])
        self.mods_sb = P.sb([128, 2, 144], F32, "mods")
        self.mods_buf = Buf()
        self.cols = P.sb([128, 3, KC], F32, "cols")
        self.cols_buf = Buf()
        P.barrier()

        self.stage_mods()

        subs = [
            (0, 0, "ffn", 0), (0, 1, "hy", None), (0, 2, "ffn", 1),
            (1, 0, "ffn", 0), (1, 1, "dsa", None), (1, 2, "ffn", 1),
        ][: self.n_sub] if isinstance(self.n_sub, int) else list(self.n_sub)
        for si, (layer, j, kind, f) in enumerate(subs):
            src, sbuf_ = (self.xin, self.xin_buf) if si == 0 else (self.xr, self.xr_buf)
            last = si == len(subs) - 1
            dst, dbuf = (self.yout, self.yout_buf) if last else (self.xr, self.xr_buf)
            self.cur = dict(layer=layer, j=j, src=src, sbuf=sbuf_, dst=dst, dbuf=dbuf,
                            rw=0.5 if kind == "ffn" else 1.0)
            self.make_cols(layer, j)
            m = P.mark()
            hT, hbuf = self.stage_pre()
            if kind == "ffn":
                self.stage_ffn_up(hT, hbuf, layer, f)
                P.release(m)
                self.stage_down(self.hid_d, self.hid_buf, self.wd[layer, f], DFF // 128)
            elif kind == "hy":
                self.stage_hy(hT, hbuf, m)
            else:
                self.stage_dsa(hT, hbuf, m)
            P.release(m)
        P.barrier()

    def stage_mods(self):
        P = self.P
        cb = Buf()
        self.mods_cb = cb
        c_sb = P.sb([128, KC], F32, "c_sb")
        self.cond = P.sb([128, KC], F32, "cond")
        self.mb = P.sb([128, 2, 144], F32, "mb")
        P.dma("sp", c_sb[:], self.cin, writes=[cb])
        for i in range(2):
            P.dma("sp", self.mb[:, i, :], self.mod_b[i], writes=[cb])
        P.act(self.cond[:], c_sb[:], AF.Silu, reads=[cb], writes=[cb])
        m = P.mark()
        layers = [0] if self.overlap_mods else [0, 1]
        for layer in layers:
            for _ in self.mods_gen(layer, None):
                pass
        P.release(m)

    def mods_gen(self, layer, bank):
        P = self.P
        cb = self.mods_cb
        NB = 512 if bank is None else 128
        wt = [(P.sb([128, KC, NB], F32, "modw%d_%d" % (layer, i)), Buf()) for i in range(2)]
        for blk in range(9 * D // NB):
            w, wb = wt[blk % 2]
            P.dma("sp", w[:, :, :],
                  self.mod_w[layer, :, blk * NB:(blk + 1) * NB].rearrange("(k p) n -> p k n", p=128), writes=[wb])
            yield
            pt, pb = P.ps() if bank is None else bank
            for cc in range(NB // 128):
                for k in range(KC):
                    P.mm(pt[:, cc:cc + 1], lhsT=w[:, k, cc * 128:(cc + 1) * 128], rhs=self.cond[:, k:k + 1],
                         start=(k == 0), stop=(k == KC - 1), reads=[wb, cb], writes=[pb],
                         inc=(k == KC - 1))
                if bank is not None:
                    yield
            c0 = blk * (NB // 128)
            P.tt("dve", self.mods_sb[:, layer, c0:c0 + NB // 128], pt[:, 0:NB // 128],
                 self.mb[:, layer, c0:c0 + NB // 128], ALU.add, reads=[pb, cb], writes=[self.mods_buf])
            yield

    def make_cols(self, layer, j):
        P = self.P
        ms = self.mods_sb
        nw = self.normw_sb
        shift = ms[:, layer, (3 * j) * KC:(3 * j + 1) * KC]
        scale = ms[:, layer, (3 * j + 1) * KC:(3 * j + 2) * KC]
        gate = ms[:, layer, (3 * j + 2) * KC:(3 * j + 3) * KC]
        pre_w = nw[:, layer, (2 * j) * KC:(2 * j + 1) * KC]
        post_w = nw[:, layer, (2 * j + 1) * KC:(2 * j + 2) * KC]
        rd = [self.mods_buf, self.cbuf]
        P.stt(self.cols[:, 0, :], scale, 1.0, pre_w, ALU.add, ALU.mult, reads=rd, writes=[self.cols_buf])
        P.copy("dve", self.cols[:, 1, :], shift, reads=rd, writes=[self.cols_buf])
        P.stt(self.cols[:, 2, :], gate, self.cur["rw"], post_w, ALU.mult, ALU.mult, reads=rd, writes=[self.cols_buf])

    def rstd_from_ss(self, ss_ps, ss_buf, n, width, out_t, out_buf):
        P = self.P
        P.act(out_t, ss_ps, AF.Sqrt, reads=[ss_buf], writes=[out_buf], bias=self.eps_t[:, 0:1], scale=1.0 / n)
        P.op("dve", lambda en: en.reciprocal(out=out_t, in_=out_t), reads=[out_buf], writes=[out_buf])

    def stage_pre(self):
        P = self.P
        cur = self.cur
        hT = P.sb([128, KC, T], BF16, "hT")
        hbuf = [Buf() for _ in range(T // 512)]
        m = P.mark()
        xt = [(P.sb([128, KC, 512], F32, "xt%d" % i), Buf()) for i in range(2)]
        sq = [(P.sb([128, 512], F32, "sq%d" % i), Buf()) for i in range(2)]
        rs = [(P.sb([128, 512], F32, "rs%d" % i), Buf()) for i in range(2)]
        tmp = [(P.sb([128, 512], F32, "tmp%d" % i), Buf()) for i in range(2)]
        srcv = cur["src"].rearrange("(k p) t -> p k t", p=128)
        for tt in range(T // 512):
            x, xb = xt[tt % 2]
            P.dma("sp", x[:, :, :], srcv[:, :, tt * 512:(tt + 1) * 512], reads=[cur["sbuf"]], writes=[xb])
            pt, pb = P.ps()
            for k in range(KC):
                s, sbf = sq[k % 2]
                P.act(s[:], x[:, k, :], AF.Square, reads=[xb], writes=[sbf])
                P.mm(pt[:, :], lhsT=self.cs["ones"][:], rhs=s[:], start=(k == 0), stop=(k == KC - 1),
                     reads=[sbf, self.cbuf], writes=[pb], inc=True)
            r, rb = rs[tt % 2]
            self.rstd_from_ss(pt[:, :], pb, D, 512, r[:], rb)
            for k in range(KC):
                tm, tb = tmp[k % 2]
                P.tt("dve", tm[:], x[:, k, :], r[:], ALU.mult, reads=[xb, rb], writes=[tb])
                P.act(hT[:, k, tt * 512:(tt + 1) * 512], tm[:], AF.Identity, reads=[tb, self.cols_buf],
                      writes=[hbuf[tt]], bias=self.cols[:, 1, k:k + 1], scale=self.cols[:, 0, k:k + 1])
        P.release(m)
        return hT, hbuf

    def wload(self, w_t, w_buf, src_ap):
        self.P.dma("pool", w_t, src_ap, writes=[w_buf])

    def stage_ffn_up(self, hT, hbuf, layer, f):
        P = self.P
        CB = 256
        wgt = [(P.sb([128, KC, CB], BF16, "wg%d" % i), Buf()) for i in range(2)]
        wut = [(P.sb([128, KC, CB], BF16, "wu%d" % i), Buf()) for i in range(2)]
        ho = [(P.sb([128, T], BF16, "ho%d" % i), Buf()) for i in range(2)]
        sg = [(P.sb([128, 512], F32, "sg%d" % i), Buf()) for i in range(2)]
        Wg = self.wg[layer, f]
        Wu = self.wu[layer, f]
        nblk = DFF // CB
        it = 0
        for blk in range(nblk):
            g, gb = wgt[blk % 2]
            u, ub = wut[blk % 2]
            self.wload(g[:, :, :], gb, Wg[:, blk * CB:(blk + 1) * CB].rearrange("(k p) n -> p k n", p=128))
            self.wload(u[:, :, :], ub, Wu[:, blk * CB:(blk + 1) * CB].rearrange("(k p) n -> p k n", p=128))
            for cc in range(CB // 128):
                fc = blk * (CB // 128) + cc
                h_o, hob = ho[fc % 2]
                for tt in range(T // 512):
                    pg, pgb = P.ps()
                    pu, pub = P.ps()
                    for k in range(KC):
                        P.mm(pg[:, :], lhsT=g[:, k, cc * 128:(cc + 1) * 128], rhs=hT[:, k, tt * 512:(tt + 1) * 512],
                             start=(k == 0), stop=(k == KC - 1), reads=[gb, hbuf[tt]], writes=[pgb])
                    for k in range(KC):
                        P.mm(pu[:, :], lhsT=u[:, k, cc * 128:(cc + 1) * 128], rhs=hT[:, k, tt * 512:(tt + 1) * 512],
                             start=(k == 0), stop=(k == KC - 1), reads=[ub, hbuf[tt]], writes=[pub])
                    s, sb_ = sg[it % 2]
                    it += 1
                    P.act(s[:], pg[:, :], AF.Silu, reads=[pgb], writes=[sb_])
                    P.tt("dve", h_o[:, tt * 512:(tt + 1) * 512], s[:], pu[:, :], ALU.mult, reads=[sb_, pub],
                         writes=[hob])
                P.dma("sp", self.hid_d[fc * 128:(fc + 1) * 128, :], h_o[:, :], reads=[hob], writes=[self.hid_buf])

    def stage_down(self, act_d, act_buf, W, nkc):
        P = self.P
        cur = self.cur
        NT = 1024
        a_t = P.sb([128, nkc, NT], BF16, "a_t")
        a_bufs = [Buf() for _ in range(nkc)]
        wt = [(P.sb([128, nkc, 128], BF16, "wd%d" % i), Buf()) for i in range(2)]
        y = P.sb([128, KC, NT], F32, "ydown")
        ybufs = [Buf() for _ in range(KC)]
        sq = [(P.sb([128, 512], F32, "sqd%d" % i), Buf()) for i in range(2)]
        rs = P.sb([128, NT], F32, "rsd")
        rsb = Buf()
        xc = [(P.sb([128, NT], F32, "xc%d" % i), Buf()) for i in range(2)]
        actv = act_d.rearrange("(k p) t -> p k t", p=128)
        srcv = cur["src"].rearrange("(k p) t -> p k t", p=128)
        dstv = cur["dst"].rearrange("(k p) t -> p k t", p=128)
        it = 0
        for nt in range(T // NT):
            t0 = nt * NT
            for k in range(nkc):
                P.dma("sp", a_t[:, k, :], actv[:, k, t0:t0 + NT], reads=[act_buf], writes=[a_bufs[k]])
            for dc in range(KC):
                w, wb = wt[it % 2]
                it += 1
                self.wload(w[:, :, :], wb, W[:, dc * 128:(dc + 1) * 128].rearrange("(k p) n -> p k n", p=128))
                for hh in range(NT // 512):
                    pt, pb = P.ps()
                    for k in range(nkc):
                        P.mm(pt[:, :], lhsT=w[:, k, :], rhs=a_t[:, k, hh * 512:(hh + 1) * 512],
                             start=(k == 0), stop=(k == nkc - 1), reads=[wb, a_bufs[k]], writes=[pb])
                    P.copy("act" if hh == 0 else "dve", y[:, dc, hh * 512:(hh + 1) * 512], pt[:, :], reads=[pb],
                           writes=[ybufs[dc]])
            pss = [P.ps() for _ in range(NT // 512)]
            for k in range(KC):
                for hh in range(NT // 512):
                    s, sbf = sq[hh % 2]
                    P.act(s[:], y[:, k, hh * 512:(hh + 1) * 512], AF.Square, reads=[ybufs[k]], writes=[sbf])
                    P.mm(pss[hh][0][:, :], lhsT=self.cs["ones"][:], rhs=s[:],
                         start=(k == 0), stop=(k == KC - 1), reads=[sbf, self.cbuf], writes=[pss[hh][1]], inc=True)
            for hh in range(NT // 512):
                self.rstd_from_ss(pss[hh][0][:, :], pss[hh][1], D, 512, rs[:, hh * 512:(hh + 1) * 512], rsb)
            for k in range(KC):
                x, xb = xc[k % 2]
                o, ob = x, xb
                P.dma("sp", x[:], srcv[:, k, t0:t0 + NT], reads=[cur["sbuf"]], writes=[xb])
                P.tt("pool" if k % 2 else "dve", y[:, k, :], y[:, k, :], rs[:], ALU.mult, reads=[rsb],
                     writes=[ybufs[k]])
                P.stt(o[:], y[:, k, :], self.cols[:, 2, k:k + 1], x[:], ALU.mult, ALU.add,
                      reads=[ybufs[k], xb, self.cols_buf], writes=[ob])
                P.dma("act", dstv[:, k, t0:t0 + NT], o[:], reads=[ob], writes=[cur["dbuf"]])

    def stage_hy(self, hT, hbuf, m):
        P = self.P
        W = self.hy_in
        if not hasattr(self, "zs_d"):
            self.zs_d = self.dscr("zs_d", [D, T], F32)
            self.xs_d = self.dscr("xs_d", [D, T], BF16)
            self.B_d = self.dscr("B_d", [1024, T], BF16)
            self.C_d = self.dscr("C_d", [1024, T], BF16)
            self.rq_d = self.dscr("rq_d", [D, T], BF16)
            self.rk_d = self.dscr("rk_d", [D, T], BF16)
            self.rv_d = self.dscr("rv_d", [T, D], BF16)
            self.rg_d = self.dscr("rg_d", [D, T], F32)
            self.dt_d = self.dscr("dt_d", [128, T], F32)
            self.cum_d = self.dscr("cum_d", [128, T], F32)
            self.ym_d = self.dscr("ym_d", [4096, T], BF16)
        zsb, xsb, Bb, Cb, rqb, rkb, rvb, rgb, dtb, cumb, ymb = [Buf() for _ in range(11)]
        self.pw = [(P.sb([128, KC, 128], BF16, "pw%d" % i), Buf()) for i in range(2)]
        self.pw_i = 0
        rowf = [(P.sb([128, T], F32, "rowf%d" % i), Buf()) for i in range(2)]
        rowb = [(P.sb([128, T], BF16, "rowb%d" % i), Buf()) for i in range(2)]
        smallb = Buf()
        small = P.sb([128, 4], F32, "ssdsmall")
        P.dma("sp", small[:], self.ssd_small, writes=[smallb])
        mA = P.mark()
        cw = P.sb([128, 32 * 4], F32, "cw")
        cb_ = P.sb([128, 32], F32, "cb")
        P.dma("sp", cw[:], self.conv_w, writes=[smallb])
        P.dma("sp", cb_[:], self.conv_b, writes=[smallb])
        ci = [0]

        def silu_to(dst_d, dbuf, row0):
            rt, rb = rowf[ci[0] % 2]
            ci[0] += 1

            def h(tt, pt, pb):
                P.act(rt[:, tt * 512:(tt + 1) * 512], pt[:, :], AF.Silu, reads=[pb], writes=[rb])
            return h, (lambda: P.dma("sp", dst_d[row0:row0 + 128, :], rt[:, :], reads=[rb], writes=[dbuf]))

        for c in range(16):
            h, fin = silu_to(self.zs_d, zsb, c * 128)
            self.proj_fm(hT, hbuf, W, IN0_OFF["z"] + c * 128, 128, h)
            fin()
        for c in range(16):
            h, fin = silu_to(self.rg_d, rgb, c * 128)
            self.proj_fm(hT, hbuf, W, IN0_OFF["rg"] + c * 128, 128, h)
            fin()
        us = [(P.sb([128, T + 4], F32, "u%d" % i), Buf()) for i in range(2)]
        for u, ub in us:
            P.memset("pool", u[:, 0:3], 0.0, writes=[ub])
        cacc = P.sb([128, T], F32, "cacc")
        caccb = Buf()
        for c in range(32):
            u, ub = us[c % 2]
            rt, rb = rowb[c % 2]

            def hx(tt, pt, pb, u=u, ub=ub):
                P.copy("act", u[:, 3 + tt * 512:3 + (tt + 1) * 512], pt[:, :], reads=[pb], writes=[ub])
            self.proj_fm(hT, hbuf, W, IN0_OFF["xbc"] + c * 128, 128, hx)
            P.ts("dve", cacc[:], u[:, 0:T], cw[:, c * 4:c * 4 + 1], ALU.mult, cb_[:, c:c + 1], ALU.add,
                 reads=[ub, smallb], writes=[caccb])
            for i in range(1, 4):
                P.stt(cacc[:], u[:, i:i + T], cw[:, c * 4 + i:c * 4 + i + 1], cacc[:], ALU.mult, ALU.add,
                      reads=[ub, smallb, caccb], writes=[caccb])
            P.act(rt[:], cacc[:], AF.Silu, reads=[caccb], writes=[rb])
            if c < 16:
                P.dma("sp", self.xs_d[c * 128:(c + 1) * 128, :], rt[:, :], reads=[rb], writes=[xsb])
            elif c < 24:
                P.dma("sp", self.B_d[(c - 16) * 128:(c - 15) * 128, :], rt[:, :], reads=[rb], writes=[Bb])
            else:
                P.dma("sp", self.C_d[(c - 24) * 128:(c - 23) * 128, :], rt[:, :], reads=[rb], writes=[Cb])
        dtT, dtTb = rowf[0]
        cumT, cumTb = rowf[1]
        acol = P.sb([128, 1], F32, "acol")
        P.act(acol[:], small[:, 0:1], AF.Exp, reads=[smallb], writes=[smallb])
        P.ts("dve", acol[:], acol[:], -1.0, ALU.mult, reads=[smallb], writes=[smallb])

        def hdt(tt, pt, pb):
            sl = slice(tt * 512, (tt + 1) * 512)
            P.act(dtT[:, sl], pt[:, :], AF.Exp, reads=[pb, smallb], writes=[dtTb], bias=small[:, 1:2], scale=1.0)
            P.act(dtT[:, sl], dtT[:, sl], AF.Ln, reads=[dtTb, self.cbuf], writes=[dtTb],
                  bias=self.cs["ones"][:, 0:1], scale=1.0)
        self.proj_fm(hT, hbuf, W, IN0_OFF["dt"], 32, hdt)
        P.ts("dve", cacc[:], dtT[:], acol[:, 0:1], ALU.mult, reads=[dtTb, smallb], writes=[caccb])
        for c in range(16):
            sl = slice(c * 128, (c + 1) * 128)
            P.op("dve", lambda e, sl=sl: e.tensor_tensor_scan(out=cumT[:, sl], data0=self.cs["ones"][:, 0:128],
                                                             data1=cacc[:, sl], initial=0.0, op0=ALU.mult,
                                                             op1=ALU.add),
                 reads=[caccb, self.cbuf], writes=[cumTb])
        P.dma("sp", self.dt_d, dtT[:, :], reads=[dtTb], writes=[dtb])
        P.dma("sp", self.cum_d, cumT[:, :], reads=[cumTb], writes=[cumb])
        P.release(mA)
        cosr = P.sb([128, T], F32, "cosr")
        sinr = P.sb([128, T], F32, "sinr")
        trb = Buf()
        m2 = P.mark()
        pos_f, pb_ = self.load_pos()
        tmp = (P.sb([128, T], F32, "tv"), P.sb([128, T], I32, "tvi"), P.sb([128, T], F32, "tvf"), Buf())
        self.trig(cosr, trb, pos_f, pb_, self.cs["inv"][:, 0:1], 0.25, tmp)
        self.trig(sinr, trb, pos_f, pb_, self.cs["inv"][:, 0:1], 0.0, tmp)
        P.release(m2)
        t1 = P.sb([128, T], F32, "rt1")
        t2 = P.sb([128, T], F32, "rt2")
        t12b = Buf()
        rowf4 = rowf + [(P.sb([128, T], F32, "rowf%d" % i), Buf()) for i in (2, 3)]
        hq = 0
        for name, dst, dbuf in (("rq", self.rq_d, rqb), ("rk", self.rk_d, rkb)):
            for hh in range(8):
                x1, x1b = rowf4[(hq % 2) * 2]
                x2, x2b = rowf4[(hq % 2) * 2 + 1]
                hq += 1

                def h1(tt, pt, pb):
                    P.copy("act", x1[:, tt * 512:(tt + 1) * 512], pt[:, :], reads=[pb], writes=[x1b])

                def h2(tt, pt, pb):
                    P.copy("act", x2[:, tt * 512:(tt + 1) * 512], pt[:, :], reads=[pb], writes=[x2b])
                self.proj_fm(hT, hbuf, W, IN0_OFF[name] + hh * 256, 128, h1)
                self.proj_fm(hT, hbuf, W, IN0_OFF[name] + hh * 256 + 128, 128, h2)
                o1, o1b = rowb[0]
                o2, o2b = rowb[1]
                P.tt("dve", t1[:], x1[:], cosr[:], ALU.mult, reads=[x1b, trb], writes=[t12b])
                P.tt("pool", t2[:], x2[:], sinr[:], ALU.mult, reads=[x2b, trb], writes=[t12b])
                P.tt("dve", o1[:], t1[:], t2[:], ALU.subtract, reads=[t12b], writes=[o1b])
                P.tt("dve", t1[:], x1[:], sinr[:], ALU.mult, reads=[x1b, trb, o1b], writes=[t12b])
                P.tt("pool", t2[:], x2[:], cosr[:], ALU.mult, reads=[x2b, trb, o1b], writes=[t12b])
                P.tt("dve", o2[:], t1[:], t2[:], ALU.add, reads=[t12b], writes=[o2b])
                P.dma("sp", dst[hh * 256:hh * 256 + 128, :], o1[:, :], reads=[o1b], writes=[dbuf])
                P.dma("sp", dst[hh * 256 + 128:hh * 256 + 256, :], o2[:, :], reads=[o2b], writes=[dbuf])
        wv = P.sb([128, KC, 512], BF16, "wv")
        wvb = Buf()
        vo = [(P.sb([128, 512], BF16, "vo%d" % i), Buf()) for i in range(2)]
        for cbk in range(4):
            c0 = IN0_OFF["rv"] + cbk * 512
            self.wload(wv[:, :, :], wvb, W[:, c0:c0 + 512].rearrange("(k p) n -> p k n", p=128))
            for t16 in range(16):
                pt, pb = P.ps()
                for k in range(KC):
                    P.mm(pt[:, :], lhsT=hT[:, k, t16 * 128:(t16 + 1) * 128], rhs=wv[:, k, :], start=(k == 0),
                         stop=(k == KC - 1), reads=[wvb, hbuf[t16 // 4]], writes=[pb])
                v_, vb_ = vo[t16 % 2]
                P.copy("act" if t16 % 2 else "dve", v_[:], pt[:, :], reads=[pb], writes=[vb_])
                P.dma("sp", self.rv_d[t16 * 128:(t16 + 1) * 128, cbk * 512:(cbk + 1) * 512], v_[:, :], reads=[vb_],
                      writes=[rvb])
        P.release(m)

        ld = lambda shp, dt, nm: P.sb(shp, dt, nm)
        selh = ld([128, 32 * 128], F32, "selh")
        expd = ld([128, 2048], F32, "expd")
        Dc = ld([128, 16], F32, "Dc")
        snw = ld([128, 16], F32, "snw")
        cbf = Buf()
        P.dma("sp", selh[:], self.cst_d["selh"], writes=[cbf])
        P.dma("sp", expd[:], self.cst_d["expand"], writes=[cbf])
        P.dma("sp", Dc[:], self.ssd_Dc, writes=[cbf])
        P.dma("sp", snw[:], self.ssd_nw, writes=[cbf])
        S = ld([128, 2048], F32, "S")
        Sbf_e = ld([128, 16, 128], BF16, "Sbfe")
        Sbf_o = ld([128, 16, 128], BF16, "Sbfo")
        xdt_e = ld([128, 16, 128], BF16, "xdte")
        xdt_o = ld([128, 16, 128], BF16, "xdto")
        xdtw = ld([128, 2048], BF16, "xdtw")
        Btm = ld([128, 8, 128], BF16, "Btm")
        Sb, xdb, Btb = Buf(), Buf(), Buf()
        for t_ in (S, Sbf_e, Sbf_o, xdt_e, xdt_o):
            P.memset("pool", t_[:], 0.0, writes=[Sb])
        ach = ld([128, 2048], F32, "ach")
        achb = Buf()
        D2 = ld([128, 2048], F32, "D2")
        yraw = ld([128, 16, 128], F32, "yraw")
        yrb = Buf()
        sqy = ld([128, 2048], F32, "sqy")
        sqb = Buf()
        yab = ld([128, 16, 128], BF16, "yab")
        yabb = Buf()
        sms = [{k: ld([128, 32], F32, k + str(i)) for k in ("dttm", "cumc", "negc", "ds", "w2", "D1")} for i in range(2)]
        smbs = [Buf(), Buf()]
        CBm = [(ld([128, 128], F32, "CBm%d" % i), Buf()) for i in range(3)]
        NH = 4
        hb = [dict(arg=ld([128, 128], F32, "arg%d" % i), E=ld([128, 128], F32, "E%d" % i),
                   G=ld([128, 128], BF16, "G%d" % i), ec=ld([128, 128], F32, "ec%d" % i),
                   Cd=ld([128, 128], BF16, "Cd%d" % i), argb=Buf(), Eb=Buf(), Gb=Buf(), ecb=Buf(), Cdb=Buf())
              for i in range(NH)]
        yraws = [(yraw, yrb), (ld([128, 16, 128], F32, "yraw2"), Buf())]
        rstd = ld([128, 128], F32, "rstdy")
        lds = []
        for i in range(2):
            lds.append(dict(BT=ld([128, 8, 128], BF16, "BT%d" % i),
                            CT=ld([128, 8, 128], BF16, "CT%d" % i),
                            dt=ld([128, 128], F32, "dtc%d" % i), cum=ld([128, 128], F32, "cumc%d" % i), b=Buf()))
        eds = [dict(xsT=ld([128, 16, 128], BF16, "xsT%d" % i), zs=ld([128, 16, 128], F32, "zs%d" % i), b=Buf())
               for i in range(3)]
        xdt_e2 = ld([128, 16, 128], BF16, "xdte2")
        xdt_o2 = ld([128, 16, 128], BF16, "xdto2")
        xdtw2 = ld([128, 2048], BF16, "xdtw2")
        Btm2 = ld([128, 8, 128], BF16, "Btm2")
        ach2 = ld([128, 2048], F32, "ach2")
        xdb2, Btb2, achb2 = Buf(), Buf(), Buf()
        for t_ in (xdt_e2, xdt_o2):
            P.memset("pool", t_[:], 0.0, writes=[xdb2])
        pro = [dict(xe=xdt_e, xo=xdt_o, xw=xdtw, xb=xdb, Bt=Btm, Btb=Btb, ach=ach, achb=achb),
               dict(xe=xdt_e2, xo=xdt_o2, xw=xdtw2, xb=xdb2, Bt=Btm2, Btb=Btb2, ach=ach2, achb=achb2)]
        D2b = Buf()
        iy0, (yp0, yp0b) = P.ps_reserve()
        iy1, (yp1, yp1b) = P.ps_reserve()
        yps = [(yp0, yp0b), (yp1, yp1b)]
        mgen = None
        if self.overlap_mods:
            imods, mods_bank = P.ps_reserve()
            mgen = self.mods_gen(1, mods_bank)
        ident = self.cs["ident"]
        ones = self.cs["ones"]
        cnt = dict(h=0, g=0)

        def prologue(c):
            L = lds[c % 2]
            lb = L["b"]
            sm = sms[c % 2]
            smb = smbs[c % 2]
            Q = pro[c % 2]
            sl = slice(c * 128, (c + 1) * 128)
            E = eds[c % 3]
            eb = E["b"]
            P.dma("sp", E["xsT"][:, :, :], self.xs_d[:, sl].rearrange("(k p) t -> p k t", p=128), reads=[xsb], writes=[eb])
            P.dma("sp", L["BT"][:, :, :], self.B_d[:, sl].rearrange("(k p) t -> p k t", p=128), reads=[Bb, lb], writes=[lb])
            P.dma("sp", L["CT"][:, :, :], self.C_d[:, sl].rearrange("(k p) t -> p k t", p=128), reads=[Cb, lb], writes=[lb])
            P.dma("sp", E["zs"][:, :, :], self.zs_d[:, sl].rearrange("(k p) t -> p k t", p=128), reads=[zsb, eb], writes=[eb])
            P.dma("sp", L["dt"][:, :], self.dt_d[:, sl], reads=[dtb, lb], writes=[lb])
            P.dma("sp", L["cum"][:, :], self.cum_d[:, sl], reads=[cumb, lb], writes=[lb])
            pt, pb = P.ps()
            P.tr(pt[:, 0:128], L["dt"][:, :], ident[:], reads=[lb, self.cbuf], writes=[pb], inc=False)
            P.tr(pt[:, 128:256], L["cum"][:, :], ident[:], reads=[lb, self.cbuf], writes=[pb])
            P.ts("dve", sm["D1"][:], ident[:, 0:32], L["cum"][:, 127:128], ALU.mult, reads=[lb, self.cbuf, smb],
                 writes=[smb])
            pt1, pb1 = P.ps()
            P.mm(pt1[:, 0:32], lhsT=ones[:], rhs=sm["D1"][:], start=True, stop=True, reads=[smb, self.cbuf], writes=[pb1])
            P.copy("dve", sm["dttm"][:], pt[:, 0:32], reads=[pb], writes=[smb])
            P.copy("dve", sm["cumc"][:], pt[:, 128:160], reads=[pb, smb], writes=[smb])
            P.ts("dve", sm["negc"][:], sm["cumc"][:], -1.0, ALU.mult, reads=[smb], writes=[smb])
            P.tt("dve", sm["ds"][:], pt1[:, 0:32], sm["cumc"][:], ALU.subtract, reads=[pb1, smb], writes=[smb])
            P.act(sm["ds"][:], sm["ds"][:], AF.Exp, reads=[smb], writes=[smb])
            P.tt("dve", sm["w2"][:], sm["ds"][:], sm["dttm"][:], ALU.mult, reads=[smb], writes=[smb])
            P.ts("dve", D2[:], expd[:], L["cum"][:, 127:128], ALU.mult, reads=[lb, cbf], writes=[D2b])
            pds = []
            for q4 in range(4):
                ptd, pbd = P.ps()
                P.mm(ptd[:, :], lhsT=ones[:], rhs=D2[:, q4 * 512:(q4 + 1) * 512], start=True, stop=True,
                     reads=[D2b, self.cbuf], writes=[pbd])
                pds.append((ptd, pbd))
            for q4 in range(4):
                ptd, pbd = pds[q4]
                P.act(Q["ach"][:, q4 * 512:(q4 + 1) * 512], ptd[:, :], AF.Exp, reads=[pbd], writes=[Q["achb"]])
            for q4 in range(4):
                ptx, pbx = P.ps()
                for i4 in range(4):
                    P.tr(ptx[:, i4 * 128:(i4 + 1) * 128], E["xsT"][:, q4 * 4 + i4, :], self.ident_bf[:],
                         reads=[eb, self.cbuf], writes=[pbx], inc=(i4 == 3))
                src = ptx[:, 0:512].rearrange("p (a b d) -> p a b d", a=4, b=2)
                dtv = sm["dttm"][:, q4 * 8:(q4 + 1) * 8].rearrange("p (a b) -> p a b", b=2)
                w2v = sm["w2"][:, q4 * 8:(q4 + 1) * 8].rearrange("p (a b) -> p a b", b=2)
                xwv = Q["xw"][:, q4 * 512:(q4 + 1) * 512].rearrange("p (a b d) -> p a b d", a=4, b=2)
                P.tt("dve", Q["xe"][:, q4 * 4:(q4 + 1) * 4, 0:64], src[:, :, 0, :],
                     dtv[:, :, 0].unsqueeze(2).broadcast_to([128, 4, 64]), ALU.mult, reads=[pbx, smb], writes=[Q["xb"]])
                P.tt("dve", Q["xo"][:, q4 * 4:(q4 + 1) * 4, 64:128], src[:, :, 1, :],
                     dtv[:, :, 1].unsqueeze(2).broadcast_to([128, 4, 64]), ALU.mult, reads=[pbx, smb], writes=[Q["xb"]])
                for par in range(2):
                    P.tt("dve", xwv[:, :, par, :], src[:, :, par, :],
                         w2v[:, :, par].unsqueeze(2).broadcast_to([128, 4, 64]), ALU.mult, reads=[pbx, smb],
                         writes=[Q["xb"]])
            for q2 in range(2):
                ptx, pbx = P.ps()
                for i4 in range(4):
                    P.tr(ptx[:, i4 * 128:(i4 + 1) * 128], L["BT"][:, q2 * 4 + i4, :], self.ident_bf[:],
                         reads=[lb, self.cbuf], writes=[pbx], inc=(i4 == 3))
                P.copy("act", Q["Bt"][:, q2 * 4:(q2 + 1) * 4, :], ptx[:, 0:512].rearrange("p (a d) -> p a d", a=4),
                       reads=[pbx], writes=[Q["Btb"]])

        def heads(c, gens=()):
            L = lds[c % 2]
            lb = L["b"]
            sm = sms[c % 2]
            smb = smbs[c % 2]
            Q = pro[c % 2]
            live = {}
            cms = {}

            def front(h):
                g = h // 4
                if h % 4 == 0:
                    cm, cmb = CBm[cnt["g"] % 3]
                    cnt["g"] += 1
                    pt, pb = P.ps()
                    P.mm(pt[:, 0:128], lhsT=L["BT"][:, g, :], rhs=L["CT"][:, g, :], start=True, stop=True, reads=[lb],
                         writes=[pb])
                    P.tt("dve", cm[:], pt[:, 0:128], self.cs["triT"][:], ALU.mult, reads=[pb, self.cbuf], writes=[cmb])
                    cms[g] = (cm, cmb)
                cm, cmb = cms[g]
                H = hb[cnt["h"] % NH]
                cnt["h"] += 1
                pt, pb = P.ps()
                P.mm(pt[:, 0:128], lhsT=selh[:, h * 128:(h + 1) * 128], rhs=L["cum"][:, :], start=True, stop=True,
                     reads=[lb, cbf], writes=[pb])
                P.ts("dve", H["arg"][:], pt[:, 0:128], sm["negc"][:, h:h + 1], ALU.add, 0.0, ALU.min,
                     reads=[pb, smb], writes=[H["argb"]])
                P.act(H["ec"][:], pt[:, 0:128], AF.Exp, reads=[pb, H["argb"]], writes=[H["ecb"]])
                P.act(H["E"][:], H["arg"][:], AF.Exp, reads=[H["argb"]], writes=[H["Eb"]])
                P.tt("pool", H["G"][:], H["E"][:], cm[:], ALU.mult, reads=[H["Eb"], cmb], writes=[H["Gb"]])
                P.tt("dve", H["Cd"][:], L["CT"][:, g, :], H["ec"][:], ALU.mult, reads=[lb, H["ecb"]], writes=[H["Cdb"]])
                live[h] = H

            def back(h):
                g = h // 4
                H = live.pop(h)
                yp, ypb = yps[g % 2]
                xd = Q["xe"] if h % 2 == 0 else Q["xo"]
                sb_ = Sbf_e if h % 2 == 0 else Sbf_o
                col = ((h // 2) % 2) * 128
                P.mm(yp[:, col:col + 128], lhsT=xd[:, h // 2, :], rhs=H["G"][:], start=(h % 2 == 0), stop=False,
                     reads=[Q["xb"], H["Gb"]], writes=[ypb], inc=False)
                P.mm(yp[:, col:col + 128], lhsT=sb_[:, h // 2, :], rhs=H["Cd"][:], start=False, stop=(h % 2 == 1),
                     reads=[Sb, H["Cdb"]], writes=[ypb], inc=True)
                if h % 4 == 3:
                    yr_, yrb_ = yraws[c % 2]
                    P.copy("act", yr_[:, 2 * g:2 * g + 2, :], yp[:, 0:256].rearrange("p (a d) -> p a d", a=2),
                           reads=[ypb], writes=[yrb_])
                for gen in gens:
                    next(gen, None)

            LA = NH - 2
            for i in range(32 + LA):
                if i < 32:
                    front(i)
                if i - LA >= 0:
                    back(i - LA)

        def update(c):
            Q = pro[c % 2]
            pts = []
            for q4 in range(4):
                pt, pb = P.ps()
                for gg in range(2):
                    g = q4 * 2 + gg
                    P.mm(pt[:, gg * 256:(gg + 1) * 256], lhsT=Q["Bt"][:, g, :], rhs=Q["xw"][:, g * 256:(g + 1) * 256],
                         start=True, stop=True, reads=[Q["Btb"], Q["xb"]], writes=[pb], inc=(gg == 1))
                pts.append((pt, pb))
            for q4 in range(4):
                pt, pb = pts[q4]
                sl4 = slice(q4 * 512, (q4 + 1) * 512)
                P.tt("dve", S[:, sl4], S[:, sl4], Q["ach"][:, sl4], ALU.mult, reads=[Q["achb"], Sb], writes=[Sb])
                P.tt("dve", S[:, sl4], S[:, sl4], pt[:, :], ALU.add, reads=[pb, Sb], writes=[Sb])
            Sv = S[:, :].rearrange("p (a b d) -> p a b d", a=16, b=2)
            P.copy("pool", Sbf_e[:, :, 0:64], Sv[:, :, 0, :], reads=[Sb], writes=[Sb])
            P.copy("pool", Sbf_o[:, :, 64:128], Sv[:, :, 1, :], reads=[Sb], writes=[Sb])

        def epilogue(c):
            E = eds[c % 3]
            eb = E["b"]
            yraw, yrb = yraws[c % 2]
            sl = slice(c * 128, (c + 1) * 128)
            for k in range(16):
                P.stt(yraw[:, k, :], E["xsT"][:, k, :], Dc[:, k:k + 1], yraw[:, k, :], ALU.mult, ALU.add,
                      reads=[eb, cbf, yrb], writes=[yrb])
                if k % 4 == 3:
                    yield
            yflat = yraw[:, :, :].rearrange("p a d -> p (a d)")
            P.tt("dve", yflat, yflat, E["zs"][:, :, :].rearrange("p a d -> p (a d)"), ALU.mult, reads=[eb, yrb],
                 writes=[yrb])
            yield
            P.act(sqy[:], yflat, AF.Square, reads=[yrb], writes=[sqb])
            yield
            pt, pb = P.ps()
            for k in range(16):
                P.mm(pt[:, 0:128], lhsT=ones[:], rhs=sqy[:, k * 128:(k + 1) * 128], start=(k == 0), stop=(k == 15),
                     reads=[sqb, self.cbuf], writes=[pb])
            rb_ = Buf()
            self.rstd_from_ss(pt[:, 0:128], pb, 2048, 128, rstd[:], rb_)
            yield
            P.tt("dve", yraw[:, :, :], yraw[:, :, :], rstd[:, :].unsqueeze(1).broadcast_to([128, 16, 128]), ALU.mult,
                 reads=[rb_, yrb], writes=[yrb])
            yield
            for k in range(16):
                P.act(yab[:, k, :], yraw[:, k, :], AF.Copy, reads=[yrb, cbf], writes=[yabb], scale=snw[:, k:k + 1])
                if k % 4 == 3:
                    yield
            P.dma("sp", self.ym_d[0:2048, sl].rearrange("(k p) t -> p k t", p=128), yab[:, :, :], reads=[yabb],
                  writes=[ymb])

        prologue(0)
        prev_ep = None
        for c in range(16):
            if c + 1 < 16:
                prologue(c + 1)
            gl = ([mgen] if mgen is not None else []) + ([prev_ep] if prev_ep is not None else [])
            heads(c, gens=gl)
            if prev_ep is not None:
                for _ in prev_ep:
                    pass
            update(c)
            prev_ep = epilogue(c)
        for _ in prev_ep:
            pass
        if mgen is not None:
            for _ in mgen:
                pass
            P.ps_unreserve(imods)
        P.ps_unreserve(iy0)
        P.ps_unreserve(iy1)
        P.release(m)

        dm = ld([128, 8, 128], F32, "dm")
        qdt = ld([128, 8, 128], F32, "qdt")
        rnw = ld([128, 16], F32, "rnw")
        cbf = Buf()
        P.dma("sp", dm[:, :, :].rearrange("p a d -> p (a d)"), self.cst_d["ret_dm"], writes=[cbf])
        P.dma("sp", qdt[:, :, :].rearrange("p a d -> p (a d)"), self.cst_d["ret_qd"], writes=[cbf])
        P.dma("sp", rnw[:], self.ret_nw, writes=[cbf])
        kd = self.cs["ret_kd"]
        cdv = host_consts()["_ret_cd"]
        R = ld([128, 8, 512], F32, "R")
        Rbf = ld([128, 8, 512], BF16, "Rbf")
        Rb = Buf()
        P.memset("pool", R[:], 0.0, writes=[Rb])
        P.memset("pool", Rbf[:], 0.0, writes=[Rb])
        yraw = ld([128, 16, 128], F32, "yrawr")
        yrb = Buf()
        sqy = ld([128, 2048], F32, "sqyr")
        sqb = Buf()
        yab = ld([128, 16, 128], BF16, "yabr")
        yabb = Buf()
        rstd8 = ld([128, 8, 128], F32, "rstd8")
        hb = [dict(G=ld([128, 128], BF16, "rG%d" % i), qd=ld([128, 2, 128], BF16, "rqd%d" % i),
                   kw=ld([128, 256], BF16, "rkw%d" % i), Gb=Buf(), qdb=Buf(), kwb=Buf()) for i in range(4)]
        cnt = dict(h=0)
        lds = []
        for i in range(3):
            lds.append(dict(qT=ld([128, 16, 128], BF16, "rqT%d" % i), kT=ld([128, 16, 128], BF16, "rkT%d" % i),
                            v=ld([128, 2048], BF16, "rv%d" % i), gs=ld([128, 16, 128], F32, "rgs%d" % i), b=Buf()))
        yraws = [(yraw, yrb), (ld([128, 16, 128], F32, "yrawr2"), Buf())]
        iy0, (yp0, yp0b) = P.ps_reserve()
        iy1, (yp1, yp1b) = P.ps_reserve()
        yps = [(yp0, yp0b), (yp1, yp1b)]

        def loads(c):
            L = lds[c % 3]
            lb = L["b"]
            sl = slice(c * 128, (c + 1) * 128)
            P.dma("sp", L["qT"][:, :, :], self.rq_d[:, sl].rearrange("(k p) t -> p k t", p=128), reads=[rqb], writes=[lb])
            P.dma("sp", L["kT"][:, :, :], self.rk_d[:, sl].rearrange("(k p) t -> p k t", p=128), reads=[rkb, lb], writes=[lb])
            P.dma("sp", L["v"][:, :], self.rv_d[sl, :], reads=[rvb, lb], writes=[lb])
            P.dma("sp", L["gs"][:, :, :], self.rg_d[:, sl].rearrange("(k p) t -> p k t", p=128), reads=[rgb, lb], writes=[lb])

        def rheads(c, gens=()):
            L = lds[c % 3]
            lb = L["b"]
            yraw_, yrb_ = yraws[c % 2]
            live = {}

            def front(h):
                H = hb[cnt["h"] % 4]
                cnt["h"] += 1
                pt, pb = P.ps()
                P.mm(pt[:, 0:128], lhsT=L["kT"][:, 2 * h, :], rhs=L["qT"][:, 2 * h, :], start=True, stop=False,
                     reads=[lb], writes=[pb], inc=False)
                P.mm(pt[:, 0:128], lhsT=L["kT"][:, 2 * h + 1, :], rhs=L["qT"][:, 2 * h + 1, :], start=False, stop=True,
                     reads=[lb], writes=[pb])
                pt2, pb2 = P.ps()
                P.tr(pt2[:, 0:128], L["kT"][:, 2 * h, :], self.ident_bf[:], reads=[lb, self.cbuf], writes=[pb2], inc=False)
                P.tr(pt2[:, 128:256], L["kT"][:, 2 * h + 1, :], self.ident_bf[:], reads=[lb, self.cbuf], writes=[pb2])
                P.tt("dve", H["G"][:], pt[:, 0:128], dm[:, h, :], ALU.mult, reads=[pb, cbf], writes=[H["Gb"]])
                P.tt("pool", H["qd"][:, :, :], L["qT"][:, 2 * h:2 * h + 2, :],
                     qdt[:, h, :].unsqueeze(1).broadcast_to([128, 2, 128]), ALU.mult, reads=[lb, cbf], writes=[H["qdb"]])
                P.ts("dve", H["kw"][:], pt2[:, 0:256], kd[:, h:h + 1], ALU.mult, reads=[pb2, self.cbuf], writes=[H["kwb"]])
                live[h] = H

            def back(h):
                H = live.pop(h)
                yp, ypb = yps[h % 2]
                for vc in range(2):
                    col = vc * 128
                    P.mm(yp[:, col:col + 128], lhsT=L["v"][:, h * 256 + vc * 128:h * 256 + (vc + 1) * 128], rhs=H["G"][:],
                         start=True, stop=False, reads=[lb, H["Gb"]], writes=[ypb], inc=False)
                    P.mm(yp[:, col:col + 128], lhsT=Rbf[:, h, vc * 128:(vc + 1) * 128], rhs=H["qd"][:, 0, :],
                         start=False, stop=False, reads=[Rb, H["qdb"]], writes=[ypb], inc=False)
                    P.mm(yp[:, col:col + 128], lhsT=Rbf[:, h, 256 + vc * 128:256 + (vc + 1) * 128], rhs=H["qd"][:, 1, :],
                         start=False, stop=True, reads=[Rb, H["qdb"]], writes=[ypb], inc=True)
                pt3, pb3 = P.ps()
                for dc in range(2):
                    P.mm(pt3[:, dc * 256:(dc + 1) * 256], lhsT=H["kw"][:, dc * 128:(dc + 1) * 128],
                         rhs=L["v"][:, h * 256:(h + 1) * 256], start=True, stop=True, reads=[lb, H["kwb"]], writes=[pb3],
                         inc=(dc == 1))
                P.copy("act", yraw_[:, 2 * h:2 * h + 2, :], yp[:, 0:256].rearrange("p (a d) -> p a d", a=2),
                       reads=[ypb], writes=[yrb_])
                P.stt(R[:, h, :], R[:, h, :], float(cdv[h]), pt3[:, :], ALU.mult, ALU.add, reads=[pb3, Rb], writes=[Rb])
                P.copy("pool", Rbf[:, h, :], R[:, h, :], reads=[Rb], writes=[Rb])
                for gen in gens:
                    next(gen, None)
                    next(gen, None)

            LA = 2
            for i in range(8 + LA):
                if i < 8:
                    front(i)
                if i - LA >= 0:
                    back(i - LA)

        def repilogue(c):
            L = lds[c % 3]
            lb = L["b"]
            yraw_, yrb_ = yraws[c % 2]
            sl = slice(c * 128, (c + 1) * 128)
            yflat = yraw_[:, :, :].rearrange("p a d -> p (a d)")
            P.act(sqy[:], yflat, AF.Square, reads=[yrb_], writes=[sqb])
            yield
            for q2 in range(2):
                pt, pb = P.ps()
                for hh in range(4):
                    h = q2 * 4 + hh
                    for vc in range(2):
                        P.mm(pt[:, hh * 128:(hh + 1) * 128], lhsT=ones[:],
                             rhs=sqy[:, (2 * h + vc) * 128:(2 * h + vc + 1) * 128], start=(vc == 0), stop=(vc == 1),
                             reads=[sqb, self.cbuf], writes=[pb], inc=(vc == 1 and hh == 3))
                rb_ = Buf()
                self.rstd_from_ss(pt[:, :], pb, 256, 512, rstd8[:, q2 * 4:(q2 + 1) * 4, :].rearrange("p a d -> p (a d)"),
                                  rb_)
                yv = yraw_[:, q2 * 8:(q2 + 1) * 8, :].rearrange("p (a b) d -> p a b d", b=2)
                for vc in range(2):
                    P.tt("dve", yv[:, :, vc, :], yv[:, :, vc, :], rstd8[:, q2 * 4:(q2 + 1) * 4, :], ALU.mult,
                         reads=[rb_, yrb_], writes=[yrb_])
                yield
            P.tt("dve", yflat, yflat, L["gs"][:, :, :].rearrange("p a d -> p (a d)"), ALU.mult, reads=[lb, yrb_],
                 writes=[yrb_])
            yield
            for k in range(16):
                P.act(yab[:, k, :], yraw_[:, k, :], AF.Copy, reads=[yrb_, cbf], writes=[yabb], scale=rnw[:, k:k + 1])
                if k % 4 == 3:
                    yield
            P.dma("sp", self.ym_d[2048:4096, sl].rearrange("(k p) t -> p k t", p=128), yab[:, :, :], reads=[yabb],
                  writes=[ymb])

        loads(0)
        prev = None
        for c in range(16):
            if c + 1 < 16:
                loads(c + 1)
            rheads(c, gens=[prev] if prev is not None else [])
            if prev is not None:
                for _ in prev:
                    pass
            prev = repilogue(c)
        for _ in prev:
            pass
        P.ps_unreserve(iy0)
        P.ps_unreserve(iy1)
        P.release(m)
        self.stage_down(self.ym_d, ymb, self.hy_out, 32)

    def load_pos(self):
        P = self.P
        pb = Buf()
        pos_i = P.sb([128, T], I32, "pos_i")
        pos_f = P.sb([128, T], F32, "pos_f")
        P.dma("sp", pos_i[:], self.posin.partition_broadcast(128), writes=[pb])
        P.copy("dve", pos_f[:], pos_i[:], reads=[pb], writes=[pb])
        return pos_f, pb

    def trig(self, out_t, out_buf, pos_f, pb, invcol, phase, tmp):
        P = self.P
        v, vi, vf, tb = tmp
        INV2PI = float(1.0 / (2 * np.pi))
        P.ts("dve", v[:], pos_f[:], invcol, ALU.mult, reads=[pb, self.cbuf], writes=[tb])
        P.ts("dve", v[:], v[:], INV2PI, ALU.mult, float(phase), ALU.add, reads=[tb], writes=[tb])
        P.copy("dve", vi[:], v[:], reads=[tb], writes=[tb])
        P.copy("dve", vf[:], vi[:], reads=[tb], writes=[tb])
        P.tt("dve", v[:], v[:], vf[:], ALU.subtract, reads=[tb], writes=[tb])
        P.stt(v[:], v[:], 0.5, v[:], ALU.is_gt, ALU.subtract, reads=[tb], writes=[tb])
        P.stt(v[:], v[:], 0.5, v[:], ALU.is_gt, ALU.subtract, reads=[tb], writes=[tb])
        P.act(out_t[:], v[:], AF.Sin, reads=[tb], writes=[out_buf], scale=float(2 * np.pi))

    def proj_fm(self, hT, hbuf, W, col0, ncols, handler, dup=False):
        P = self.P
        w, wb = self.pw[self.pw_i % 2]
        self.pw_i += 1
        if ncols < 128:
            P.memset("pool", w[:, :, :], 0.0, writes=[wb])
        self.wload(w[:, :, 0:ncols], wb, W[:, col0:col0 + ncols].rearrange("(k p) n -> p k n", p=128))
        if dup:
            wb2 = Buf()
            P.dma("pool", w[:, :, ncols:2 * ncols], W[:, col0:col0 + ncols].rearrange("(k p) n -> p k n", p=128),
                  writes=[wb2])
        prev = None
        for tt in range(T // 512):
            pt, pb = P.ps()
            for k in range(KC):
                P.mm(pt[:, :], lhsT=w[:, k, :], rhs=hT[:, k, tt * 512:(tt + 1) * 512], start=(k == 0),
                     stop=(k == KC - 1), reads=[wb, hbuf[tt]] + ([wb2] if dup else []), writes=[pb])
            if prev is not None:
                handler(*prev)
            prev = (tt, pt, pb)
        handler(*prev)

    def rope_fm(self, xf, xfb, pm, cos_t, sin_t, tbuf, tt, out_ap, out_buf, tmp2):
        P = self.P
        pt, pb = P.ps()
        P.mm(pt[:, :], lhsT=pm[:], rhs=xf[:], start=True, stop=True, reads=[xfb, self.cbuf], writes=[pb])
        t1, t2, t12b = tmp2
        sl = slice(tt * 512, (tt + 1) * 512)
        P.tt("pool", t1[:], xf[:], cos_t[:, sl], ALU.mult, reads=[xfb, tbuf], writes=[t12b])
        P.tt("dve", t2[:], pt[:, :], sin_t[:, sl], ALU.mult, reads=[pb, tbuf], writes=[t12b])
        P.tt("dve", out_ap, t1[:], t2[:], ALU.add, reads=[t12b], writes=[out_buf])

    def stage_dsa(self, hT, hbuf, m):
        P = self.P
        W = self.dsa_in
        if not hasattr(self, "q_d"):
            self.q_d = self.dscr("q_d", [D, T], BF16)
            self.qi_d = self.dscr("qi_d", [1024, T], F32)
            self.ao_d = self.dscr("ao_d", [D, T], BF16)
        qdb, qidb, aob = Buf(), Buf(), Buf()
        kT = P.sb([128, T], BF16, "kT")
        vtm = P.sb([128, 16, 128], BF16, "vtm")
        kiA = P.sb([128, T], F32, "kiA")
        kiB = P.sb([128, T], F32, "kiB")
        witm = P.sb([128, 16, 16], F32, "witm")
        resb = Buf()
        idxnw = P.sb([128, 1], F32, "idxnw")
        P.dma("sp", idxnw[:], self.idx_nw, writes=[resb])
        P.memset("pool", kiA[:], 0.0, writes=[resb])
        P.memset("pool", kiB[:], 0.0, writes=[resb])
        mp = P.mark()
        cosq = P.sb([128, T], F32, "cosq")
        sinq = P.sb([128, T], F32, "sinq")
        cosi = P.sb([128, T], F32, "cosi")
        sini = P.sb([128, T], F32, "sini")
        trb = Buf()
        m2 = P.mark()
        pos_f, pb_ = self.load_pos()
        tmp = (P.sb([128, T], F32, "tv"), P.sb([128, T], I32, "tvi"), P.sb([128, T], F32, "tvf"), Buf())
        inv = self.cs["inv"]
        self.trig(cosq, trb, pos_f, pb_, inv[:, 1:2], 0.25, tmp)
        self.trig(sinq, trb, pos_f, pb_, inv[:, 1:2], 0.0, tmp)
        self.trig(cosi, trb, pos_f, pb_, inv[:, 2:3], 0.25, tmp)
        self.trig(sini, trb, pos_f, pb_, inv[:, 2:3], 0.0, tmp)
        P.release(m2)
        self.pw = [(P.sb([128, KC, 128], BF16, "pw%d" % i), Buf()) for i in range(2)]
        self.pw_i = 0
        xfs = [(P.sb([128, 512], F32, "xf%d" % i), Buf()) for i in range(2)]
        tmp2s = [(P.sb([128, 512], F32, "t1_%d" % i), P.sb([128, 512], F32, "t2_%d" % i), Buf()) for i in range(2)]
        rowb = [(P.sb([128, T], BF16, "rowb%d" % i), Buf()) for i in range(2)]
        rowf = [(P.sb([128, T], F32, "rowf%d" % i), Buf()) for i in range(2)]
        cnt = [0]

        def roped(pm, cos_t, sin_t, out_tile, out_buf, scale_in=None):
            def h(tt, pt, pb):
                xf, xfb = xfs[cnt[0] % 2]
                t2 = tmp2s[cnt[0] % 2]
                cnt[0] += 1
                P.copy("act", xf[:], pt[:, :], reads=[pb], writes=[xfb])
                self.rope_fm(xf, xfb, pm, cos_t, sin_t, trb, tt, out_tile[:, tt * 512:(tt + 1) * 512], out_buf, t2)
            return h

        for hh in range(16):
            rt, rb = rowb[hh % 2]
            self.proj_fm(hT, hbuf, W, IN1_OFF["q"] + hh * 128, 128, roped(self.cs["pm_q"], cosq, sinq, rt, rb))
            P.dma("sp", self.q_d[hh * 128:(hh + 1) * 128, :], rt[:, :], reads=[rb], writes=[qdb])
        self.proj_fm(hT, hbuf, W, IN1_OFF["k"], 128, roped(self.cs["pm_q"], cosq, sinq, kT, resb))
        vT, vTb = rowb[0]

        def hv(tt, pt, pb):
            P.copy("act", vT[:, tt * 512:(tt + 1) * 512], pt[:, :], reads=[pb], writes=[vTb])
        self.proj_fm(hT, hbuf, W, IN1_OFF["v"], 128, hv)
        for kt in range(16):
            pt, pb = P.ps()
            ptb = pt
            P.tr(ptb[:, 0:128], vT[:, kt * 128:(kt + 1) * 128], self.ident_bf[:], reads=[vTb, self.cbuf], writes=[pb])
            P.copy("dve", vtm[:, kt, :], ptb[:, 0:128], reads=[pb], writes=[resb])
        for ch in range(8):
            rt, rb = rowf[ch % 2]
            self.proj_fm(hT, hbuf, W, IN1_OFF["qi"] + ch * 128, 128, roped(self.cs["pm_i"], cosi, sini, rt, rb))
            P.dma("sp", self.qi_d[ch * 128:(ch + 1) * 128, :], rt[:, :], reads=[rb], writes=[qidb])
        kir, kirb = rowf[0]

        def hki(tt, pt, pb):
            xf, xfb = xfs[cnt[0] % 2]
            t1, t2, t12b = tmp2s[cnt[0] % 2]
            cnt[0] += 1
            P.copy("act", xf[:], pt[:, :], reads=[pb], writes=[xfb])
            P.act(t1[:], pt[:, :], AF.Square, reads=[pb], writes=[t12b])
            p2, p2b = P.ps()
            P.mm(p2[:, :], lhsT=self.cs["bones"][:], rhs=t1[:], start=True, stop=True, reads=[t12b, self.cbuf],
                 writes=[p2b])
            self.rstd_from_ss(p2[:, :], p2b, 64, 512, t2[:], t12b)
            P.stt(xf[:], xf[:], idxnw[:, 0:1], t2[:], ALU.mult, ALU.mult, reads=[xfb, t12b, resb], writes=[xfb])
            t3 = tmp2s[cnt[0] % 2]
            self.rope_fm(xf, xfb, self.cs["pm_i"], cosi, sini, trb, tt, kir[:, tt * 512:(tt + 1) * 512], kirb, t3)
        self.proj_fm(hT, hbuf, W, IN1_OFF["ki"], 64, hki, dup=True)
        P.copy("dve", kiA[0:64, :], kir[0:64, :], reads=[kirb], writes=[resb])
        P.copy("dve", kiB[64:128, :], kir[64:128, :], reads=[kirb], writes=[resb])
        wiT, wiTb = rowf[1]

        def hwi(tt, pt, pb):
            P.act(wiT[:, tt * 512:(tt + 1) * 512], pt[:, :], AF.Copy, reads=[pb], writes=[wiTb], scale=1.0 / 32.0)
        self.proj_fm(hT, hbuf, W, IN1_OFF["wi"], 16, hwi)
        for j in range(16):
            pt, pb = P.ps()
            P.tr(pt[:, 0:128], wiT[:, j * 128:(j + 1) * 128], self.cs["ident"][:], reads=[wiTb, self.cbuf],
                 writes=[pb])
            P.copy("dve", witm[:, j, :], pt[:, 0:16], reads=[pb], writes=[resb])
        P.release(mp)

        accs = [(P.sb([128, T], F32, "acc%d" % i), Buf()) for i in range(2)]
        work = P.sb([128, T], F32, "work")
        workb = Buf()
        m8 = P.sb([128, 8], F32, "m8")
        thr = P.sb([128, 1], F32, "thr")
        sels = [(P.sb([128, T], BF16, "sel%d" % i), Buf()) for i in range(2)]
        selTs = [(P.sb([128, 16, 128], BF16, "selT%d" % i), Buf()) for i in range(2)]
        qTs = [(P.sb([128, 16 * 128], BF16, "qT%d" % i), Buf()) for i in range(2)]
        qiTs = [(P.sb([128, 8, 128], F32, "qiT%d" % i), Buf()) for i in range(2)]
        rl = [(P.sb([128, 512], F32, "rl%d" % i), Buf()) for i in range(3)]
        rw = [(P.sb([128, 512], F32, "rw%d" % i), Buf()) for i in range(2)]
        ef = [(P.sb([128, 512], BF16, "ef%d" % i), Buf()) for i in range(4)]
        pTs = [(P.sb([128, 512], BF16, "pT%d" % i), Buf()) for i in range(4)]
        rsum = P.sb([128, 512], F32, "rsum")
        rsb = Buf()
        aos = [(P.sb([128, 512], BF16, "ao%d" % i), Buf()) for i in range(2)]
        iO, (accO, accOb) = P.ps_reserve()
        iS, (accS, accSb) = P.ps_reserve()
        iO2, (accO2, accOb2) = P.ps_reserve()
        iS2, (accS2, accSb2) = P.ps_reserve()
        accsets = [(accO, accOb, accS, accSb), (accO2, accOb2, accS2, accSb2)]
        SC = float(128.0 ** -0.5)
        cn = dict(c1=0, c2=0, c3=0)

        def score_part(j):
            Wj = 128 * (j + 1)
            nseg = (Wj + 511) // 512
            qT, qTb = qTs[j % 2]
            qiT, qiTb = qiTs[j % 2]
            acc, accb = accs[j % 2]
            sel, selb = sels[j % 2]
            P.dma("sp", qT[:, :].rearrange("p (h t) -> p h t", h=16),
                  self.q_d[:, j * 128:(j + 1) * 128].rearrange("(h p) t -> p h t", p=128), reads=[qdb], writes=[qTb])
            P.dma("sp", qiT[:, :, :],
                  self.qi_d[:, j * 128:(j + 1) * 128].rearrange("(h p) t -> p h t", p=128), reads=[qidb],
                  writes=[qiTb])
            if j > 0:
                P.memset("pool", acc[:, 0:Wj - 128], 0.0, writes=[accb])
            P.copy("pool", acc[:, Wj - 128:Wj], self.cs["cbias"][:], reads=[self.cbuf], writes=[accb])
            for hi in range(16):
                rk = kiA if hi % 2 == 0 else kiB
                for sg in range(nseg):
                    s0 = sg * 512
                    s1 = min(Wj, s0 + 512)
                    pt, pb = P.ps()
                    P.mm(pt[:, 0:s1 - s0], lhsT=qiT[:, hi // 2, :], rhs=rk[:, s0:s1], start=True, stop=True,
                         reads=[qiTb, resb], writes=[pb])
                    r, rb_ = rl[cn["c1"] % 3]
                    r2, rb2 = rw[cn["c1"] % 2]
                    cn["c1"] += 1
                    P.act(r[:, 0:s1 - s0], pt[:, 0:s1 - s0], AF.Relu, reads=[pb], writes=[rb_])
                    P.stt(acc[:, s0:s1], r[:, 0:s1 - s0], witm[:, j, hi:hi + 1], acc[:, s0:s1], ALU.mult, ALU.add,
                          reads=[rb_, resb, accb], writes=[accb])
            if j >= 2:
                P.copy("dve", work[:, 0:Wj], acc[:, 0:Wj], reads=[accb], writes=[workb])
                for r_ in range(32):
                    P.op("dve", lambda e: e.max(out=m8[:], in_=work[:, 0:Wj]), reads=[workb], writes=[workb])
                    if r_ < 31:
                        P.op("dve", lambda e: e.match_replace(out=work[:, 0:Wj], in_to_replace=m8[:],
                                                               in_values=work[:, 0:Wj], imm_value=NEG),
                             reads=[workb], writes=[workb])
                P.ts("dve", thr[:], m8[:, 7:8], -1.0e29, ALU.max, reads=[workb], writes=[workb])
            else:
                P.memset("dve", thr[:], -1.0e29, writes=[workb])
            P.ts("dve", sel[:, 0:Wj], acc[:, 0:Wj], thr[:, 0:1], ALU.is_ge, reads=[accb, workb], writes=[selb])

        def attn_part(j):
            qT, qTb = qTs[j % 2]
            selT, selTb = selTs[j % 2]
            sel, selb = sels[j % 2]
            for kt in range(j + 1):
                pt, pb = P.ps()
                P.tr(pt[:, 0:128], sel[:, kt * 128:(kt + 1) * 128], self.ident_bf[:], reads=[selb, self.cbuf],
                     writes=[pb])
                P.copy("act", selT[:, kt, :], pt[:, 0:128], reads=[pb], writes=[selTb])
            steps = [(g, kt) for g in range(4) for kt in range(j + 1)]
            LA = 2
            live = {}

            def front(i):
                g, kt = steps[i]
                pt, pb = P.ps()
                P.mm(pt[:, :], lhsT=kT[:, kt * 128:(kt + 1) * 128], rhs=qT[:, g * 512:(g + 1) * 512],
                     start=True, stop=True, reads=[resb, qTb], writes=[pb])
                e_, eb = ef[cn["c2"] % 4]
                p_, pb2 = pTs[cn["c2"] % 4]
                cn["c2"] += 1
                P.act(e_[:], pt[:, :], AF.Exp, reads=[pb], writes=[eb], scale=SC)
                P.tt("pool", p_[:, :].rearrange("p (h q) -> p h q", h=4),
                     e_[:, :].rearrange("p (h q) -> p h q", h=4),
                     selT[:, kt, :].unsqueeze(1).broadcast_to([128, 4, 128]), ALU.mult,
                     reads=[eb, selTb], writes=[pb2])
                live[i] = (p_, pb2)

            def back(i):
                g, kt = steps[i]
                p_, pb2 = live.pop(i)
                accO, accOb, accS, accSb = accsets[g % 2]
                P.mm(accO[:, :], lhsT=vtm[:, kt, :], rhs=p_[:, :], start=(kt == 0), stop=(kt == j),
                     reads=[resb, pb2], writes=[accOb], inc=False)
                P.mm(accS[:, :], lhsT=self.ones_bf[:], rhs=p_[:, :], start=(kt == 0), stop=(kt == j),
                     reads=[self.cbuf, pb2], writes=[accSb], inc=True)
                if kt == j:
                    P.op("dve", lambda e: e.reciprocal(out=rsum[:], in_=accS[:, :]), reads=[accSb], writes=[rsb])
                    ao, aob_ = aos[cn["c3"] % 2]
                    cn["c3"] += 1
                    P.tt("dve", ao[:], accO[:, :], rsum[:], ALU.mult, reads=[accOb, rsb], writes=[aob_])
                    P.dma("sp", self.ao_d[g * 512:(g + 1) * 512, j * 128:(j + 1) * 128].rearrange(
                        "(h p) t -> p h t", p=128), ao[:, :].rearrange("p (h q) -> p h q", h=4), reads=[aob_],
                        writes=[aob])

            n = len(steps)
            for i in range(n + LA):
                if i < n:
                    front(i)
                if i - LA >= 0:
                    back(i - LA)

        score_part(0)
        for j in range(16):
            if j + 1 < 16:
                score_part(j + 1)
            attn_part(j)
        for i_ in (iO, iS, iO2, iS2):
            P.ps_unreserve(i_)
        P.release(m)
        self.stage_down(self.ao_d, aob, self.dsa_out, 16)

_CACHE = {}


def col_layout(v, nchunk):
    return np.ascontiguousarray(np.asarray(v).reshape(nchunk, 128).T)


def make_inmaps(inputs, ncores=4):
    g = lambda k: np.asarray(inputs[k])
    hc = host_consts()
    shared = {}
    shared["mod_w"] = np.ascontiguousarray(g("mod_w"), dtype=np.float32)
    shared["mod_b"] = np.stack([col_layout(g("mod_b")[i], 144) for i in range(2)]).astype(np.float32)
    nw = g("norm_w")
    shared["norm_w"] = np.stack(
        [np.concatenate([col_layout(nw[i, s], KC) for s in range(6)], axis=1) for i in range(2)]).astype(np.float32)
    shared["ffn_w_gate"] = np.ascontiguousarray(g("ffn_w_gate"), dtype=np.float32)
    shared["ffn_w_up"] = np.ascontiguousarray(g("ffn_w_up"), dtype=np.float32)
    shared["ffn_w_down"] = np.ascontiguousarray(g("ffn_w_down"), dtype=np.float32)
    shared["hy_w_in"] = np.ascontiguousarray(g("hy_w_in")[0], dtype=np.float32)
    cw = g("hy_conv_w")[0]
    shared["conv_w"] = np.ascontiguousarray(
        cw.reshape(4, 32, 128).transpose(2, 1, 0).reshape(128, 128)).astype(np.float32)
    shared["conv_b"] = col_layout(g("hy_conv_b")[0], 32).astype(np.float32)
    sm = np.zeros((128, 4), np.float32)
    sm[:32, 0] = g("ssd_A_log")[0]
    sm[:32, 1] = g("ssd_dt_bias")[0]
    shared["ssd_small"] = sm
    shared["ssd_Dc"] = col_layout(np.repeat(g("ssd_D")[0], 64), 16).astype(np.float32)
    shared["ssd_nw"] = col_layout(g("ssd_norm_w")[0], 16).astype(np.float32)
    shared["ret_nw"] = col_layout(g("ret_norm_w")[0], 16).astype(np.float32)
    shared["hy_w_out"] = np.ascontiguousarray(g("hy_w_out")[0], dtype=np.float32)
    shared["dsa_w_in"] = np.ascontiguousarray(g("dsa_w_in")[0], dtype=np.float32)
    shared["idx_nw"] = np.concatenate([g("idx_k_norm_w")[0]] * 2).reshape(128, 1).astype(np.float32)
    shared["dsa_w_out"] = np.ascontiguousarray(g("dsa_w_out")[0], dtype=np.float32)
    for k in CONST_SHAPES:
        shared["c_" + k] = hc[k]
    maps = []
    x = g("x")
    c = g("c")
    pos = g("positions")
    for b in range(ncores):
        mp = dict(shared)
        mp["xT"] = np.ascontiguousarray(x[b].T, dtype=np.float32)
        mp["c_col"] = col_layout(c[b], KC).astype(np.float32)
        mp["pos"] = np.ascontiguousarray(pos[b], dtype=np.int32)
        maps.append(mp)
    return maps


def run(inputs, n_sub=6, trace=False, ncores=4):
    key = str(n_sub)
    if key not in _CACHE:
        _CACHE[key] = Builder(n_sub)
    bld = _CACHE[key]
    maps = make_inmaps(inputs, ncores)
    used = set(bld.inp.keys())
    maps = [{k: v for k, v in mp.items() if k in used} for mp in maps]
    res = run_bass_kernel_spmd(bld.nc, maps, core_ids=list(range(ncores)), trace=trace)
    out = np.stack([np.ascontiguousarray(res.results[b]["yT"].T) for b in range(ncores)])
    return out.astype(np.float32), res


def kernel(**inputs):
    out, _ = run(inputs, 6)
    return out
```

```python
import numpy as np
import concourse.bass as bass
import concourse.mybir as mybir
from concourse.bass_utils import run_bass_kernel_spmd

F32 = mybir.dt.float32
BF16 = mybir.dt.bfloat16
I32 = mybir.dt.int32
AF = mybir.ActivationFunctionType
ALU = mybir.AluOpType

D = 2048
T = 2048
DFF = 5632
KC = 16
NEG = -1.0e30
EPS = 1e-6
IN0_OFF = dict(z=0, xbc=2048, dt=6144, rq=6176, rk=8224, rv=10272, rg=12320)
IN1_OFF = dict(q=0, k=2048, v=2176, qi=2304, ki=3328, wi=3392)


class Buf:
    __slots__ = ("w", "r", "const", "excl")

    def __init__(self, const=False, excl=False):
        self.w = None
        self.r = []
        self.const = const
        self.excl = excl


class Prog:
    def __init__(self, nc):
        self.nc = nc
        self.eng = {"pe": nc.tensor, "dve": nc.vector, "act": nc.scalar, "pool": nc.gpsimd, "sp": nc.sync}
        self.sem = {k: nc.alloc_semaphore("s_" + k) for k in self.eng}
        self.cnt = {k: 0 for k in self.eng}
        self.pend = {k: 0 for k in self.eng}
        self.seen = {k: {} for k in self.eng}
        self.nslots = 10
        self.slots = {}
        self.rr = {}
        for q in ("sp", "pool", "act"):
            self.rr[q] = 0
            for i in range(self.nslots):
                key = "d_%s%d" % (q, i)
                self.slots[key] = [nc.alloc_semaphore(key), 0]
        self.sb_off = 16384
        self.sb_max = 16384 + 212000
        self.uid = 0
        self.psum = []
        for i in range(8):
            self.psum.append((nc.alloc_psum_tensor("psb%d" % i, [128, 512], F32), Buf(excl=True)))
        self.prr = 0
        self.reserved = set()
        self.log = {k: [] for k in self.eng}

    def sb(self, shape, dtype, name=None):
        self.uid += 1
        esz = 2 if dtype == BF16 else 4
        n = 1
        for s in shape[1:]:
            n *= s
        nbytes = (n * esz + 63) // 64 * 64
        off = self.sb_off
        assert off + nbytes <= self.sb_max, ("SBUF overflow", name, off, nbytes)
        self.sb_off += nbytes
        t = self.nc.alloc_sbuf_tensor_at("%s_%d" % (name or "t", self.uid), list(shape), dtype, offset=off)
        return t

    def mark(self):
        return self.sb_off

    def release(self, m):
        self.barrier()
        self.sb_off = m

    def ps(self):
        while True:
            i = self.prr % 8
            self.prr += 1
            if i not in self.reserved:
                return self.psum[i]

    def ps_reserve(self):
        for i in range(7, -1, -1):
            if i not in self.reserved:
                self.reserved.add(i)
                return i, self.psum[i]
        raise RuntimeError("no psum")

    def ps_unreserve(self, i):
        self.reserved.discard(i)

    def _semof(self, key):
        if key in self.sem:
            return self.sem[key]
        return self.slots[key][0]

    def _wait(self, e, deps):
        for o, v in deps.items():
            if self.seen[e].get(o, 0) < v:
                self.eng[e].wait_ge(self._semof(o), v)
                self.seen[e][o] = v
                self.log[e].append(("w", o, v))

    def _collect(self, e, reads, writes):
        deps = {}

        def add(x):
            if x is None:
                return
            o, v = x
            if o == e and e == "pe":
                return
            if deps.get(o, 0) < v:
                deps[o] = v

        for b in reads:
            add(b.w)
            if b.excl:
                for r in b.r:
                    if r[0] != e:
                        add(r)
        for b in writes:
            add(b.w)
            for r in b.r:
                if r[0] != e:
                    add(r)
        return deps

    def op(self, e, fn, reads=(), writes=(), inc=True):
        deps = self._collect(e, reads, writes)
        self._wait(e, deps)
        ins = fn(self.eng[e])
        val = self.cnt[e] + 1
        if inc:
            ins.then_inc(self.sem[e], 1)
            self.cnt[e] = val
            self.pend[e] = 0
            self.log[e].append(("i", e, 1))
        else:
            self.pend[e] += 1
        for b in reads:
            if not b.const:
                b.r.append((e, val))
        for b in writes:
            b.w = (e, val)
            b.r = []
        return ins

    def dma(self, q, out, in_, reads=(), writes=()):
        key = "d_%s%d" % (q, self.rr[q] % self.nslots)
        self.rr[q] += 1
        slot = self.slots[key]
        deps = self._collect(key, reads, writes)
        if slot[1] > 0:
            deps[key] = max(deps.get(key, 0), slot[1])
        self._wait(q, deps)
        slot[1] += 16
        self.eng[q].dma_start(out=out, in_=in_).then_inc(slot[0], 16)
        self.log[q].append(("i", key, 16))
        for b in reads:
            if not b.const:
                b.r.append((key, slot[1]))
        for b in writes:
            b.w = (key, slot[1])
            b.r = []

    def barrier(self):
        for e in self.eng:
            assert self.pend[e] == 0, ("pending non-inc ops on", e)
        tgt = dict(self.cnt)
        for k, s in self.slots.items():
            if s[1] > 0:
                tgt[k] = s[1]
        for e in self.eng:
            deps = {o: v for o, v in tgt.items() if o != e and v > 0}
            self._wait(e, deps)

    def mm(self, out, lhsT, rhs, start, stop, reads=(), writes=(), inc=None):
        if inc is None:
            inc = stop
        return self.op("pe", lambda e: e.matmul(out, lhsT=lhsT, rhs=rhs, start=start, stop=stop),
                       reads, writes, inc=inc)

    def tr(self, out, in_, ident, reads=(), writes=(), inc=True):
        return self.op("pe", lambda e: e.matmul(out, lhsT=in_, rhs=ident, start=True, stop=True), reads, writes,
                       inc=inc)

    def act(self, out, in_, func, reads=(), writes=(), bias=None, scale=None, e="act"):
        kw = {}
        if bias is not None:
            kw["bias"] = bias
        if scale is not None:
            kw["scale"] = scale
        return self.op("act", lambda en: en.activation(out=out, in_=in_, func=func, **kw), reads, writes)

    def tt(self, e, out, in0, in1, op, reads=(), writes=()):
        return self.op(e, lambda en: en.tensor_tensor(out=out, in0=in0, in1=in1, op=op), reads, writes)

    def ts(self, e, out, in0, s1, op0, s2=None, op1=None, reads=(), writes=()):
        if op1 is None:
            return self.op(e, lambda en: en.tensor_scalar(out=out, in0=in0, scalar1=s1, scalar2=None, op0=op0),
                           reads, writes)
        return self.op(e, lambda en: en.tensor_scalar(out=out, in0=in0, scalar1=s1, scalar2=s2, op0=op0, op1=op1),
                       reads, writes)

    def stt(self, out, in0, scalar, in1, op0, op1, reads=(), writes=()):
        return self.op("dve", lambda en: en.scalar_tensor_tensor(out=out, in0=in0, scalar=scalar, in1=in1,
                                                                  op0=op0, op1=op1), reads, writes)

    def copy(self, e, out, in_, reads=(), writes=()):
        if e == "act":
            return self.op("act", lambda en: en.activation(out=out, in_=in_, func=AF.Copy), reads, writes)
        return self.op(e, lambda en: en.tensor_copy(out=out, in_=in_), reads, writes)

    def memset(self, e, ap, val, writes=()):
        return self.op(e, lambda en: en.memset(ap, val), (), writes)


def host_consts():
    c = {}
    c["ident"] = np.eye(128, dtype=np.float32)
    c["ones"] = np.ones((128, 128), np.float32)
    bo = np.zeros((128, 128), np.float32)
    bo[:64, :64] = 1
    bo[64:, 64:] = 1
    c["bones"] = bo
    s = np.arange(128)[:, None]
    l = np.arange(128)[None, :]
    c["triT"] = (l >= s).astype(np.float32)
    sel = np.zeros((32, 32, 128), np.float32)
    for h in range(32):
        sel[h, h, :] = 1
    c["selh"] = np.concatenate([sel.reshape(32, 32 * 128), np.zeros((96, 4096), np.float32)])
    ex = np.zeros((32, 32, 64), np.float32)
    for h in range(32):
        ex[h, h, :] = 1
    c["expand"] = np.concatenate([ex.reshape(32, 2048), np.zeros((96, 2048), np.float32)])
    lg = np.log(1.0 - 2.0 ** (-5.0 - np.arange(8, dtype=np.float64)))
    sc = 256.0 ** -0.5
    dm = np.zeros((128, 8, 128), np.float64)
    qd = np.zeros((128, 8, 128), np.float64)
    kd = np.zeros((128, 8), np.float64)
    for h in range(8):
        dm[:, h, :] = np.where(l >= s, np.exp((l - s) * lg[h]), 0.0) * sc
        qd[:, h, :] = np.exp((l + 1) * lg[h])
        kd[:, h] = np.exp((127 - np.arange(128)) * lg[h]) * sc
    c["ret_dm"] = dm.reshape(128, 1024).astype(np.float32)
    c["ret_qd"] = qd.reshape(128, 1024).astype(np.float32)
    c["ret_kd"] = kd.astype(np.float32)
    c["_ret_cd"] = [float(np.exp(128 * lg[h])) for h in range(8)]
    inv = np.zeros((128, 4), np.float32)
    inv[:, 0] = (10000.0 ** (-np.arange(128, dtype=np.float32) / 128)).astype(np.float32)
    th = 500000.0
    for p in range(128):
        if p < 32:
            inv[p, 1] = np.float32(th) ** np.float32(-(p % 16) / 16.0)
        if p % 64 < 16:
            inv[p, 2] = np.float32(th) ** np.float32(-((p % 64) % 8) / 8.0)
    c["inv"] = inv
    pq = np.zeros((128, 128), np.float32)
    for m in range(16):
        pq[m + 16, m] = -1.0
        pq[m, m + 16] = 1.0
    c["pm_q"] = pq
    pi = np.zeros((128, 128), np.float32)
    for b in (0, 64):
        for m in range(8):
            pi[b + m + 8, b + m] = -1.0
            pi[b + m, b + m + 8] = 1.0
    c["pm_i"] = pi
    c["cbias"] = np.where(l <= s, 0.0, NEG).astype(np.float32)
    return c


CONST_SHAPES = dict(ident=(128, 128), ones=(128, 128), bones=(128, 128), triT=(128, 128), selh=(128, 4096),
                    expand=(128, 2048), ret_dm=(128, 1024), ret_qd=(128, 1024), ret_kd=(128, 8), inv=(128, 4),
                    pm_q=(128, 128), pm_i=(128, 128), cbias=(128, 128))


class Builder:
    def __init__(self, n_sub=6):
        self.n_sub = n_sub
        self.overlap_mods = (n_sub == 6)
        nc = bass.Bass("TRN2", target_bir_lowering=False)
        self.nc = nc
        self.P = Prog(nc)
        self.inp = {}
        self.build()

    def din(self, name, shape, dtype=F32):
        t = self.nc.dram_tensor(name, list(shape), dtype, kind="ExternalInput").ap()
        self.inp[name] = t
        return t

    def dscr(self, name, shape, dtype):
        return self.nc.dram_tensor(name, list(shape), dtype, kind="Internal").ap()

    def build(self):
        nc, P = self.nc, self.P
        self.xin = self.din("xT", [D, T])
        self.cin = self.din("c_col", [128, KC])
        self.posin = self.din("pos", [T], I32)
        self.mod_w = self.din("mod_w", [2, D, 9 * D])
        self.mod_b = self.din("mod_b", [2, 128, 144])
        self.norm_w = self.din("norm_w", [2, 128, 6 * KC])
        self.wg = self.din("ffn_w_gate", [2, 2, D, DFF])
        self.wu = self.din("ffn_w_up", [2, 2, D, DFF])
        self.wd = self.din("ffn_w_down", [2, 2, DFF, D])
        self.hy_in = self.din("hy_w_in", [D, 14368])
        self.conv_w = self.din("conv_w", [128, 32 * 4])
        self.conv_b = self.din("conv_b", [128, 32])
        self.ssd_small = self.din("ssd_small", [128, 4])
        self.ssd_Dc = self.din("ssd_Dc", [128, 16])
        self.ssd_nw = self.din("ssd_nw", [128, 16])
        self.ret_nw = self.din("ret_nw", [128, 16])
        self.hy_out = self.din("hy_w_out", [4096, D])
        self.dsa_in = self.din("dsa_w_in", [D, 3408])
        self.idx_nw = self.din("idx_nw", [128, 1])
        self.dsa_out = self.din("dsa_w_out", [D, D])
        self.cst_d = {k: self.din("c_" + k, shp) for k, shp in CONST_SHAPES.items()}
        self.yout = nc.dram_tensor("yT", [D, T], F32, kind="ExternalOutput").ap()
        self.xr = self.dscr("xr", [D, T], F32)
        self.xr_buf = Buf()
        self.xin_buf = Buf()
        self.yout_buf = Buf()
        self.hid_d = self.dscr("hid_d", [DFF, T], BF16)
        self.hid_buf = Buf()

        self.cs = {}
        self.cbuf = Buf(const=True)
        for k in ("ident", "ones", "bones", "triT", "inv", "pm_q", "pm_i", "cbias", "ret_kd"):
            shp = CONST_SHAPES[k]
            t = P.sb(list(shp), F32, "c_" + k)
            P.dma("sp", t[:], self.cst_d[k], writes=[self.cbuf])
            self.cs[k] = t
        self.eps_t = P.sb([128, 1], F32, "eps")
        P.memset("dve", self.eps_t[:], EPS, writes=[self.cbuf])
        self.ident_bf = P.sb([128, 128], BF16, "identbf")
        self.ones_bf = P.sb([128, 128], BF16, "onesbf")
        P.copy("dve", self.ident_bf[:], self.cs["ident"][:], reads=[self.cbuf], writes=[self.cbuf])
        P.copy("dve", self.ones_bf[:], self.cs["ones"][:], reads=[self.cbuf], writes=[self.cbuf])
        self.normw_sb = P.sb([128, 2, 6 * KC], F32, "normw")
        for i in range(2):
            P.dma("sp", self.normw_sb[:, i, :], self.norm_w[i], writes=[self.cbuf])
        self.mods_sb = P.sb([128, 2, 144], F32, "mods")
        self.mods_buf = Buf()
        self.cols = P.sb([128, 3, KC], F32, "cols")
        self.cols_buf = Buf()
        P.barrier()

        self.stage_mods()

        subs = [
            (0, 0, "ffn", 0), (0, 1, "hy", None), (0, 2, "ffn", 1),
            (1, 0, "ffn", 0), (1, 1, "dsa", None), (1, 2, "ffn", 1),
        ][: self.n_sub] if isinstance(self.n_sub, int) else list(self.n_sub)
        for si, (layer, j, kind, f) in enumerate(subs):
            src, sbuf_ = (self.xin, self.xin_buf) if si == 0 else (self.xr, self.xr_buf)
            last = si == len(subs) - 1
            dst, dbuf = (self.yout, self.yout_buf) if last else (self.xr, self.xr_buf)
            self.cur = dict(layer=layer, j=j, src=src, sbuf=sbuf_, dst=dst, dbuf=dbuf,
                            rw=0.5 if kind == "ffn" else 1.0)
            self.make_cols(layer, j)
            m = P.mark()
            hT, hbuf = self.stage_pre()
            if kind == "ffn":
                self.stage_ffn_up(hT, hbuf, layer, f)
                P.release(m)
                self.stage_down(self.hid_d, self.hid_buf, self.wd[layer, f], DFF // 128)
            elif kind == "hy":
                self.stage_hy(hT, hbuf, m)
            else:
                self.stage_dsa(hT, hbuf, m)
            P.release(m)
        P.barrier()

    def stage_mods(self):
        P = self.P
        cb = Buf()
        self.mods_cb = cb
        c_sb = P.sb([128, KC], F32, "c_sb")
        self.cond = P.sb([128, KC], F32, "cond")
        self.mb = P.sb([128, 2, 144], F32, "mb")
        P.dma("sp", c_sb[:], self.cin, writes=[cb])
        for i in range(2):
            P.dma("sp", self.mb[:, i, :], self.mod_b[i], writes=[cb])
        P.act(self.cond[:], c_sb[:], AF.Silu, reads=[cb], writes=[cb])
        m = P.mark()
        layers = [0] if self.overlap_mods else [0, 1]
        for layer in layers:
            for _ in self.mods_gen(layer, None):
                pass
        P.release(m)

    def mods_gen(self, layer, bank):
        P = self.P
        cb = self.mods_cb
        NB = 512 if bank is None else 128
        wt = [(P.sb([128, KC, NB], F32, "modw%d_%d" % (layer, i)), Buf()) for i in range(2)]
        for blk in range(9 * D // NB):
            w, wb = wt[blk % 2]
            P.dma("sp", w[:, :, :],
                  self.mod_w[layer, :, blk * NB:(blk + 1) * NB].rearrange("(k p) n -> p k n", p=128), writes=[wb])
            yield
            pt, pb = P.ps() if bank is None else bank
            for cc in range(NB // 128):
                for k in range(KC):
                    P.mm(pt[:, cc:cc + 1], lhsT=w[:, k, cc * 128:(cc + 1) * 128], rhs=self.cond[:, k:k + 1],
                         start=(k == 0), stop=(k == KC - 1), reads=[wb, cb], writes=[pb],
                         inc=(k == KC - 1))
                if bank is not None:
                    yield
            c0 = blk * (NB // 128)
            P.tt("dve", self.mods_sb[:, layer, c0:c0 + NB // 128], pt[:, 0:NB // 128],
                 self.mb[:, layer, c0:c0 + NB // 128], ALU.add, reads=[pb, cb], writes=[self.mods_buf])
            yield

    def make_cols(self, layer, j):
        P = self.P
        ms = self.mods_sb
        nw = self.normw_sb
        shift = ms[:, layer, (3 * j) * KC:(3 * j + 1) * KC]
        scale = ms[:, layer, (3 * j + 1) * KC:(3 * j + 2) * KC]
        gate = ms[:, layer, (3 * j + 2) * KC:(3 * j + 3) * KC]
        pre_w = nw[:, layer, (2 * j) * KC:(2 * j + 1) * KC]
        post_w = nw[:, layer, (2 * j + 1) * KC:(2 * j + 2) * KC]
        rd = [self.mods_buf, self.cbuf]
        P.stt(self.cols[:, 0, :], scale, 1.0, pre_w, ALU.add, ALU.mult, reads=rd, writes=[self.cols_buf])
        P.copy("dve", self.cols[:, 1, :], shift, reads=rd, writes=[self.cols_buf])
        P.stt(self.cols[:, 2, :], gate, self.cur["rw"], post_w, ALU.mult, ALU.mult, reads=rd, writes=[self.cols_buf])

    def rstd_from_ss(self, ss_ps, ss_buf, n, width, out_t, out_buf):
        P = self.P
        P.act(out_t, ss_ps, AF.Sqrt, reads=[ss_buf], writes=[out_buf], bias=self.eps_t[:, 0:1], scale=1.0 / n)
        P.op("dve", lambda en: en.reciprocal(out=out_t, in_=out_t), reads=[out_buf], writes=[out_buf])

    def stage_pre(self):
        P = self.P
        cur = self.cur
        hT = P.sb([128, KC, T], BF16, "hT")
        hbuf = [Buf() for _ in range(T // 512)]
        m = P.mark()
        xt = [(P.sb([128, KC, 512], F32, "xt%d" % i), Buf()) for i in range(2)]
        sq = [(P.sb([128, 4, 512], F32, "sq%d" % i), Buf()) for i in range(2)]
        rs = [(P.sb([128, 512], F32, "rs%d" % i), Buf()) for i in range(2)]
        tmp = [(P.sb([128, 4, 512], F32, "tmp%d" % i), Buf()) for i in range(2)]
        srcv = cur["src"].rearrange("(k p) t -> p k t", p=128)
        it = 0
        for tt in range(T // 512):
            x, xb = xt[tt % 2]
            P.dma("sp", x[:, :, :], srcv[:, :, tt * 512:(tt + 1) * 512], reads=[cur["sbuf"]], writes=[xb])
            pt, pb = P.ps()
            for k4 in range(KC // 4):
                s_, sbf = sq[k4 % 2]
                P.act(s_[:, :, :], x[:, k4 * 4:(k4 + 1) * 4, :], AF.Square, reads=[xb], writes=[sbf])
                for kk in range(4):
                    k = k4 * 4 + kk
                    P.mm(pt[:, :], lhsT=self.cs["ones"][:], rhs=s_[:, kk, :], start=(k == 0), stop=(k == KC - 1),
                         reads=[sbf, self.cbuf], writes=[pb], inc=(kk == 3))
            r, rb = rs[tt % 2]
            self.rstd_from_ss(pt[:, :], pb, D, 512, r[:], rb)
            for k4 in range(KC // 4):
                tm, tb = tmp[it % 2]
                it += 1
                P.tt("dve" if k4 % 2 == 0 else "pool", tm[:, :, :], x[:, k4 * 4:(k4 + 1) * 4, :],
                     r[:, :].unsqueeze(1).broadcast_to([128, 4, 512]), ALU.mult, reads=[xb, rb], writes=[tb])
                for kk in range(4):
                    k = k4 * 4 + kk
                    P.act(hT[:, k, tt * 512:(tt + 1) * 512], tm[:, kk, :], AF.Identity, reads=[tb, self.cols_buf],
                          writes=[hbuf[tt]], bias=self.cols[:, 1, k:k + 1], scale=self.cols[:, 0, k:k + 1])
        P.release(m)
        return hT, hbuf

    def wload(self, w_t, w_buf, src_ap):
        self.P.dma("pool", w_t, src_ap, writes=[w_buf])

    def stage_ffn_up(self, hT, hbuf, layer, f):
        P = self.P
        CB = 256
        wgt = [(P.sb([128, KC, CB], BF16, "wg%d" % i), Buf()) for i in range(2)]
        wut = [(P.sb([128, KC, CB], BF16, "wu%d" % i), Buf()) for i in range(2)]
        ho = [(P.sb([128, T], BF16, "ho%d" % i), Buf()) for i in range(2)]
        sg = [(P.sb([128, 512], F32, "sg%d" % i), Buf()) for i in range(2)]
        Wg = self.wg[layer, f]
        Wu = self.wu[layer, f]
        nblk = DFF // CB
        it = 0
        for blk in range(nblk):
            g, gb = wgt[blk % 2]
            u, ub = wut[blk % 2]
            self.wload(g[:, :, :], gb, Wg[:, blk * CB:(blk + 1) * CB].rearrange("(k p) n -> p k n", p=128))
            self.wload(u[:, :, :], ub, Wu[:, blk * CB:(blk + 1) * CB].rearrange("(k p) n -> p k n", p=128))
            for cc in range(CB // 128):
                fc = blk * (CB // 128) + cc
                h_o, hob = ho[fc % 2]
                for tt in range(T // 512):
                    pg, pgb = P.ps()
                    pu, pub = P.ps()
                    for k in range(KC):
                        P.mm(pg[:, :], lhsT=g[:, k, cc * 128:(cc + 1) * 128], rhs=hT[:, k, tt * 512:(tt + 1) * 512],
                             start=(k == 0), stop=(k == KC - 1), reads=[gb, hbuf[tt]], writes=[pgb])
                    for k in range(KC):
                        P.mm(pu[:, :], lhsT=u[:, k, cc * 128:(cc + 1) * 128], rhs=hT[:, k, tt * 512:(tt + 1) * 512],
                             start=(k == 0), stop=(k == KC - 1), reads=[ub, hbuf[tt]], writes=[pub])
                    s, sb_ = sg[it % 2]
                    it += 1
                    P.act(s[:], pg[:, :], AF.Silu, reads=[pgb], writes=[sb_])
                    P.tt("dve", h_o[:, tt * 512:(tt + 1) * 512], s[:], pu[:, :], ALU.mult, reads=[sb_, pub],
                         writes=[hob])
                P.dma("sp", self.hid_d[fc * 128:(fc + 1) * 128, :], h_o[:, :], reads=[hob], writes=[self.hid_buf])

    def stage_down(self, act_d, act_buf, W, nkc):
        P = self.P
        cur = self.cur
        NT = 1024
        a_t = P.sb([128, nkc, NT], BF16, "a_t")
        a_bufs = [Buf() for _ in range(nkc)]
        wt = [(P.sb([128, nkc, 128], BF16, "wd%d" % i), Buf()) for i in range(2)]
        y = P.sb([128, KC, NT], F32, "ydown")
        ybufs = [Buf() for _ in range(KC)]
        sq = [(P.sb([128, 512], F32, "sqd%d" % i), Buf()) for i in range(2)]
        rs = P.sb([128, NT], F32, "rsd")
        rsb = Buf()
        xc = [(P.sb([128, NT], F32, "xc%d" % i), Buf()) for i in range(2)]
        actv = act_d.rearrange("(k p) t -> p k t", p=128)
        srcv = cur["src"].rearrange("(k p) t -> p k t", p=128)
        dstv = cur["dst"].rearrange("(k p) t -> p k t", p=128)
        res = [P.ps_reserve() for _ in range(NT // 512)]
        pss = [r_[1] for r_ in res]
        it = 0
        ntiles = T // NT

        def load_a(nt):
            t0 = nt * NT
            for k in range(nkc):
                P.dma("sp", a_t[:, k, :], actv[:, k, t0:t0 + NT], reads=[act_buf], writes=[a_bufs[k]])

        load_a(0)
        for nt in range(ntiles):
            t0 = nt * NT
            for dc in range(KC):
                w, wb = wt[it % 2]
                it += 1
                self.wload(w[:, :, :], wb, W[:, dc * 128:(dc + 1) * 128].rearrange("(k p) n -> p k n", p=128))
                for hh in range(NT // 512):
                    pt, pb = P.ps()
                    for k in range(nkc):
                        P.mm(pt[:, :], lhsT=w[:, k, :], rhs=a_t[:, k, hh * 512:(hh + 1) * 512],
                             start=(k == 0), stop=(k == nkc - 1), reads=[wb, a_bufs[k]], writes=[pb])
                    P.copy("act" if hh == 0 else "dve", y[:, dc, hh * 512:(hh + 1) * 512], pt[:, :], reads=[pb],
                           writes=[ybufs[dc]])
                if dc > 0:
                    for hh in range(NT // 512):
                        s_, sbf = sq[hh % 2]
                        P.act(s_[:], y[:, dc - 1, hh * 512:(hh + 1) * 512], AF.Square, reads=[ybufs[dc - 1]],
                              writes=[sbf])
                        P.mm(pss[hh][0][:, :], lhsT=self.cs["ones"][:], rhs=s_[:], start=(dc == 1), stop=False,
                             reads=[sbf, self.cbuf], writes=[pss[hh][1]], inc=True)
            for hh in range(NT // 512):
                s_, sbf = sq[hh % 2]
                P.act(s_[:], y[:, KC - 1, hh * 512:(hh + 1) * 512], AF.Square, reads=[ybufs[KC - 1]], writes=[sbf])
                P.mm(pss[hh][0][:, :], lhsT=self.cs["ones"][:], rhs=s_[:], start=False, stop=True,
                     reads=[sbf, self.cbuf], writes=[pss[hh][1]], inc=True)
            if nt + 1 < ntiles:
                load_a(nt + 1)
            for hh in range(NT // 512):
                self.rstd_from_ss(pss[hh][0][:, :], pss[hh][1], D, 512, rs[:, hh * 512:(hh + 1) * 512], rsb)
            for k in range(KC):
                x, xb = xc[k % 2]
                P.dma("pool", x[:], srcv[:, k, t0:t0 + NT], reads=[cur["sbuf"]], writes=[xb])
                P.tt("pool" if k % 2 else "dve", y[:, k, :], y[:, k, :], rs[:], ALU.mult, reads=[rsb],
                     writes=[ybufs[k]])
                P.stt(x[:], y[:, k, :], self.cols[:, 2, k:k + 1], x[:], ALU.mult, ALU.add,
                      reads=[ybufs[k], xb, self.cols_buf], writes=[xb])
                P.dma("act", dstv[:, k, t0:t0 + NT], x[:], reads=[xb], writes=[cur["dbuf"]])
        for r_ in res:
            P.ps_unreserve(r_[0])

    def stage_hy(self, hT, hbuf, m):
        P = self.P
        W = self.hy_in
        if not hasattr(self, "zs_d"):
            self.zs_d = self.dscr("zs_d", [D, T], F32)
            self.xs_d = self.dscr("xs_d", [D, T], BF16)
            self.B_d = self.dscr("B_d", [1024, T], BF16)
            self.C_d = self.dscr("C_d", [1024, T], BF16)
            self.rq_d = self.dscr("rq_d", [D, T], BF16)
            self.rk_d = self.dscr("rk_d", [D, T], BF16)
            self.rv_d = self.dscr("rv_d", [T, D], BF16)
            self.rg_d = self.dscr("rg_d", [D, T], F32)
            self.dt_d = self.dscr("dt_d", [128, T], F32)
            self.cum_d = self.dscr("cum_d", [128, T], F32)
            self.ym_d = self.dscr("ym_d", [4096, T], BF16)
        zsb, xsb, Bb, Cb, rqb, rkb, rvb, rgb, dtb, cumb, ymb = [Buf() for _ in range(11)]
        self.pw = [(P.sb([128, KC, 128], BF16, "pw%d" % i), Buf()) for i in range(2)]
        self.pw_i = 0
        rowf = [(P.sb([128, T], F32, "rowf%d" % i), Buf()) for i in range(2)]
        rowb = [(P.sb([128, T], BF16, "rowb%d" % i), Buf()) for i in range(2)]
        smallb = Buf()
        small = P.sb([128, 4], F32, "ssdsmall")
        P.dma("sp", small[:], self.ssd_small, writes=[smallb])
        mA = P.mark()
        cw = P.sb([128, 32 * 4], F32, "cw")
        cb_ = P.sb([128, 32], F32, "cb")
        P.dma("sp", cw[:], self.conv_w, writes=[smallb])
        P.dma("sp", cb_[:], self.conv_b, writes=[smallb])
        ci = [0]

        def silu_to(dst_d, dbuf, row0):
            rt, rb = rowf[ci[0] % 2]
            ci[0] += 1

            def h(tt, pt, pb):
                P.act(rt[:, tt * 512:(tt + 1) * 512], pt[:, :], AF.Silu, reads=[pb], writes=[rb])
            return h, (lambda: P.dma("sp", dst_d[row0:row0 + 128, :], rt[:, :], reads=[rb], writes=[dbuf]))

        for c in range(16):
            h, fin = silu_to(self.zs_d, zsb, c * 128)
            self.proj_fm(hT, hbuf, W, IN0_OFF["z"] + c * 128, 128, h)
            fin()
        for c in range(16):
            h, fin = silu_to(self.rg_d, rgb, c * 128)
            self.proj_fm(hT, hbuf, W, IN0_OFF["rg"] + c * 128, 128, h)
            fin()
        us = [(P.sb([128, T + 4], F32, "u%d" % i), Buf()) for i in range(2)]
        for u, ub in us:
            P.memset("pool", u[:, 0:3], 0.0, writes=[ub])
        cacc = P.sb([128, T], F32, "cacc")
        caccb = Buf()
        for c in range(32):
            u, ub = us[c % 2]
            rt, rb = rowb[c % 2]

            def hx(tt, pt, pb, u=u, ub=ub):
                P.copy("act", u[:, 3 + tt * 512:3 + (tt + 1) * 512], pt[:, :], reads=[pb], writes=[ub])
            self.proj_fm(hT, hbuf, W, IN0_OFF["xbc"] + c * 128, 128, hx)
            P.ts("dve", cacc[:], u[:, 0:T], cw[:, c * 4:c * 4 + 1], ALU.mult, cb_[:, c:c + 1], ALU.add,
                 reads=[ub, smallb], writes=[caccb])
            for i in range(1, 4):
                P.stt(cacc[:], u[:, i:i + T], cw[:, c * 4 + i:c * 4 + i + 1], cacc[:], ALU.mult, ALU.add,
                      reads=[ub, smallb, caccb], writes=[caccb])
            P.act(rt[:], cacc[:], AF.Silu, reads=[caccb], writes=[rb])
            if c < 16:
                P.dma("sp", self.xs_d[c * 128:(c + 1) * 128, :], rt[:, :], reads=[rb], writes=[xsb])
            elif c < 24:
                P.dma("sp", self.B_d[(c - 16) * 128:(c - 15) * 128, :], rt[:, :], reads=[rb], writes=[Bb])
            else:
                P.dma("sp", self.C_d[(c - 24) * 128:(c - 23) * 128, :], rt[:, :], reads=[rb], writes=[Cb])
        dtT, dtTb = rowf[0]
        cumT, cumTb = rowf[1]
        acol = P.sb([128, 1], F32, "acol")
        P.act(acol[:], small[:, 0:1], AF.Exp, reads=[smallb], writes=[smallb])
        P.ts("dve", acol[:], acol[:], -1.0, ALU.mult, reads=[smallb], writes=[smallb])

        def hdt(tt, pt, pb):
            sl = slice(tt * 512, (tt + 1) * 512)
            P.act(dtT[:, sl], pt[:, :], AF.Exp, reads=[pb, smallb], writes=[dtTb], bias=small[:, 1:2], scale=1.0)
            P.act(dtT[:, sl], dtT[:, sl], AF.Ln, reads=[dtTb, self.cbuf], writes=[dtTb],
                  bias=self.cs["ones"][:, 0:1], scale=1.0)
        self.proj_fm(hT, hbuf, W, IN0_OFF["dt"], 32, hdt)
        P.ts("dve", cacc[:], dtT[:], acol[:, 0:1], ALU.mult, reads=[dtTb, smallb], writes=[caccb])
        for c in range(16):
            sl = slice(c * 128, (c + 1) * 128)
            P.op("dve", lambda e, sl=sl: e.tensor_tensor_scan(out=cumT[:, sl], data0=self.cs["ones"][:, 0:128],
                                                             data1=cacc[:, sl], initial=0.0, op0=ALU.mult,
                                                             op1=ALU.add),
                 reads=[caccb, self.cbuf], writes=[cumTb])
        P.dma("sp", self.dt_d, dtT[:, :], reads=[dtTb], writes=[dtb])
        P.dma("sp", self.cum_d, cumT[:, :], reads=[cumTb], writes=[cumb])
        P.release(mA)
        cosr = P.sb([128, T], F32, "cosr")
        sinr = P.sb([128, T], F32, "sinr")
        trb = Buf()
        m2 = P.mark()
        pos_f, pb_ = self.load_pos()
        tmp = (P.sb([128, T], F32, "tv"), P.sb([128, T], I32, "tvi"), P.sb([128, T], F32, "tvf"), Buf())
        self.trig(cosr, trb, pos_f, pb_, self.cs["inv"][:, 0:1], 0.25, tmp)
        self.trig(sinr, trb, pos_f, pb_, self.cs["inv"][:, 0:1], 0.0, tmp)
        P.release(m2)
        t1 = P.sb([128, T], F32, "rt1")
        t2 = P.sb([128, T], F32, "rt2")
        t12b = Buf()
        rowf4 = rowf + [(P.sb([128, T], F32, "rowf%d" % i), Buf()) for i in (2, 3)]
        hq = 0
        for name, dst, dbuf in (("rq", self.rq_d, rqb), ("rk", self.rk_d, rkb)):
            for hh in range(8):
                x1, x1b = rowf4[(hq % 2) * 2]
                x2, x2b = rowf4[(hq % 2) * 2 + 1]
                hq += 1

                def h1(tt, pt, pb):
                    P.copy("act", x1[:, tt * 512:(tt + 1) * 512], pt[:, :], reads=[pb], writes=[x1b])

                def h2(tt, pt, pb):
                    P.copy("act", x2[:, tt * 512:(tt + 1) * 512], pt[:, :], reads=[pb], writes=[x2b])
                self.proj_fm(hT, hbuf, W, IN0_OFF[name] + hh * 256, 128, h1)
                self.proj_fm(hT, hbuf, W, IN0_OFF[name] + hh * 256 + 128, 128, h2)
                o1, o1b = rowb[0]
                o2, o2b = rowb[1]
                P.tt("dve", t1[:], x1[:], cosr[:], ALU.mult, reads=[x1b, trb], writes=[t12b])
                P.tt("pool", t2[:], x2[:], sinr[:], ALU.mult, reads=[x2b, trb], writes=[t12b])
                P.tt("dve", o1[:], t1[:], t2[:], ALU.subtract, reads=[t12b], writes=[o1b])
                P.tt("dve", t1[:], x1[:], sinr[:], ALU.mult, reads=[x1b, trb, o1b], writes=[t12b])
                P.tt("pool", t2[:], x2[:], cosr[:], ALU.mult, reads=[x2b, trb, o1b], writes=[t12b])
                P.tt("dve", o2[:], t1[:], t2[:], ALU.add, reads=[t12b], writes=[o2b])
                P.dma("sp", dst[hh * 256:hh * 256 + 128, :], o1[:, :], reads=[o1b], writes=[dbuf])
                P.dma("sp", dst[hh * 256 + 128:hh * 256 + 256, :], o2[:, :], reads=[o2b], writes=[dbuf])
        wv = P.sb([128, KC, 512], BF16, "wv")
        wvb = Buf()
        vo = [(P.sb([128, 512], BF16, "vo%d" % i), Buf()) for i in range(2)]
        for cbk in range(4):
            c0 = IN0_OFF["rv"] + cbk * 512
            self.wload(wv[:, :, :], wvb, W[:, c0:c0 + 512].rearrange("(k p) n -> p k n", p=128))
            for t16 in range(16):
                pt, pb = P.ps()
                for k in range(KC):
                    P.mm(pt[:, :], lhsT=hT[:, k, t16 * 128:(t16 + 1) * 128], rhs=wv[:, k, :], start=(k == 0),
                         stop=(k == KC - 1), reads=[wvb, hbuf[t16 // 4]], writes=[pb])
                v_, vb_ = vo[t16 % 2]
                P.copy("act" if t16 % 2 else "dve", v_[:], pt[:, :], reads=[pb], writes=[vb_])
                P.dma("sp", self.rv_d[t16 * 128:(t16 + 1) * 128, cbk * 512:(cbk + 1) * 512], v_[:, :], reads=[vb_],
                      writes=[rvb])
        P.release(m)

        ld = lambda shp, dt, nm: P.sb(shp, dt, nm)
        selh = ld([128, 32 * 128], F32, "selh")
        expd = ld([128, 2048], F32, "expd")
        Dc = ld([128, 16], F32, "Dc")
        snw = ld([128, 16], F32, "snw")
        cbf = Buf()
        P.dma("sp", selh[:], self.cst_d["selh"], writes=[cbf])
        P.dma("sp", expd[:], self.cst_d["expand"], writes=[cbf])
        P.dma("sp", Dc[:], self.ssd_Dc, writes=[cbf])
        P.dma("sp", snw[:], self.ssd_nw, writes=[cbf])
        S = ld([128, 2048], F32, "S")
        Sbf_e = ld([128, 16, 128], BF16, "Sbfe")
        Sbf_o = ld([128, 16, 128], BF16, "Sbfo")
        xdt_e = ld([128, 16, 128], BF16, "xdte")
        xdt_o = ld([128, 16, 128], BF16, "xdto")
        xdtw = ld([128, 2048], BF16, "xdtw")
        Btm = ld([128, 8, 128], BF16, "Btm")
        Sb, xdb, Btb = Buf(), Buf(), Buf()
        for t_ in (S, Sbf_e, Sbf_o, xdt_e, xdt_o):
            P.memset("pool", t_[:], 0.0, writes=[Sb])
        ach = ld([128, 2048], F32, "ach")
        achb = Buf()
        D2 = ld([128, 2048], F32, "D2")
        yraw = ld([128, 16, 128], F32, "yraw")
        yrb = Buf()
        sqy = ld([128, 2048], F32, "sqy")
        sqb = Buf()
        yab = ld([128, 16, 128], BF16, "yab")
        yabb = Buf()
        sms = [{k: ld([128, 32], F32, k + str(i)) for k in ("dttm", "cumc", "negc", "ds", "w2", "D1")} for i in range(2)]
        smbs = [Buf(), Buf()]
        CBm = [(ld([128, 128], F32, "CBm%d" % i), Buf()) for i in range(3)]
        NH = 4
        hb = [dict(arg=ld([128, 128], F32, "arg%d" % i), E=ld([128, 128], F32, "E%d" % i),
                   G=ld([128, 128], BF16, "G%d" % i), ec=ld([128, 128], F32, "ec%d" % i),
                   Cd=ld([128, 128], BF16, "Cd%d" % i), argb=Buf(), Eb=Buf(), Gb=Buf(), ecb=Buf(), Cdb=Buf())
              for i in range(NH)]
        yraws = [(yraw, yrb), (ld([128, 16, 128], F32, "yraw2"), Buf())]
        rstd = ld([128, 128], F32, "rstdy")
        lds = []
        for i in range(2):
            lds.append(dict(BT=ld([128, 8, 128], BF16, "BT%d" % i),
                            CT=ld([128, 8, 128], BF16, "CT%d" % i),
                            dt=ld([128, 128], F32, "dtc%d" % i), cum=ld([128, 128], F32, "cumc%d" % i), b=Buf()))
        eds = [dict(xsT=ld([128, 16, 128], BF16, "xsT%d" % i), zs=ld([128, 16, 128], F32, "zs%d" % i), b=Buf())
               for i in range(3)]
        xdt_e2 = ld([128, 16, 128], BF16, "xdte2")
        xdt_o2 = ld([128, 16, 128], BF16, "xdto2")
        xdtw2 = ld([128, 2048], BF16, "xdtw2")
        Btm2 = ld([128, 8, 128], BF16, "Btm2")
        ach2 = ld([128, 2048], F32, "ach2")
        xdb2, Btb2, achb2 = Buf(), Buf(), Buf()
        for t_ in (xdt_e2, xdt_o2):
            P.memset("pool", t_[:], 0.0, writes=[xdb2])
        pro = [dict(xe=xdt_e, xo=xdt_o, xw=xdtw, xb=xdb, Bt=Btm, Btb=Btb, ach=ach, achb=achb),
               dict(xe=xdt_e2, xo=xdt_o2, xw=xdtw2, xb=xdb2, Bt=Btm2, Btb=Btb2, ach=ach2, achb=achb2)]
        D2b = Buf()
        iy0, (yp0, yp0b) = P.ps_reserve()
        iy1, (yp1, yp1b) = P.ps_reserve()
        yps = [(yp0, yp0b), (yp1, yp1b)]
        mgen = None
        if self.overlap_mods:
            imods, mods_bank = P.ps_reserve()
            mgen = self.mods_gen(1, mods_bank)
        ident = self.cs["ident"]
        ones = self.cs["ones"]
        cnt = dict(h=0, g=0)

        def prologue(c):
            L = lds[c % 2]
            lb = L["b"]
            sm = sms[c % 2]
            smb = smbs[c % 2]
            Q = pro[c % 2]
            sl = slice(c * 128, (c + 1) * 128)
            E = eds[c % 3]
            eb = E["b"]
            P.dma("sp", E["xsT"][:, :, :], self.xs_d[:, sl].rearrange("(k p) t -> p k t", p=128), reads=[xsb], writes=[eb])
            P.dma("sp", L["BT"][:, :, :], self.B_d[:, sl].rearrange("(k p) t -> p k t", p=128), reads=[Bb, lb], writes=[lb])
            P.dma("sp", L["CT"][:, :, :], self.C_d[:, sl].rearrange("(k p) t -> p k t", p=128), reads=[Cb, lb], writes=[lb])
            P.dma("sp", E["zs"][:, :, :], self.zs_d[:, sl].rearrange("(k p) t -> p k t", p=128), reads=[zsb, eb], writes=[eb])
            P.dma("sp", L["dt"][:, :], self.dt_d[:, sl], reads=[dtb, lb], writes=[lb])
            P.dma("sp", L["cum"][:, :], self.cum_d[:, sl], reads=[cumb, lb], writes=[lb])
            pt, pb = P.ps()
            P.tr(pt[:, 0:128], L["dt"][:, :], ident[:], reads=[lb, self.cbuf], writes=[pb], inc=False)
            P.tr(pt[:, 128:256], L["cum"][:, :], ident[:], reads=[lb, self.cbuf], writes=[pb])
            P.ts("dve", sm["D1"][:], ident[:, 0:32], L["cum"][:, 127:128], ALU.mult, reads=[lb, self.cbuf, smb],
                 writes=[smb])
            pt1, pb1 = P.ps()
            P.mm(pt1[:, 0:32], lhsT=ones[:], rhs=sm["D1"][:], start=True, stop=True, reads=[smb, self.cbuf], writes=[pb1])
            P.copy("dve", sm["dttm"][:], pt[:, 0:32], reads=[pb], writes=[smb])
            P.copy("dve", sm["cumc"][:], pt[:, 128:160], reads=[pb, smb], writes=[smb])
            P.ts("dve", sm["negc"][:], sm["cumc"][:], -1.0, ALU.mult, reads=[smb], writes=[smb])
            P.tt("dve", sm["ds"][:], pt1[:, 0:32], sm["cumc"][:], ALU.subtract, reads=[pb1, smb], writes=[smb])
            P.act(sm["ds"][:], sm["ds"][:], AF.Exp, reads=[smb], writes=[smb])
            P.tt("dve", sm["w2"][:], sm["ds"][:], sm["dttm"][:], ALU.mult, reads=[smb], writes=[smb])
            P.ts("dve", D2[:], expd[:], L["cum"][:, 127:128], ALU.mult, reads=[lb, cbf], writes=[D2b])
            pds = []
            for q4 in range(4):
                ptd, pbd = P.ps()
                P.mm(ptd[:, :], lhsT=ones[:], rhs=D2[:, q4 * 512:(q4 + 1) * 512], start=True, stop=True,
                     reads=[D2b, self.cbuf], writes=[pbd])
                pds.append((ptd, pbd))
            for q4 in range(4):
                ptd, pbd = pds[q4]
                P.act(Q["ach"][:, q4 * 512:(q4 + 1) * 512], ptd[:, :], AF.Exp, reads=[pbd], writes=[Q["achb"]])
            for q4 in range(4):
                ptx, pbx = P.ps()
                for i4 in range(4):
                    P.tr(ptx[:, i4 * 128:(i4 + 1) * 128], E["xsT"][:, q4 * 4 + i4, :], self.ident_bf[:],
                         reads=[eb, self.cbuf], writes=[pbx], inc=(i4 == 3))
                src = ptx[:, 0:512].rearrange("p (a b d) -> p a b d", a=4, b=2)
                dtv = sm["dttm"][:, q4 * 8:(q4 + 1) * 8].rearrange("p (a b) -> p a b", b=2)
                w2v = sm["w2"][:, q4 * 8:(q4 + 1) * 8].rearrange("p (a b) -> p a b", b=2)
                xwv = Q["xw"][:, q4 * 512:(q4 + 1) * 512].rearrange("p (a b d) -> p a b d", a=4, b=2)
                P.tt("dve", Q["xe"][:, q4 * 4:(q4 + 1) * 4, 0:64], src[:, :, 0, :],
                     dtv[:, :, 0].unsqueeze(2).broadcast_to([128, 4, 64]), ALU.mult, reads=[pbx, smb], writes=[Q["xb"]])
                P.tt("dve", Q["xo"][:, q4 * 4:(q4 + 1) * 4, 64:128], src[:, :, 1, :],
                     dtv[:, :, 1].unsqueeze(2).broadcast_to([128, 4, 64]), ALU.mult, reads=[pbx, smb], writes=[Q["xb"]])
                for par in range(2):
                    P.tt("dve", xwv[:, :, par, :], src[:, :, par, :],
                         w2v[:, :, par].unsqueeze(2).broadcast_to([128, 4, 64]), ALU.mult, reads=[pbx, smb],
                         writes=[Q["xb"]])
            for q2 in range(2):
                ptx, pbx = P.ps()
                for i4 in range(4):
                    P.tr(ptx[:, i4 * 128:(i4 + 1) * 128], L["BT"][:, q2 * 4 + i4, :], self.ident_bf[:],
                         reads=[lb, self.cbuf], writes=[pbx], inc=(i4 == 3))
                P.copy("act", Q["Bt"][:, q2 * 4:(q2 + 1) * 4, :], ptx[:, 0:512].rearrange("p (a d) -> p a d", a=4),
                       reads=[pbx], writes=[Q["Btb"]])

        def heads(c, gens=()):
            L = lds[c % 2]
            lb = L["b"]
            sm = sms[c % 2]
            smb = smbs[c % 2]
            Q = pro[c % 2]
            live = {}
            cms = {}

            def front(h):
                g = h // 4
                if h % 4 == 0:
                    cm, cmb = CBm[cnt["g"] % 3]
                    cnt["g"] += 1
                    pt, pb = P.ps()
                    P.mm(pt[:, 0:128], lhsT=L["BT"][:, g, :], rhs=L["CT"][:, g, :], start=True, stop=True, reads=[lb],
                         writes=[pb])
                    P.tt("dve", cm[:], pt[:, 0:128], self.cs["triT"][:], ALU.mult, reads=[pb, self.cbuf], writes=[cmb])
                    cms[g] = (cm, cmb)
                cm, cmb = cms[g]
                H = hb[cnt["h"] % NH]
                cnt["h"] += 1
                pt, pb = P.ps()
                P.mm(pt[:, 0:128], lhsT=selh[:, h * 128:(h + 1) * 128], rhs=L["cum"][:, :], start=True, stop=True,
                     reads=[lb, cbf], writes=[pb])
                P.ts("dve", H["arg"][:], pt[:, 0:128], sm["negc"][:, h:h + 1], ALU.add, 0.0, ALU.min,
                     reads=[pb, smb], writes=[H["argb"]])
                P.act(H["ec"][:], pt[:, 0:128], AF.Exp, reads=[pb, H["argb"]], writes=[H["ecb"]])
                P.act(H["E"][:], H["arg"][:], AF.Exp, reads=[H["argb"]], writes=[H["Eb"]])
                P.tt("pool", H["G"][:], H["E"][:], cm[:], ALU.mult, reads=[H["Eb"], cmb], writes=[H["Gb"]])
                P.tt("dve", H["Cd"][:], L["CT"][:, g, :], H["ec"][:], ALU.mult, reads=[lb, H["ecb"]], writes=[H["Cdb"]])
                live[h] = H

            def back(h):
                g = h // 4
                H = live.pop(h)
                yp, ypb = yps[g % 2]
                xd = Q["xe"] if h % 2 == 0 else Q["xo"]
                sb_ = Sbf_e if h % 2 == 0 else Sbf_o
                col = ((h // 2) % 2) * 128
                P.mm(yp[:, col:col + 128], lhsT=xd[:, h // 2, :], rhs=H["G"][:], start=(h % 2 == 0), stop=False,
                     reads=[Q["xb"], H["Gb"]], writes=[ypb], inc=False)
                P.mm(yp[:, col:col + 128], lhsT=sb_[:, h // 2, :], rhs=H["Cd"][:], start=False, stop=(h % 2 == 1),
                     reads=[Sb, H["Cdb"]], writes=[ypb], inc=True)
                if h % 4 == 3:
                    yr_, yrb_ = yraws[c % 2]
                    P.copy("act", yr_[:, 2 * g:2 * g + 2, :], yp[:, 0:256].rearrange("p (a d) -> p a d", a=2),
                           reads=[ypb], writes=[yrb_])
                for gen in gens:
                    next(gen, None)

            LA = NH - 2
            for i in range(32 + LA):
                if i < 32:
                    front(i)
                if i - LA >= 0:
                    back(i - LA)

        def update(c):
            Q = pro[c % 2]
            pts = []
            for q4 in range(4):
                pt, pb = P.ps()
                for gg in range(2):
                    g = q4 * 2 + gg
                    P.mm(pt[:, gg * 256:(gg + 1) * 256], lhsT=Q["Bt"][:, g, :], rhs=Q["xw"][:, g * 256:(g + 1) * 256],
                         start=True, stop=True, reads=[Q["Btb"], Q["xb"]], writes=[pb], inc=(gg == 1))
                pts.append((pt, pb))
            for q4 in range(4):
                pt, pb = pts[q4]
                sl4 = slice(q4 * 512, (q4 + 1) * 512)
                P.tt("dve", S[:, sl4], S[:, sl4], Q["ach"][:, sl4], ALU.mult, reads=[Q["achb"], Sb], writes=[Sb])
                P.tt("dve", S[:, sl4], S[:, sl4], pt[:, :], ALU.add, reads=[pb, Sb], writes=[Sb])
            Sv = S[:, :].rearrange("p (a b d) -> p a b d", a=16, b=2)
            P.copy("pool", Sbf_e[:, :, 0:64], Sv[:, :, 0, :], reads=[Sb], writes=[Sb])
            P.copy("pool", Sbf_o[:, :, 64:128], Sv[:, :, 1, :], reads=[Sb], writes=[Sb])

        def epilogue(c):
            E = eds[c % 3]
            eb = E["b"]
            yraw, yrb = yraws[c % 2]
            sl = slice(c * 128, (c + 1) * 128)
            for k in range(16):
                P.stt(yraw[:, k, :], E["xsT"][:, k, :], Dc[:, k:k + 1], yraw[:, k, :], ALU.mult, ALU.add,
                      reads=[eb, cbf, yrb], writes=[yrb])
                if k % 4 == 3:
                    yield
            yflat = yraw[:, :, :].rearrange("p a d -> p (a d)")
            P.tt("dve", yflat, yflat, E["zs"][:, :, :].rearrange("p a d -> p (a d)"), ALU.mult, reads=[eb, yrb],
                 writes=[yrb])
            yield
            P.act(sqy[:], yflat, AF.Square, reads=[yrb], writes=[sqb])
            yield
            pt, pb = P.ps()
            for k in range(16):
                P.mm(pt[:, 0:128], lhsT=ones[:], rhs=sqy[:, k * 128:(k + 1) * 128], start=(k == 0), stop=(k == 15),
                     reads=[sqb, self.cbuf], writes=[pb])
            rb_ = Buf()
            self.rstd_from_ss(pt[:, 0:128], pb, 2048, 128, rstd[:], rb_)
            yield
            P.tt("dve", yraw[:, :, :], yraw[:, :, :], rstd[:, :].unsqueeze(1).broadcast_to([128, 16, 128]), ALU.mult,
                 reads=[rb_, yrb], writes=[yrb])
            yield
            for k in range(16):
                P.act(yab[:, k, :], yraw[:, k, :], AF.Copy, reads=[yrb, cbf], writes=[yabb], scale=snw[:, k:k + 1])
                if k % 4 == 3:
                    yield
            P.dma("sp", self.ym_d[0:2048, sl].rearrange("(k p) t -> p k t", p=128), yab[:, :, :], reads=[yabb],
                  writes=[ymb])

        prologue(0)
        prev_ep = None
        for c in range(16):
            if c + 1 < 16:
                prologue(c + 1)
            gl = ([mgen] if mgen is not None else []) + ([prev_ep] if prev_ep is not None else [])
            heads(c, gens=gl)
            if prev_ep is not None:
                for _ in prev_ep:
                    pass
            update(c)
            prev_ep = epilogue(c)
        for _ in prev_ep:
            pass
        if mgen is not None:
            for _ in mgen:
                pass
            P.ps_unreserve(imods)
        P.ps_unreserve(iy0)
        P.ps_unreserve(iy1)
        P.release(m)

        dm = ld([128, 8, 128], F32, "dm")
        qdt = ld([128, 8, 128], F32, "qdt")
        rnw = ld([128, 16], F32, "rnw")
        cbf = Buf()
        P.dma("sp", dm[:, :, :].rearrange("p a d -> p (a d)"), self.cst_d["ret_dm"], writes=[cbf])
        P.dma("sp", qdt[:, :, :].rearrange("p a d -> p (a d)"), self.cst_d["ret_qd"], writes=[cbf])
        P.dma("sp", rnw[:], self.ret_nw, writes=[cbf])
        kd = self.cs["ret_kd"]
        cdv = host_consts()["_ret_cd"]
        R = ld([128, 8, 512], F32, "R")
        Rbf = ld([128, 8, 512], BF16, "Rbf")
        Rb = Buf()
        P.memset("pool", R[:], 0.0, writes=[Rb])
        P.memset("pool", Rbf[:], 0.0, writes=[Rb])
        yraw = ld([128, 16, 128], F32, "yrawr")
        yrb = Buf()
        sqy = ld([128, 2048], F32, "sqyr")
        sqb = Buf()
        yab = ld([128, 16, 128], BF16, "yabr")
        yabb = Buf()
        rstd8 = ld([128, 8, 128], F32, "rstd8")
        hb = [dict(G=ld([128, 128], BF16, "rG%d" % i), qd=ld([128, 2, 128], BF16, "rqd%d" % i),
                   kw=ld([128, 256], BF16, "rkw%d" % i), Gb=Buf(), qdb=Buf(), kwb=Buf()) for i in range(4)]
        cnt = dict(h=0)
        lds = []
        for i in range(3):
            lds.append(dict(qT=ld([128, 16, 128], BF16, "rqT%d" % i), kT=ld([128, 16, 128], BF16, "rkT%d" % i),
                            v=ld([128, 2048], BF16, "rv%d" % i), gs=ld([128, 16, 128], F32, "rgs%d" % i), b=Buf()))
        yraws = [(yraw, yrb), (ld([128, 16, 128], F32, "yrawr2"), Buf())]
        iy0, (yp0, yp0b) = P.ps_reserve()
        iy1, (yp1, yp1b) = P.ps_reserve()
        yps = [(yp0, yp0b), (yp1, yp1b)]

        def loads(c):
            L = lds[c % 3]
            lb = L["b"]
            sl = slice(c * 128, (c + 1) * 128)
            P.dma("sp", L["qT"][:, :, :], self.rq_d[:, sl].rearrange("(k p) t -> p k t", p=128), reads=[rqb], writes=[lb])
            P.dma("sp", L["kT"][:, :, :], self.rk_d[:, sl].rearrange("(k p) t -> p k t", p=128), reads=[rkb, lb], writes=[lb])
            P.dma("sp", L["v"][:, :], self.rv_d[sl, :], reads=[rvb, lb], writes=[lb])
            P.dma("sp", L["gs"][:, :, :], self.rg_d[:, sl].rearrange("(k p) t -> p k t", p=128), reads=[rgb, lb], writes=[lb])

        def rheads(c, gens=()):
            L = lds[c % 3]
            lb = L["b"]
            yraw_, yrb_ = yraws[c % 2]
            live = {}

            def front(h):
                H = hb[cnt["h"] % 4]
                cnt["h"] += 1
                pt, pb = P.ps()
                P.mm(pt[:, 0:128], lhsT=L["kT"][:, 2 * h, :], rhs=L["qT"][:, 2 * h, :], start=True, stop=False,
                     reads=[lb], writes=[pb], inc=False)
                P.mm(pt[:, 0:128], lhsT=L["kT"][:, 2 * h + 1, :], rhs=L["qT"][:, 2 * h + 1, :], start=False, stop=True,
                     reads=[lb], writes=[pb])
                pt2, pb2 = P.ps()
                P.tr(pt2[:, 0:128], L["kT"][:, 2 * h, :], self.ident_bf[:], reads=[lb, self.cbuf], writes=[pb2], inc=False)
                P.tr(pt2[:, 128:256], L["kT"][:, 2 * h + 1, :], self.ident_bf[:], reads=[lb, self.cbuf], writes=[pb2])
                P.tt("dve", H["G"][:], pt[:, 0:128], dm[:, h, :], ALU.mult, reads=[pb, cbf], writes=[H["Gb"]])
                P.tt("pool", H["qd"][:, :, :], L["qT"][:, 2 * h:2 * h + 2, :],
                     qdt[:, h, :].unsqueeze(1).broadcast_to([128, 2, 128]), ALU.mult, reads=[lb, cbf], writes=[H["qdb"]])
                P.ts("dve", H["kw"][:], pt2[:, 0:256], kd[:, h:h + 1], ALU.mult, reads=[pb2, self.cbuf], writes=[H["kwb"]])
                live[h] = H

            def back(h):
                H = live.pop(h)
                yp, ypb = yps[h % 2]
                for vc in range(2):
                    col = vc * 128
                    P.mm(yp[:, col:col + 128], lhsT=L["v"][:, h * 256 + vc * 128:h * 256 + (vc + 1) * 128], rhs=H["G"][:],
                         start=True, stop=False, reads=[lb, H["Gb"]], writes=[ypb], inc=False)
                    P.mm(yp[:, col:col + 128], lhsT=Rbf[:, h, vc * 128:(vc + 1) * 128], rhs=H["qd"][:, 0, :],
                         start=False, stop=False, reads=[Rb, H["qdb"]], writes=[ypb], inc=False)
                    P.mm(yp[:, col:col + 128], lhsT=Rbf[:, h, 256 + vc * 128:256 + (vc + 1) * 128], rhs=H["qd"][:, 1, :],
                         start=False, stop=True, reads=[Rb, H["qdb"]], writes=[ypb], inc=True)
                pt3, pb3 = P.ps()
                for dc in range(2):
                    P.mm(pt3[:, dc * 256:(dc + 1) * 256], lhsT=H["kw"][:, dc * 128:(dc + 1) * 128],
                         rhs=L["v"][:, h * 256:(h + 1) * 256], start=True, stop=True, reads=[lb, H["kwb"]], writes=[pb3],
                         inc=(dc == 1))
                P.copy("act", yraw_[:, 2 * h:2 * h + 2, :], yp[:, 0:256].rearrange("p (a d) -> p a d", a=2),
                       reads=[ypb], writes=[yrb_])
                P.stt(R[:, h, :], R[:, h, :], float(cdv[h]), pt3[:, :], ALU.mult, ALU.add, reads=[pb3, Rb], writes=[Rb])
                P.copy("pool", Rbf[:, h, :], R[:, h, :], reads=[Rb], writes=[Rb])
                for gen in gens:
                    next(gen, None)
                    next(gen, None)

            LA = 2
            for i in range(8 + LA):
                if i < 8:
                    front(i)
                if i - LA >= 0:
                    back(i - LA)

        def repilogue(c):
            L = lds[c % 3]
            lb = L["b"]
            yraw_, yrb_ = yraws[c % 2]
            sl = slice(c * 128, (c + 1) * 128)
            yflat = yraw_[:, :, :].rearrange("p a d -> p (a d)")
            P.act(sqy[:], yflat, AF.Square, reads=[yrb_], writes=[sqb])
            yield
            for q2 in range(2):
                pt, pb = P.ps()
                for hh in range(4):
                    h = q2 * 4 + hh
                    for vc in range(2):
                        P.mm(pt[:, hh * 128:(hh + 1) * 128], lhsT=ones[:],
                             rhs=sqy[:, (2 * h + vc) * 128:(2 * h + vc + 1) * 128], start=(vc == 0), stop=(vc == 1),
                             reads=[sqb, self.cbuf], writes=[pb], inc=(vc == 1 and hh == 3))
                rb_ = Buf()
                self.rstd_from_ss(pt[:, :], pb, 256, 512, rstd8[:, q2 * 4:(q2 + 1) * 4, :].rearrange("p a d -> p (a d)"),
                                  rb_)
                yv = yraw_[:, q2 * 8:(q2 + 1) * 8, :].rearrange("p (a b) d -> p a b d", b=2)
                for vc in range(2):
                    P.tt("dve", yv[:, :, vc, :], yv[:, :, vc, :], rstd8[:, q2 * 4:(q2 + 1) * 4, :], ALU.mult,
                         reads=[rb_, yrb_], writes=[yrb_])
                yield
            P.tt("dve", yflat, yflat, L["gs"][:, :, :].rearrange("p a d -> p (a d)"), ALU.mult, reads=[lb, yrb_],
                 writes=[yrb_])
            yield
            for k in range(16):
                P.act(yab[:, k, :], yraw_[:, k, :], AF.Copy, reads=[yrb_, cbf], writes=[yabb], scale=rnw[:, k:k + 1])
                if k % 4 == 3:
                    yield
            P.dma("sp", self.ym_d[2048:4096, sl].rearrange("(k p) t -> p k t", p=128), yab[:, :, :], reads=[yabb],
                  writes=[ymb])

        loads(0)
        prev = None
        for c in range(16):
            if c + 1 < 16:
                loads(c + 1)
            rheads(c, gens=[prev] if prev is not None else [])
            if prev is not None:
                for _ in prev:
                    pass
            prev = repilogue(c)
        for _ in prev:
            pass
        P.ps_unreserve(iy0)
        P.ps_unreserve(iy1)
        P.release(m)
        self.stage_down(self.ym_d, ymb, self.hy_out, 32)

    def load_pos(self):
        P = self.P
        pb = Buf()
        pos_i = P.sb([128, T], I32, "pos_i")
        pos_f = P.sb([128, T], F32, "pos_f")
        P.dma("sp", pos_i[:], self.posin.partition_broadcast(128), writes=[pb])
        P.copy("dve", pos_f[:], pos_i[:], reads=[pb], writes=[pb])
        return pos_f, pb

    def trig(self, out_t, out_buf, pos_f, pb, invcol, phase, tmp):
        P = self.P
        v, vi, vf, tb = tmp
        INV2PI = float(1.0 / (2 * np.pi))
        P.ts("dve", v[:], pos_f[:], invcol, ALU.mult, reads=[pb, self.cbuf], writes=[tb])
        P.ts("dve", v[:], v[:], INV2PI, ALU.mult, float(phase), ALU.add, reads=[tb], writes=[tb])
        P.copy("dve", vi[:], v[:], reads=[tb], writes=[tb])
        P.copy("dve", vf[:], vi[:], reads=[tb], writes=[tb])
        P.tt("dve", v[:], v[:], vf[:], ALU.subtract, reads=[tb], writes=[tb])
        P.stt(v[:], v[:], 0.5, v[:], ALU.is_gt, ALU.subtract, reads=[tb], writes=[tb])
        P.stt(v[:], v[:], 0.5, v[:], ALU.is_gt, ALU.subtract, reads=[tb], writes=[tb])
        P.act(out_t[:], v[:], AF.Sin, reads=[tb], writes=[out_buf], scale=float(2 * np.pi))

    def proj_fm(self, hT, hbuf, W, col0, ncols, handler, dup=False):
        P = self.P
        w, wb = self.pw[self.pw_i % 2]
        self.pw_i += 1
        if ncols < 128:
            P.memset("pool", w[:, :, :], 0.0, writes=[wb])
        self.wload(w[:, :, 0:ncols], wb, W[:, col0:col0 + ncols].rearrange("(k p) n -> p k n", p=128))
        if dup:
            wb2 = Buf()
            P.dma("pool", w[:, :, ncols:2 * ncols], W[:, col0:col0 + ncols].rearrange("(k p) n -> p k n", p=128),
                  writes=[wb2])
        prev = None
        for tt in range(T // 512):
            pt, pb = P.ps()
            for k in range(KC):
                P.mm(pt[:, :], lhsT=w[:, k, :], rhs=hT[:, k, tt * 512:(tt + 1) * 512], start=(k == 0),
                     stop=(k == KC - 1), reads=[wb, hbuf[tt]] + ([wb2] if dup else []), writes=[pb])
            if prev is not None:
                handler(*prev)
            prev = (tt, pt, pb)
        handler(*prev)

    def rope_fm(self, xf, xfb, pm, cos_t, sin_t, tbuf, tt, out_ap, out_buf, tmp2):
        P = self.P
        pt, pb = P.ps()
        P.mm(pt[:, :], lhsT=pm[:], rhs=xf[:], start=True, stop=True, reads=[xfb, self.cbuf], writes=[pb])
        t1, t2, t12b = tmp2
        sl = slice(tt * 512, (tt + 1) * 512)
        P.tt("pool", t1[:], xf[:], cos_t[:, sl], ALU.mult, reads=[xfb, tbuf], writes=[t12b])
        P.tt("dve", t2[:], pt[:, :], sin_t[:, sl], ALU.mult, reads=[pb, tbuf], writes=[t12b])
        P.tt("dve", out_ap, t1[:], t2[:], ALU.add, reads=[t12b], writes=[out_buf])

    def stage_dsa(self, hT, hbuf, m):
        P = self.P
        W = self.dsa_in
        if not hasattr(self, "q_d"):
            self.q_d = self.dscr("q_d", [D, T], BF16)
            self.qi_d = self.dscr("qi_d", [1024, T], F32)
            self.ao_d = self.dscr("ao_d", [D, T], BF16)
        qdb, qidb, aob = Buf(), Buf(), Buf()
        kT = P.sb([128, T], BF16, "kT")
        vtm = P.sb([128, 16, 128], BF16, "vtm")
        kiA = P.sb([128, T], F32, "kiA")
        kiB = P.sb([128, T], F32, "kiB")
        witm = P.sb([128, 16, 16], F32, "witm")
        resb = Buf()
        idxnw = P.sb([128, 1], F32, "idxnw")
        P.dma("sp", idxnw[:], self.idx_nw, writes=[resb])
        P.memset("pool", kiA[:], 0.0, writes=[resb])
        P.memset("pool", kiB[:], 0.0, writes=[resb])
        mp = P.mark()
        cosq = P.sb([128, T], F32, "cosq")
        sinq = P.sb([128, T], F32, "sinq")
        cosi = P.sb([128, T], F32, "cosi")
        sini = P.sb([128, T], F32, "sini")
        trb = Buf()
        m2 = P.mark()
        pos_f, pb_ = self.load_pos()
        tmp = (P.sb([128, T], F32, "tv"), P.sb([128, T], I32, "tvi"), P.sb([128, T], F32, "tvf"), Buf())
        inv = self.cs["inv"]
        self.trig(cosq, trb, pos_f, pb_, inv[:, 1:2], 0.25, tmp)
        self.trig(sinq, trb, pos_f, pb_, inv[:, 1:2], 0.0, tmp)
        self.trig(cosi, trb, pos_f, pb_, inv[:, 2:3], 0.25, tmp)
        self.trig(sini, trb, pos_f, pb_, inv[:, 2:3], 0.0, tmp)
        P.release(m2)
        self.pw = [(P.sb([128, KC, 128], BF16, "pw%d" % i), Buf()) for i in range(2)]
        self.pw_i = 0
        xfs = [(P.sb([128, 512], F32, "xf%d" % i), Buf()) for i in range(2)]
        tmp2s = [(P.sb([128, 512], F32, "t1_%d" % i), P.sb([128, 512], F32, "t2_%d" % i), Buf()) for i in range(2)]
        rowb = [(P.sb([128, T], BF16, "rowb%d" % i), Buf()) for i in range(2)]
        rowf = [(P.sb([128, T], F32, "rowf%d" % i), Buf()) for i in range(2)]
        cnt = [0]

        def roped(pm, cos_t, sin_t, out_tile, out_buf, scale_in=None):
            def h(tt, pt, pb):
                xf, xfb = xfs[cnt[0] % 2]
                t2 = tmp2s[cnt[0] % 2]
                cnt[0] += 1
                P.copy("act", xf[:], pt[:, :], reads=[pb], writes=[xfb])
                self.rope_fm(xf, xfb, pm, cos_t, sin_t, trb, tt, out_tile[:, tt * 512:(tt + 1) * 512], out_buf, t2)
            return h

        for hh in range(16):
            rt, rb = rowb[hh % 2]
            self.proj_fm(hT, hbuf, W, IN1_OFF["q"] + hh * 128, 128, roped(self.cs["pm_q"], cosq, sinq, rt, rb))
            P.dma("sp", self.q_d[hh * 128:(hh + 1) * 128, :], rt[:, :], reads=[rb], writes=[qdb])
        self.proj_fm(hT, hbuf, W, IN1_OFF["k"], 128, roped(self.cs["pm_q"], cosq, sinq, kT, resb))
        vT, vTb = rowb[0]

        def hv(tt, pt, pb):
            P.copy("act", vT[:, tt * 512:(tt + 1) * 512], pt[:, :], reads=[pb], writes=[vTb])
        self.proj_fm(hT, hbuf, W, IN1_OFF["v"], 128, hv)
        for kt in range(16):
            pt, pb = P.ps()
            ptb = pt
            P.tr(ptb[:, 0:128], vT[:, kt * 128:(kt + 1) * 128], self.ident_bf[:], reads=[vTb, self.cbuf], writes=[pb])
            P.copy("dve", vtm[:, kt, :], ptb[:, 0:128], reads=[pb], writes=[resb])
        for ch in range(8):
            rt, rb = rowf[ch % 2]
            self.proj_fm(hT, hbuf, W, IN1_OFF["qi"] + ch * 128, 128, roped(self.cs["pm_i"], cosi, sini, rt, rb))
            P.dma("sp", self.qi_d[ch * 128:(ch + 1) * 128, :], rt[:, :], reads=[rb], writes=[qidb])
        kir, kirb = rowf[0]

        def hki(tt, pt, pb):
            xf, xfb = xfs[cnt[0] % 2]
            t1, t2, t12b = tmp2s[cnt[0] % 2]
            cnt[0] += 1
            P.copy("act", xf[:], pt[:, :], reads=[pb], writes=[xfb])
            P.act(t1[:], pt[:, :], AF.Square, reads=[pb], writes=[t12b])
            p2, p2b = P.ps()
            P.mm(p2[:, :], lhsT=self.cs["bones"][:], rhs=t1[:], start=True, stop=True, reads=[t12b, self.cbuf],
                 writes=[p2b])
            self.rstd_from_ss(p2[:, :], p2b, 64, 512, t2[:], t12b)
            P.stt(xf[:], xf[:], idxnw[:, 0:1], t2[:], ALU.mult, ALU.mult, reads=[xfb, t12b, resb], writes=[xfb])
            t3 = tmp2s[cnt[0] % 2]
            self.rope_fm(xf, xfb, self.cs["pm_i"], cosi, sini, trb, tt, kir[:, tt * 512:(tt + 1) * 512], kirb, t3)
        self.proj_fm(hT, hbuf, W, IN1_OFF["ki"], 64, hki, dup=True)
        P.copy("dve", kiA[0:64, :], kir[0:64, :], reads=[kirb], writes=[resb])
        P.copy("dve", kiB[64:128, :], kir[64:128, :], reads=[kirb], writes=[resb])
        wiT, wiTb = rowf[1]

        def hwi(tt, pt, pb):
            P.act(wiT[:, tt * 512:(tt + 1) * 512], pt[:, :], AF.Copy, reads=[pb], writes=[wiTb], scale=1.0 / 32.0)
        self.proj_fm(hT, hbuf, W, IN1_OFF["wi"], 16, hwi)
        for j in range(16):
            pt, pb = P.ps()
            P.tr(pt[:, 0:128], wiT[:, j * 128:(j + 1) * 128], self.cs["ident"][:], reads=[wiTb, self.cbuf],
                 writes=[pb])
            P.copy("dve", witm[:, j, :], pt[:, 0:16], reads=[pb], writes=[resb])
        P.release(mp)

        accs = [(P.sb([128, T], F32, "acc%d" % i), Buf()) for i in range(2)]
        work = P.sb([128, T], F32, "work")
        workb = Buf()
        m8 = P.sb([128, 8], F32, "m8")
        thr = P.sb([128, 1], F32, "thr")
        sels = [(P.sb([128, T], BF16, "sel%d" % i), Buf()) for i in range(2)]
        selTs = [(P.sb([128, 16, 128], BF16, "selT%d" % i), Buf()) for i in range(2)]
        qTs = [(P.sb([128, 16 * 128], BF16, "qT%d" % i), Buf()) for i in range(2)]
        qiTs = [(P.sb([128, 8, 128], F32, "qiT%d" % i), Buf()) for i in range(2)]
        rl = [(P.sb([128, 512], F32, "rl%d" % i), Buf()) for i in range(3)]
        rw = [(P.sb([128, 512], F32, "rw%d" % i), Buf()) for i in range(2)]
        ef = [(P.sb([128, 512], BF16, "ef%d" % i), Buf()) for i in range(4)]
        pTs = [(P.sb([128, 512], BF16, "pT%d" % i), Buf()) for i in range(4)]
        rsum = P.sb([128, 512], F32, "rsum")
        rsb = Buf()
        aos = [(P.sb([128, 512], BF16, "ao%d" % i), Buf()) for i in range(2)]
        iO, (accO, accOb) = P.ps_reserve()
        iS, (accS, accSb) = P.ps_reserve()
        iO2, (accO2, accOb2) = P.ps_reserve()
        iS2, (accS2, accSb2) = P.ps_reserve()
        accsets = [(accO, accOb, accS, accSb), (accO2, accOb2, accS2, accSb2)]
        SC = float(128.0 ** -0.5)
        cn = dict(c1=0, c2=0, c3=0)

        def score_part(j):
            Wj = 128 * (j + 1)
            nseg = (Wj + 511) // 512
            qT, qTb = qTs[j % 2]
            qiT, qiTb = qiTs[j % 2]
            acc, accb = accs[j % 2]
            sel, selb = sels[j % 2]
            P.dma("sp", qT[:, :].rearrange("p (h t) -> p h t", h=16),
                  self.q_d[:, j * 128:(j + 1) * 128].rearrange("(h p) t -> p h t", p=128), reads=[qdb], writes=[qTb])
            P.dma("sp", qiT[:, :, :],
                  self.qi_d[:, j * 128:(j + 1) * 128].rearrange("(h p) t -> p h t", p=128), reads=[qidb],
                  writes=[qiTb])
            if j > 0:
                P.memset("pool", acc[:, 0:Wj - 128], 0.0, writes=[accb])
            P.copy("pool", acc[:, Wj - 128:Wj], self.cs["cbias"][:], reads=[self.cbuf], writes=[accb])
            for hi in range(16):
                rk = kiA if hi % 2 == 0 else kiB
                for sg in range(nseg):
                    s0 = sg * 512
                    s1 = min(Wj, s0 + 512)
                    pt, pb = P.ps()
                    P.mm(pt[:, 0:s1 - s0], lhsT=qiT[:, hi // 2, :], rhs=rk[:, s0:s1], start=True, stop=True,
                         reads=[qiTb, resb], writes=[pb])
                    r, rb_ = rl[cn["c1"] % 3]
                    r2, rb2 = rw[cn["c1"] % 2]
                    cn["c1"] += 1
                    P.act(r[:, 0:s1 - s0], pt[:, 0:s1 - s0], AF.Relu, reads=[pb], writes=[rb_])
                    P.stt(acc[:, s0:s1], r[:, 0:s1 - s0], witm[:, j, hi:hi + 1], acc[:, s0:s1], ALU.mult, ALU.add,
                          reads=[rb_, resb, accb], writes=[accb])
            if j >= 2:
                P.copy("dve", work[:, 0:Wj], acc[:, 0:Wj], reads=[accb], writes=[workb])
                for r_ in range(32):
                    P.op("dve", lambda e: e.max(out=m8[:], in_=work[:, 0:Wj]), reads=[workb], writes=[workb])
                    if r_ < 31:
                        P.op("dve", lambda e: e.match_replace(out=work[:, 0:Wj], in_to_replace=m8[:],
                                                               in_values=work[:, 0:Wj], imm_value=NEG),
                             reads=[workb], writes=[workb])
                P.ts("dve", thr[:], m8[:, 7:8], -1.0e29, ALU.max, reads=[workb], writes=[workb])
            else:
                P.memset("dve", thr[:], -1.0e29, writes=[workb])
            P.ts("dve", sel[:, 0:Wj], acc[:, 0:Wj], thr[:, 0:1], ALU.is_ge, reads=[accb, workb], writes=[selb])

        def attn_part(j):
            qT, qTb = qTs[j % 2]
            selT, selTb = selTs[j % 2]
            sel, selb = sels[j % 2]
            for kt in range(j + 1):
                pt, pb = P.ps()
                P.tr(pt[:, 0:128], sel[:, kt * 128:(kt + 1) * 128], self.ident_bf[:], reads=[selb, self.cbuf],
                     writes=[pb])
                P.copy("act", selT[:, kt, :], pt[:, 0:128], reads=[pb], writes=[selTb])
            steps = [(g, kt) for g in range(4) for kt in range(j + 1)]
            LA = 2
            live = {}

            def front(i):
                g, kt = steps[i]
                pt, pb = P.ps()
                P.mm(pt[:, :], lhsT=kT[:, kt * 128:(kt + 1) * 128], rhs=qT[:, g * 512:(g + 1) * 512],
                     start=True, stop=True, reads=[resb, qTb], writes=[pb])
                e_, eb = ef[cn["c2"] % 4]
                p_, pb2 = pTs[cn["c2"] % 4]
                cn["c2"] += 1
                P.act(e_[:], pt[:, :], AF.Exp, reads=[pb], writes=[eb], scale=SC)
                P.tt("pool", p_[:, :].rearrange("p (h q) -> p h q", h=4),
                     e_[:, :].rearrange("p (h q) -> p h q", h=4),
                     selT[:, kt, :].unsqueeze(1).broadcast_to([128, 4, 128]), ALU.mult,
                     reads=[eb, selTb], writes=[pb2])
                live[i] = (p_, pb2)

            def back(i):
                g, kt = steps[i]
                p_, pb2 = live.pop(i)
                accO, accOb, accS, accSb = accsets[g % 2]
                P.mm(accO[:, :], lhsT=vtm[:, kt, :], rhs=p_[:, :], start=(kt == 0), stop=(kt == j),
                     reads=[resb, pb2], writes=[accOb], inc=False)
                P.mm(accS[:, :], lhsT=self.ones_bf[:], rhs=p_[:, :], start=(kt == 0), stop=(kt == j),
                     reads=[self.cbuf, pb2], writes=[accSb], inc=True)
                if kt == j:
                    P.op("dve", lambda e: e.reciprocal(out=rsum[:], in_=accS[:, :]), reads=[accSb], writes=[rsb])
                    ao, aob_ = aos[cn["c3"] % 2]
                    cn["c3"] += 1
                    P.tt("dve", ao[:], accO[:, :], rsum[:], ALU.mult, reads=[accOb, rsb], writes=[aob_])
                    P.dma("sp", self.ao_d[g * 512:(g + 1) * 512, j * 128:(j + 1) * 128].rearrange(
                        "(h p) t -> p h t", p=128), ao[:, :].rearrange("p (h q) -> p h q", h=4), reads=[aob_],
                        writes=[aob])

            n = len(steps)
            for i in range(n + LA):
                if i < n:
                    front(i)
                if i - LA >= 0:
                    back(i - LA)

        score_part(0)
        for j in range(16):
            if j + 1 < 16:
                score_part(j + 1)
            attn_part(j)
        for i_ in (iO, iS, iO2, iS2):
            P.ps_unreserve(i_)
        P.release(m)
        self.stage_down(self.ao_d, aob, self.dsa_out, 16)

_CACHE = {}


def col_layout(v, nchunk):
    return np.ascontiguousarray(np.asarray(v).reshape(nchunk, 128).T)


def make_inmaps(inputs, ncores=4):
    g = lambda k: np.asarray(inputs[k])
    hc = host_consts()
    shared = {}
    shared["mod_w"] = np.ascontiguousarray(g("mod_w"), dtype=np.float32)
    shared["mod_b"] = np.stack([col_layout(g("mod_b")[i], 144) for i in range(2)]).astype(np.float32)
    nw = g("norm_w")
    shared["norm_w"] = np.stack(
        [np.concatenate([col_layout(nw[i, s], KC) for s in range(6)], axis=1) for i in range(2)]).astype(np.float32)
    shared["ffn_w_gate"] = np.ascontiguousarray(g("ffn_w_gate"), dtype=np.float32)
    shared["ffn_w_up"] = np.ascontiguousarray(g("ffn_w_up"), dtype=np.float32)
    shared["ffn_w_down"] = np.ascontiguousarray(g("ffn_w_down"), dtype=np.float32)
    shared["hy_w_in"] = np.ascontiguousarray(g("hy_w_in")[0], dtype=np.float32)
    cw = g("hy_conv_w")[0]
    shared["conv_w"] = np.ascontiguousarray(
        cw.reshape(4, 32, 128).transpose(2, 1, 0).reshape(128, 128)).astype(np.float32)
    shared["conv_b"] = col_layout(g("hy_conv_b")[0], 32).astype(np.float32)
    sm = np.zeros((128, 4), np.float32)
    sm[:32, 0] = g("ssd_A_log")[0]
    sm[:32, 1] = g("ssd_dt_bias")[0]
    shared["ssd_small"] = sm
    shared["ssd_Dc"] = col_layout(np.repeat(g("ssd_D")[0], 64), 16).astype(np.float32)
    shared["ssd_nw"] = col_layout(g("ssd_norm_w")[0], 16).astype(np.float32)
    shared["ret_nw"] = col_layout(g("ret_norm_w")[0], 16).astype(np.float32)
    shared["hy_w_out"] = np.ascontiguousarray(g("hy_w_out")[0], dtype=np.float32)
    shared["dsa_w_in"] = np.ascontiguousarray(g("dsa_w_in")[0], dtype=np.float32)
    shared["idx_nw"] = np.concatenate([g("idx_k_norm_w")[0]] * 2).reshape(128, 1).astype(np.float32)
    shared["dsa_w_out"] = np.ascontiguousarray(g("dsa_w_out")[0], dtype=np.float32)
    for k in CONST_SHAPES:
        shared["c_" + k] = hc[k]
    maps = []
    x = g("x")
    c = g("c")
    pos = g("positions")
    for b in range(ncores):
        mp = dict(shared)
        mp["xT"] = np.ascontiguousarray(x[b].T, dtype=np.float32)
        mp["c_col"] = col_layout(c[b], KC).astype(np.float32)
        mp["pos"] = np.ascontiguousarray(pos[b], dtype=np.int32)
        maps.append(mp)
    return maps


def run(inputs, n_sub=6, trace=False, ncores=4):
    key = str(n_sub)
    if key not in _CACHE:
        _CACHE[key] = Builder(n_sub)
    bld = _CACHE[key]
    maps = make_inmaps(inputs, ncores)
    used = set(bld.inp.keys())
    maps = [{k: v for k, v in mp.items() if k in used} for mp in maps]
    res = run_bass_kernel_spmd(bld.nc, maps, core_ids=list(range(ncores)), trace=trace)
    out = np.stack([np.ascontiguousarray(res.results[b]["yT"].T) for b in range(ncores)])
    return out.astype(np.float32), res


def kernel(**inputs):
    out, _ = run(inputs, 6)
    return out
```

```python
import numpy as np
import concourse.bass as bass
import concourse.mybir as mybir
from concourse.bass_utils import run_bass_kernel_spmd

F32 = mybir.dt.float32
BF16 = mybir.dt.bfloat16
I32 = mybir.dt.int32
AF = mybir.ActivationFunctionType
ALU = mybir.AluOpType

D = 2048
T = 2048
DFF = 5632
KC = 16
NEG = -1.0e30
EPS = 1e-6
IN0_OFF = dict(z=0, xbc=2048, dt=6144, rq=6176, rk=8224, rv=10272, rg=12320)
IN1_OFF = dict(q=0, k=2048, v=2176, qi=2304, ki=3328, wi=3392)


class Buf:
    __slots__ = ("w", "r", "const", "excl")

    def __init__(self, const=False, excl=False):
        self.w = None
        self.r = []
        self.const = const
        self.excl = excl


class Prog:
    def __init__(self, nc):
        self.nc = nc
        self.eng = {"pe": nc.tensor, "dve": nc.vector, "act": nc.scalar, "pool": nc.gpsimd, "sp": nc.sync}
        self.sem = {k: nc.alloc_semaphore("s_" + k) for k in self.eng}
        self.cnt = {k: 0 for k in self.eng}
        self.pend = {k: 0 for k in self.eng}
        self.seen = {k: {} for k in self.eng}
        self.nslots = 10
        self.slots = {}
        self.rr = {}
        for q in ("sp", "pool", "act"):
            self.rr[q] = 0
            for i in range(self.nslots):
                key = "d_%s%d" % (q, i)
                self.slots[key] = [nc.alloc_semaphore(key), 0]
        self.sb_off = 16384
        self.sb_max = 16384 + 212000
        self.uid = 0
        self.psum = []
        for i in range(8):
            self.psum.append((nc.alloc_psum_tensor("psb%d" % i, [128, 512], F32), Buf(excl=True)))
        self.prr = 0
        self.reserved = set()
        self.log = {k: [] for k in self.eng}

    def sb(self, shape, dtype, name=None):
        self.uid += 1
        esz = 2 if dtype == BF16 else 4
        n = 1
        for s in shape[1:]:
            n *= s
        nbytes = (n * esz + 63) // 64 * 64
        off = self.sb_off
        assert off + nbytes <= self.sb_max, ("SBUF overflow", name, off, nbytes)
        self.sb_off += nbytes
        t = self.nc.alloc_sbuf_tensor_at("%s_%d" % (name or "t", self.uid), list(shape), dtype, offset=off)
        return t

    def mark(self):
        return self.sb_off

    def release(self, m):
        self.barrier()
        self.sb_off = m

    def ps(self):
        while True:
            i = self.prr % 8
            self.prr += 1
            if i not in self.reserved:
                return self.psum[i]

    def ps_reserve(self):
        for i in range(7, -1, -1):
            if i not in self.reserved:
                self.reserved.add(i)
                return i, self.psum[i]
        raise RuntimeError("no psum")

    def ps_unreserve(self, i):
        self.reserved.discard(i)

    def _semof(self, key):
        if key in self.sem:
            return self.sem[key]
        return self.slots[key][0]

    def _wait(self, e, deps):
        for o, v in deps.items():
            if self.seen[e].get(o, 0) < v:
                self.eng[e].wait_ge(self._semof(o), v)
                self.seen[e][o] = v
                self.log[e].append(("w", o, v))

    def _collect(self, e, reads, writes):
        deps = {}

        def add(x):
            if x is None:
                return
            o, v = x
            if o == e and e == "pe":
                return
            if deps.get(o, 0) < v:
                deps[o] = v

        for b in reads:
            add(b.w)
            if b.excl:
                for r in b.r:
                    if r[0] != e:
                        add(r)
        for b in writes:
            add(b.w)
            for r in b.r:
                if r[0] != e:
                    add(r)
        return deps

    def op(self, e, fn, reads=(), writes=(), inc=True):
        deps = self._collect(e, reads, writes)
        self._wait(e, deps)
        ins = fn(self.eng[e])
        val = self.cnt[e] + 1
        if inc:
            ins.then_inc(self.sem[e], 1)
            self.cnt[e] = val
            self.pend[e] = 0
            self.log[e].append(("i", e, 1))
        else:
            self.pend[e] += 1
        for b in reads:
            if not b.const:
                b.r.append((e, val))
        for b in writes:
            b.w = (e, val)
            b.r = []
        return ins

    def dma(self, q, out, in_, reads=(), writes=()):
        key = "d_%s%d" % (q, self.rr[q] % self.nslots)
        self.rr[q] += 1
        slot = self.slots[key]
        deps = self._collect(key, reads, writes)
        if slot[1] > 0:
            deps[key] = max(deps.get(key, 0), slot[1])
        self._wait(q, deps)
        slot[1] += 16
        self.eng[q].dma_start(out=out, in_=in_).then_inc(slot[0], 16)
        self.log[q].append(("i", key, 16))
        for b in reads:
            if not b.const:
                b.r.append((key, slot[1]))
        for b in writes:
            b.w = (key, slot[1])
            b.r = []

    def barrier(self):
        for e in self.eng:
            assert self.pend[e] == 0, ("pending non-inc ops on", e)
        tgt = dict(self.cnt)
        for k, s in self.slots.items():
            if s[1] > 0:
                tgt[k] = s[1]
        for e in self.eng:
            deps = {o: v for o, v in tgt.items() if o != e and v > 0}
            self._wait(e, deps)

    def mm(self, out, lhsT, rhs, start, stop, reads=(), writes=(), inc=None):
        if inc is None:
            inc = stop
        return self.op("pe", lambda e: e.matmul(out, lhsT=lhsT, rhs=rhs, start=start, stop=stop),
                       reads, writes, inc=inc)

    def tr(self, out, in_, ident, reads=(), writes=(), inc=True):
        return self.op("pe", lambda e: e.matmul(out, lhsT=in_, rhs=ident, start=True, stop=True), reads, writes,
                       inc=inc)

    def act(self, out, in_, func, reads=(), writes=(), bias=None, scale=None, e="act"):
        kw = {}
        if bias is not None:
            kw["bias"] = bias
        if scale is not None:
            kw["scale"] = scale
        return self.op("act", lambda en: en.activation(out=out, in_=in_, func=func, **kw), reads, writes)

    def tt(self, e, out, in0, in1, op, reads=(), writes=()):
        return self.op(e, lambda en: en.tensor_tensor(out=out, in0=in0, in1=in1, op=op), reads, writes)

    def ts(self, e, out, in0, s1, op0, s2=None, op1=None, reads=(), writes=()):
        if op1 is None:
            return self.op(e, lambda en: en.tensor_scalar(out=out, in0=in0, scalar1=s1, scalar2=None, op0=op0),
                           reads, writes)
        return self.op(e, lambda en: en.tensor_scalar(out=out, in0=in0, scalar1=s1, scalar2=s2, op0=op0, op1=op1),
                       reads, writes)

    def stt(self, out, in0, scalar, in1, op0, op1, reads=(), writes=()):
        return self.op("dve", lambda en: en.scalar_tensor_tensor(out=out, in0=in0, scalar=scalar, in1=in1,
                                                                  op0=op0, op1=op1), reads, writes)

    def copy(self, e, out, in_, reads=(), writes=()):
        if e == "act":
            return self.op("act", lambda en: en.activation(out=out, in_=in_, func=AF.Copy), reads, writes)
        return self.op(e, lambda en: en.tensor_copy(out=out, in_=in_), reads, writes)

    def memset(self, e, ap, val, writes=()):
        return self.op(e, lambda en: en.memset(ap, val), (), writes)


def host_consts():
    c = {}
    c["ident"] = np.eye(128, dtype=np.float32)
    c["ones"] = np.ones((128, 128), np.float32)
    bo = np.zeros((128, 128), np.float32)
    bo[:64, :64] = 1
    bo[64:, 64:] = 1
    c["bones"] = bo
    s = np.arange(128)[:, None]
    l = np.arange(128)[None, :]
    c["triT"] = (l >= s).astype(np.float32)
    sel = np.zeros((32, 32, 128), np.float32)
    for h in range(32):
        sel[h, h, :] = 1
    c["selh"] = np.concatenate([sel.reshape(32, 32 * 128), np.zeros((96, 4096), np.float32)])
    ex = np.zeros((32, 32, 64), np.float32)
    for h in range(32):
        ex[h, h, :] = 1
    c["expand"] = np.concatenate([ex.reshape(32, 2048), np.zeros((96, 2048), np.float32)])
    lg = np.log(1.0 - 2.0 ** (-5.0 - np.arange(8, dtype=np.float64)))
    sc = 256.0 ** -0.5
    dm = np.zeros((128, 8, 128), np.float64)
    qd = np.zeros((128, 8, 128), np.float64)
    kd = np.zeros((128, 8), np.float64)
    for h in range(8):
        dm[:, h, :] = np.where(l >= s, np.exp((l - s) * lg[h]), 0.0) * sc
        qd[:, h, :] = np.exp((l + 1) * lg[h])
        kd[:, h] = np.exp((127 - np.arange(128)) * lg[h]) * sc
    c["ret_dm"] = dm.reshape(128, 1024).astype(np.float32)
    c["ret_qd"] = qd.reshape(128, 1024).astype(np.float32)
    c["ret_kd"] = kd.astype(np.float32)
    c["_ret_cd"] = [float(np.exp(128 * lg[h])) for h in range(8)]
    inv = np.zeros((128, 4), np.float32)
    inv[:, 0] = (10000.0 ** (-np.arange(128, dtype=np.float32) / 128)).astype(np.float32)
    th = 500000.0
    for p in range(128):
        if p < 32:
            inv[p, 1] = np.float32(th) ** np.float32(-(p % 16) / 16.0)
        if p % 64 < 16:
            inv[p, 2] = np.float32(th) ** np.float32(-((p % 64) % 8) / 8.0)
    c["inv"] = inv
    pq = np.zeros((128, 128), np.float32)
    for m in range(16):
        pq[m + 16, m] = -1.0
        pq[m, m + 16] = 1.0
    c["pm_q"] = pq
    pi = np.zeros((128, 128), np.float32)
    for b in (0, 64):
        for m in range(8):
            pi[b + m + 8, b + m] = -1.0
            pi[b + m, b + m + 8] = 1.0
    c["pm_i"] = pi
    c["cbias"] = np.where(l <= s, 0.0, NEG).astype(np.float32)
    return c


CONST_SHAPES = dict(ident=(128, 128), ones=(128, 128), bones=(128, 128), triT=(128, 128), selh=(128, 4096),
                    expand=(128, 2048), ret_dm=(128, 1024), ret_qd=(128, 1024), ret_kd=(128, 8), inv=(128, 4),
                    pm_q=(128, 128), pm_i=(128, 128), cbias=(128, 128))


class Builder:
    def __init__(self, n_sub=6):
        self.n_sub = n_sub
        self.overlap_mods = (n_sub == 6)
        nc = bass.Bass("TRN2", target_bir_lowering=False)
        self.nc = nc
        self.P = Prog(nc)
        self.inp = {}
        self.build()

    def din(self, name, shape, dtype=F32):
        t = self.nc.dram_tensor(name, list(shape), dtype, kind="ExternalInput").ap()
        self.inp[name] = t
        return t

    def dscr(self, name, shape, dtype):
        return self.nc.dram_tensor(name, list(shape), dtype, kind="Internal").ap()

    def build(self):
        nc, P = self.nc, self.P
        self.xin = self.din("xT", [D, T])
        self.cin = self.din("c_col", [128, KC])
        self.posin = self.din("pos", [T], I32)
        self.mod_w = self.din("mod_w", [2, D, 9 * D])
        self.mod_b = self.din("mod_b", [2, 128, 144])
        self.norm_w = self.din("norm_w", [2, 128, 6 * KC])
        self.wg = self.din("ffn_w_gate", [2, 2, D, DFF])
        self.wu = self.din("ffn_w_up", [2, 2, D, DFF])
        self.wd = self.din("ffn_w_down", [2, 2, DFF, D])
        self.hy_in = self.din("hy_w_in", [D, 14368])
        self.conv_w = self.din("conv_w", [128, 32 * 4])
        self.conv_b = self.din("conv_b", [128, 32])
        self.ssd_small = self.din("ssd_small", [128, 4])
        self.ssd_Dc = self.din("ssd_Dc", [128, 16])
        self.ssd_nw = self.din("ssd_nw", [128, 16])
        self.ret_nw = self.din("ret_nw", [128, 16])
        self.hy_out = self.din("hy_w_out", [4096, D])
        self.dsa_in = self.din("dsa_w_in", [D, 3408])
        self.idx_nw = self.din("idx_nw", [128, 1])
        self.dsa_out = self.din("dsa_w_out", [D, D])
        self.cst_d = {k: self.din("c_" + k, shp) for k, shp in CONST_SHAPES.items()}
        self.yout = nc.dram_tensor("yT", [D, T], F32, kind="ExternalOutput").ap()
        self.xr = self.dscr("xr", [D, T], F32)
        self.xr_buf = Buf()
        self.xin_buf = Buf()
        self.yout_buf = Buf()
        self.hid_d = self.dscr("hid_d", [DFF, T], BF16)
        self.hid_buf = Buf()

        self.cs = {}
        self.cbuf = Buf(const=True)
        for k in ("ident", "ones", "bones", "triT", "inv", "pm_q", "pm_i", "cbias", "ret_kd"):
            shp = CONST_SHAPES[k]
            t = P.sb(list(shp), F32, "c_" + k)
            P.dma("sp", t[:], self.cst_d[k], writes=[self.cbuf])
            self.cs[k] = t
        self.eps_t = P.sb([128, 1], F32, "eps")
        P.memset("dve", self.eps_t[:], EPS, writes=[self.cbuf])
        self.ident_bf = P.sb([128, 128], BF16, "identbf")
        self.ones_bf = P.sb([128, 128], BF16, "onesbf")
        P.copy("dve", self.ident_bf[:], self.cs["ident"][:], reads=[self.cbuf], writes=[self.cbuf])
        P.copy("dve", self.ones_bf[:], self.cs["ones"][:], reads=[self.cbuf], writes=[self.cbuf])
        self.normw_sb = P.sb([128, 2, 6 * KC], F32, "normw")
        for i in range(2):
            P.dma("sp", self.normw_sb[:, i, :], self.norm_w[i], writes=[self.cbuf])
        self.mods_sb = P.sb([128, 2, 144], F32, "mods")
        self.mods_buf = Buf()
        self.cols = P.sb([128, 3, KC], F32, "cols")
        self.cols_buf = Buf()
        P.barrier()

        self.stage_mods()

        subs = [
            (0, 0, "ffn", 0), (0, 1, "hy", None), (0, 2, "ffn", 1),
            (1, 0, "ffn", 0), (1, 1, "dsa", None), (1, 2, "ffn", 1),
        ][: self.n_sub] if isinstance(self.n_sub, int) else list(self.n_sub)
        for si, (layer, j, kind, f) in enumerate(subs):
            src, sbuf_ = (self.xin, self.xin_buf) if si == 0 else (self.xr, self.xr_buf)
            last = si == len(subs) - 1
            dst, dbuf = (self.yout, self.yout_buf) if last else (self.xr, self.xr_buf)
            self.cur = dict(layer=layer, j=j, src=src, sbuf=sbuf_, dst=dst, dbuf=dbuf,
                            rw=0.5 if kind == "ffn" else 1.0)
            self.make_cols(layer, j)
            m = P.mark()
            hT, hbuf = self.stage_pre()
            if kind == "ffn":
                self.stage_ffn_up(hT, hbuf, layer, f)
                P.release(m)
                self.stage_down(self.hid_d, self.hid_buf, self.wd[layer, f], DFF // 128)
            elif kind == "hy":
                self.stage_hy(hT, hbuf, m)
            else:
                self.stage_dsa(hT, hbuf, m)
            P.release(m)
        P.barrier()

    def stage_mods(self):
        P = self.P
        cb = Buf()
        self.mods_cb = cb
        c_sb = P.sb([128, KC], F32, "c_sb")
        self.cond = P.sb([128, KC], F32, "cond")
        self.mb = P.sb([128, 2, 144], F32, "mb")
        P.dma("sp", c_sb[:], self.cin, writes=[cb])
        for i in range(2):
            P.dma("sp", self.mb[:, i, :], self.mod_b[i], writes=[cb])
        P.act(self.cond[:], c_sb[:], AF.Silu, reads=[cb], writes=[cb])
        m = P.mark()
        layers = [0] if self.overlap_mods else [0, 1]
        for layer in layers:
            for _ in self.mods_gen(layer, None):
                pass
        P.release(m)

    def mods_gen(self, layer, bank):
        P = self.P
        cb = self.mods_cb
        NB = 512 if bank is None else 128
        wt = [(P.sb([128, KC, NB], F32, "modw%d_%d" % (layer, i)), Buf()) for i in range(2)]
        for blk in range(9 * D // NB):
            w, wb = wt[blk % 2]
            P.dma("sp", w[:, :, :],
                  self.mod_w[layer, :, blk * NB:(blk + 1) * NB].rearrange("(k p) n -> p k n", p=128), writes=[wb])
            yield
            pt, pb = P.ps() if bank is None else bank
            for cc in range(NB // 128):
                for k in range(KC):
                    P.mm(pt[:, cc:cc + 1], lhsT=w[:, k, cc * 128:(cc + 1) * 128], rhs=self.cond[:, k:k + 1],
                         start=(k == 0), stop=(k == KC - 1), reads=[wb, cb], writes=[pb],
                         inc=(k == KC - 1))
                if bank is not None:
                    yield
            c0 = blk * (NB // 128)
            P.tt("dve", self.mods_sb[:, layer, c0:c0 + NB // 128], pt[:, 0:NB // 128],
                 self.mb[:, layer, c0:c0 + NB // 128], ALU.add, reads=[pb, cb], writes=[self.mods_buf])
            yield

    def make_cols(self, layer, j):
        P = self.P
        ms = self.mods_sb
        nw = self.normw_sb
        shift = ms[:, layer, (3 * j) * KC:(3 * j + 1) * KC]
        scale = ms[:, layer, (3 * j + 1) * KC:(3 * j + 2) * KC]
        gate = ms[:, layer, (3 * j + 2) * KC:(3 * j + 3) * KC]
        pre_w = nw[:, layer, (2 * j) * KC:(2 * j + 1) * KC]
        post_w = nw[:, layer, (2 * j + 1) * KC:(2 * j + 2) * KC]
        rd = [self.mods_buf, self.cbuf]
        P.stt(self.cols[:, 0, :], scale, 1.0, pre_w, ALU.add, ALU.mult, reads=rd, writes=[self.cols_buf])
        P.copy("dve", self.cols[:, 1, :], shift, reads=rd, writes=[self.cols_buf])
        P.stt(self.cols[:, 2, :], gate, self.cur["rw"], post_w, ALU.mult, ALU.mult, reads=rd, writes=[self.cols_buf])

    def rstd_from_ss(self, ss_ps, ss_buf, n, width, out_t, out_buf):
        P = self.P
        P.act(out_t, ss_ps, AF.Sqrt, reads=[ss_buf], writes=[out_buf], bias=self.eps_t[:, 0:1], scale=1.0 / n)
        P.op("dve", lambda en: en.reciprocal(out=out_t, in_=out_t), reads=[out_buf], writes=[out_buf])

    def stage_pre(self):
        P = self.P
        cur = self.cur
        hT = P.sb([128, KC, T], BF16, "hT")
        hbuf = [Buf() for _ in range(T // 512)]
        m = P.mark()
        xt = [(P.sb([128, KC, 512], F32, "xt%d" % i), Buf()) for i in range(2)]
        sq = [(P.sb([128, 4, 512], F32, "sq%d" % i), Buf()) for i in range(2)]
        rs = [(P.sb([128, 512], F32, "rs%d" % i), Buf()) for i in range(2)]
        tmp = [(P.sb([128, 4, 512], F32, "tmp%d" % i), Buf()) for i in range(2)]
        srcv = cur["src"].rearrange("(k p) t -> p k t", p=128)
        it = 0
        for tt in range(T // 512):
            x, xb = xt[tt % 2]
            P.dma("sp", x[:, :, :], srcv[:, :, tt * 512:(tt + 1) * 512], reads=[cur["sbuf"]], writes=[xb])
            pt, pb = P.ps()
            for k4 in range(KC // 4):
                s_, sbf = sq[k4 % 2]
                P.act(s_[:, :, :], x[:, k4 * 4:(k4 + 1) * 4, :], AF.Square, reads=[xb], writes=[sbf])
                for kk in range(4):
                    k = k4 * 4 + kk
                    P.mm(pt[:, :], lhsT=self.cs["ones"][:], rhs=s_[:, kk, :], start=(k == 0), stop=(k == KC - 1),
                         reads=[sbf, self.cbuf], writes=[pb], inc=(kk == 3))
            r, rb = rs[tt % 2]
            self.rstd_from_ss(pt[:, :], pb, D, 512, r[:], rb)
            for k4 in range(KC // 4):
                tm, tb = tmp[it % 2]
                it += 1
                P.tt("dve" if k4 % 2 == 0 else "pool", tm[:, :, :], x[:, k4 * 4:(k4 + 1) * 4, :],
                     r[:, :].unsqueeze(1).broadcast_to([128, 4, 512]), ALU.mult, reads=[xb, rb], writes=[tb])
                for kk in range(4):
                    k = k4 * 4 + kk
                    P.act(hT[:, k, tt * 512:(tt + 1) * 512], tm[:, kk, :], AF.Identity, reads=[tb, self.cols_buf],
                          writes=[hbuf[tt]], bias=self.cols[:, 1, k:k + 1], scale=self.cols[:, 0, k:k + 1])
        P.release(m)
        return hT, hbuf

    def wload(self, w_t, w_buf, src_ap):
        self.P.dma("pool", w_t, src_ap, writes=[w_buf])

    def stage_ffn_up(self, hT, hbuf, layer, f):
        P = self.P
        CB = 256
        wgt = [(P.sb([128, KC, CB], BF16, "wg%d" % i), Buf()) for i in range(2)]
        wut = [(P.sb([128, KC, CB], BF16, "wu%d" % i), Buf()) for i in range(2)]
        ho = [(P.sb([128, T], BF16, "ho%d" % i), Buf()) for i in range(2)]
        sg = [(P.sb([128, 512], F32, "sg%d" % i), Buf()) for i in range(2)]
        Wg = self.wg[layer, f]
        Wu = self.wu[layer, f]
        nblk = DFF // CB
        it = 0
        for blk in range(nblk):
            g, gb = wgt[blk % 2]
            u, ub = wut[blk % 2]
            self.wload(g[:, :, :], gb, Wg[:, blk * CB:(blk + 1) * CB].rearrange("(k p) n -> p k n", p=128))
            self.wload(u[:, :, :], ub, Wu[:, blk * CB:(blk + 1) * CB].rearrange("(k p) n -> p k n", p=128))
            for cc in range(CB // 128):
                fc = blk * (CB // 128) + cc
                h_o, hob = ho[fc % 2]
                for tt in range(T // 512):
                    pg, pgb = P.ps()
                    pu, pub = P.ps()
                    for k in range(KC):
                        P.mm(pg[:, :], lhsT=g[:, k, cc * 128:(cc + 1) * 128], rhs=hT[:, k, tt * 512:(tt + 1) * 512],
                             start=(k == 0), stop=(k == KC - 1), reads=[gb, hbuf[tt]], writes=[pgb])
                    for k in range(KC):
                        P.mm(pu[:, :], lhsT=u[:, k, cc * 128:(cc + 1) * 128], rhs=hT[:, k, tt * 512:(tt + 1) * 512],
                             start=(k == 0), stop=(k == KC - 1), reads=[ub, hbuf[tt]], writes=[pub])
                    s, sb_ = sg[it % 2]
                    it += 1
                    P.act(s[:], pg[:, :], AF.Silu, reads=[pgb], writes=[sb_])
                    P.tt("dve", h_o[:, tt * 512:(tt + 1) * 512], s[:], pu[:, :], ALU.mult, reads=[sb_, pub],
                         writes=[hob])
                P.dma("sp", self.hid_d[fc * 128:(fc + 1) * 128, :], h_o[:, :], reads=[hob], writes=[self.hid_buf])

    def stage_down(self, act_d, act_buf, W, nkc):
        P = self.P
        cur = self.cur
        NT = 1024
        a_t = P.sb([128, nkc, NT], BF16, "a_t")
        a_bufs = [Buf() for _ in range(nkc)]
        wt = [(P.sb([128, nkc, 128], BF16, "wd%d" % i), Buf()) for i in range(2)]
        y = P.sb([128, KC, NT], F32, "ydown")
        ybufs = [Buf() for _ in range(KC)]
        sq = [(P.sb([128, 512], F32, "sqd%d" % i), Buf()) for i in range(2)]
        rs = P.sb([128, NT], F32, "rsd")
        rsb = Buf()
        xc = [(P.sb([128, NT], F32, "xc%d" % i), Buf()) for i in range(2)]
        actv = act_d.rearrange("(k p) t -> p k t", p=128)
        srcv = cur["src"].rearrange("(k p) t -> p k t", p=128)
        dstv = cur["dst"].rearrange("(k p) t -> p k t", p=128)
        res = [P.ps_reserve() for _ in range(NT // 512)]
        pss = [r_[1] for r_ in res]
        it = 0
        ntiles = T // NT

        def load_a(nt):
            t0 = nt * NT
            for k in range(nkc):
                P.dma("sp", a_t[:, k, :], actv[:, k, t0:t0 + NT], reads=[act_buf], writes=[a_bufs[k]])

        load_a(0)
        for nt in range(ntiles):
            t0 = nt * NT
            for dc in range(KC):
                w, wb = wt[it % 2]
                it += 1
                self.wload(w[:, :, :], wb, W[:, dc * 128:(dc + 1) * 128].rearrange("(k p) n -> p k n", p=128))
                for hh in range(NT // 512):
                    pt, pb = P.ps()
                    for k in range(nkc):
                        P.mm(pt[:, :], lhsT=w[:, k, :], rhs=a_t[:, k, hh * 512:(hh + 1) * 512],
                             start=(k == 0), stop=(k == nkc - 1), reads=[wb, a_bufs[k]], writes=[pb])
                    P.copy("act" if hh == 0 else "dve", y[:, dc, hh * 512:(hh + 1) * 512], pt[:, :], reads=[pb],
                           writes=[ybufs[dc]])
                if dc > 0:
                    for hh in range(NT // 512):
                        s_, sbf = sq[hh % 2]
                        P.act(s_[:], y[:, dc - 1, hh * 512:(hh + 1) * 512], AF.Square, reads=[ybufs[dc - 1]],
                              writes=[sbf])
                        P.mm(pss[hh][0][:, :], lhsT=self.cs["ones"][:], rhs=s_[:], start=(dc == 1), stop=False,
                             reads=[sbf, self.cbuf], writes=[pss[hh][1]], inc=True)
            for hh in range(NT // 512):
                s_, sbf = sq[hh % 2]
                P.act(s_[:], y[:, KC - 1, hh * 512:(hh + 1) * 512], AF.Square, reads=[ybufs[KC - 1]], writes=[sbf])
                P.mm(pss[hh][0][:, :], lhsT=self.cs["ones"][:], rhs=s_[:], start=False, stop=True,
                     reads=[sbf, self.cbuf], writes=[pss[hh][1]], inc=True)
            if nt + 1 < ntiles:
                load_a(nt + 1)
            for hh in range(NT // 512):
                self.rstd_from_ss(pss[hh][0][:, :], pss[hh][1], D, 512, rs[:, hh * 512:(hh + 1) * 512], rsb)
            for k in range(KC):
                x, xb = xc[k % 2]
                P.dma("sp", x[:], srcv[:, k, t0:t0 + NT], reads=[cur["sbuf"]], writes=[xb])
                P.tt("pool" if k % 2 else "dve", y[:, k, :], y[:, k, :], rs[:], ALU.mult, reads=[rsb],
                     writes=[ybufs[k]])
                P.stt(x[:], y[:, k, :], self.cols[:, 2, k:k + 1], x[:], ALU.mult, ALU.add,
                      reads=[ybufs[k], xb, self.cols_buf], writes=[xb])
                P.dma("act", dstv[:, k, t0:t0 + NT], x[:], reads=[xb], writes=[cur["dbuf"]])
        for r_ in res:
            P.ps_unreserve(r_[0])

    def stage_hy(self, hT, hbuf, m):
        P = self.P
        W = self.hy_in
        if not hasattr(self, "zs_d"):
            self.zs_d = self.dscr("zs_d", [D, T], F32)
            self.xs_d = self.dscr("xs_d", [D, T], BF16)
            self.B_d = self.dscr("B_d", [1024, T], BF16)
            self.C_d = self.dscr("C_d", [1024, T], BF16)
            self.rq_d = self.dscr("rq_d", [D, T], BF16)
            self.rk_d = self.dscr("rk_d", [D, T], BF16)
            self.rv_d = self.dscr("rv_d", [T, D], BF16)
            self.rg_d = self.dscr("rg_d", [D, T], F32)
            self.dt_d = self.dscr("dt_d", [128, T], F32)
            self.cum_d = self.dscr("cum_d", [128, T], F32)
            self.ym_d = self.dscr("ym_d", [4096, T], BF16)
        zsb, xsb, Bb, Cb, rqb, rkb, rvb, rgb, dtb, cumb, ymb = [Buf() for _ in range(11)]
        self.pw = [(P.sb([128, KC, 128], BF16, "pw%d" % i), Buf()) for i in range(2)]
        self.pw_i = 0
        rowf = [(P.sb([128, T], F32, "rowf%d" % i), Buf()) for i in range(2)]
        rowb = [(P.sb([128, T], BF16, "rowb%d" % i), Buf()) for i in range(2)]
        smallb = Buf()
        small = P.sb([128, 4], F32, "ssdsmall")
        P.dma("sp", small[:], self.ssd_small, writes=[smallb])
        mA = P.mark()
        cw = P.sb([128, 32 * 4], F32, "cw")
        cb_ = P.sb([128, 32], F32, "cb")
        P.dma("sp", cw[:], self.conv_w, writes=[smallb])
        P.dma("sp", cb_[:], self.conv_b, writes=[smallb])
        ci = [0]

        def silu_to(dst_d, dbuf, row0):
            rt, rb = rowf[ci[0] % 2]
            ci[0] += 1

            def h(tt, pt, pb):
                P.act(rt[:, tt * 512:(tt + 1) * 512], pt[:, :], AF.Silu, reads=[pb], writes=[rb])
            return h, (lambda: P.dma("sp", dst_d[row0:row0 + 128, :], rt[:, :], reads=[rb], writes=[dbuf]))

        for c in range(16):
            h, fin = silu_to(self.zs_d, zsb, c * 128)
            self.proj_fm(hT, hbuf, W, IN0_OFF["z"] + c * 128, 128, h)
            fin()
        for c in range(16):
            h, fin = silu_to(self.rg_d, rgb, c * 128)
            self.proj_fm(hT, hbuf, W, IN0_OFF["rg"] + c * 128, 128, h)
            fin()
        us = [(P.sb([128, T + 4], F32, "u%d" % i), Buf()) for i in range(2)]
        for u, ub in us:
            P.memset("pool", u[:, 0:3], 0.0, writes=[ub])
        cacc = P.sb([128, T], F32, "cacc")
        caccb = Buf()
        for c in range(32):
            u, ub = us[c % 2]
            rt, rb = rowb[c % 2]

            def hx(tt, pt, pb, u=u, ub=ub):
                P.copy("act", u[:, 3 + tt * 512:3 + (tt + 1) * 512], pt[:, :], reads=[pb], writes=[ub])
            self.proj_fm(hT, hbuf, W, IN0_OFF["xbc"] + c * 128, 128, hx)
            P.ts("dve", cacc[:], u[:, 0:T], cw[:, c * 4:c * 4 + 1], ALU.mult, cb_[:, c:c + 1], ALU.add,
                 reads=[ub, smallb], writes=[caccb])
            for i in range(1, 4):
                P.stt(cacc[:], u[:, i:i + T], cw[:, c * 4 + i:c * 4 + i + 1], cacc[:], ALU.mult, ALU.add,
                      reads=[ub, smallb, caccb], writes=[caccb])
            P.act(rt[:], cacc[:], AF.Silu, reads=[caccb], writes=[rb])
            if c < 16:
                P.dma("sp", self.xs_d[c * 128:(c + 1) * 128, :], rt[:, :], reads=[rb], writes=[xsb])
            elif c < 24:
                P.dma("sp", self.B_d[(c - 16) * 128:(c - 15) * 128, :], rt[:, :], reads=[rb], writes=[Bb])
            else:
                P.dma("sp", self.C_d[(c - 24) * 128:(c - 23) * 128, :], rt[:, :], reads=[rb], writes=[Cb])
        dtT, dtTb = rowf[0]
        cumT, cumTb = rowf[1]
        acol = P.sb([128, 1], F32, "acol")
        P.act(acol[:], small[:, 0:1], AF.Exp, reads=[smallb], writes=[smallb])
        P.ts("dve", acol[:], acol[:], -1.0, ALU.mult, reads=[smallb], writes=[smallb])

        def hdt(tt, pt, pb):
            sl = slice(tt * 512, (tt + 1) * 512)
            P.act(dtT[:, sl], pt[:, :], AF.Exp, reads=[pb, smallb], writes=[dtTb], bias=small[:, 1:2], scale=1.0)
            P.act(dtT[:, sl], dtT[:, sl], AF.Ln, reads=[dtTb, self.cbuf], writes=[dtTb],
                  bias=self.cs["ones"][:, 0:1], scale=1.0)
        self.proj_fm(hT, hbuf, W, IN0_OFF["dt"], 32, hdt)
        P.ts("dve", cacc[:], dtT[:], acol[:, 0:1], ALU.mult, reads=[dtTb, smallb], writes=[caccb])
        for c in range(16):
            sl = slice(c * 128, (c + 1) * 128)
            P.op("dve", lambda e, sl=sl: e.tensor_tensor_scan(out=cumT[:, sl], data0=self.cs["ones"][:, 0:128],
                                                             data1=cacc[:, sl], initial=0.0, op0=ALU.mult,
                                                             op1=ALU.add),
                 reads=[caccb, self.cbuf], writes=[cumTb])
        P.dma("sp", self.dt_d, dtT[:, :], reads=[dtTb], writes=[dtb])
        P.dma("sp", self.cum_d, cumT[:, :], reads=[cumTb], writes=[cumb])
        P.release(mA)
        cosr = P.sb([128, T], F32, "cosr")
        sinr = P.sb([128, T], F32, "sinr")
        trb = Buf()
        m2 = P.mark()
        pos_f, pb_ = self.load_pos()
        tmp = (P.sb([128, T], F32, "tv"), P.sb([128, T], I32, "tvi"), P.sb([128, T], F32, "tvf"), Buf())
        self.trig(cosr, trb, pos_f, pb_, self.cs["inv"][:, 0:1], 0.25, tmp)
        self.trig(sinr, trb, pos_f, pb_, self.cs["inv"][:, 0:1], 0.0, tmp)
        P.release(m2)
        t1 = P.sb([128, T], F32, "rt1")
        t2 = P.sb([128, T], F32, "rt2")
        t12b = Buf()
        rowf4 = rowf + [(P.sb([128, T], F32, "rowf%d" % i), Buf()) for i in (2, 3)]
        hq = 0
        for name, dst, dbuf in (("rq", self.rq_d, rqb), ("rk", self.rk_d, rkb)):
            for hh in range(8):
                x1, x1b = rowf4[(hq % 2) * 2]
                x2, x2b = rowf4[(hq % 2) * 2 + 1]
                hq += 1

                def h1(tt, pt, pb):
                    P.copy("act", x1[:, tt * 512:(tt + 1) * 512], pt[:, :], reads=[pb], writes=[x1b])

                def h2(tt, pt, pb):
                    P.copy("act", x2[:, tt * 512:(tt + 1) * 512], pt[:, :], reads=[pb], writes=[x2b])
                self.proj_fm(hT, hbuf, W, IN0_OFF[name] + hh * 256, 128, h1)
                self.proj_fm(hT, hbuf, W, IN0_OFF[name] + hh * 256 + 128, 128, h2)
                o1, o1b = rowb[0]
                o2, o2b = rowb[1]
                P.tt("dve", t1[:], x1[:], cosr[:], ALU.mult, reads=[x1b, trb], writes=[t12b])
                P.tt("pool", t2[:], x2[:], sinr[:], ALU.mult, reads=[x2b, trb], writes=[t12b])
                P.tt("dve", o1[:], t1[:], t2[:], ALU.subtract, reads=[t12b], writes=[o1b])
                P.tt("dve", t1[:], x1[:], sinr[:], ALU.mult, reads=[x1b, trb, o1b], writes=[t12b])
                P.tt("pool", t2[:], x2[:], cosr[:], ALU.mult, reads=[x2b, trb, o1b], writes=[t12b])
                P.tt("dve", o2[:], t1[:], t2[:], ALU.add, reads=[t12b], writes=[o2b])
                P.dma("sp", dst[hh * 256:hh * 256 + 128, :], o1[:, :], reads=[o1b], writes=[dbuf])
                P.dma("sp", dst[hh * 256 + 128:hh * 256 + 256, :], o2[:, :], reads=[o2b], writes=[dbuf])
        wv = P.sb([128, KC, 512], BF16, "wv")
        wvb = Buf()
        vo = [(P.sb([128, 512], BF16, "vo%d" % i), Buf()) for i in range(2)]
        for cbk in range(4):
            c0 = IN0_OFF["rv"] + cbk * 512
            self.wload(wv[:, :, :], wvb, W[:, c0:c0 + 512].rearrange("(k p) n -> p k n", p=128))
            for t16 in range(16):
                pt, pb = P.ps()
                for k in range(KC):
                    P.mm(pt[:, :], lhsT=hT[:, k, t16 * 128:(t16 + 1) * 128], rhs=wv[:, k, :], start=(k == 0),
                         stop=(k == KC - 1), reads=[wvb, hbuf[t16 // 4]], writes=[pb])
                v_, vb_ = vo[t16 % 2]
                P.copy("act" if t16 % 2 else "dve", v_[:], pt[:, :], reads=[pb], writes=[vb_])
                P.dma("sp", self.rv_d[t16 * 128:(t16 + 1) * 128, cbk * 512:(cbk + 1) * 512], v_[:, :], reads=[vb_],
                      writes=[rvb])
        P.release(m)

        ld = lambda shp, dt, nm: P.sb(shp, dt, nm)
        selh = ld([128, 32 * 128], F32, "selh")
        expd = ld([128, 2048], F32, "expd")
        Dc = ld([128, 16], F32, "Dc")
        snw = ld([128, 16], F32, "snw")
        cbf = Buf()
        P.dma("sp", selh[:], self.cst_d["selh"], writes=[cbf])
        P.dma("sp", expd[:], self.cst_d["expand"], writes=[cbf])
        P.dma("sp", Dc[:], self.ssd_Dc, writes=[cbf])
        P.dma("sp", snw[:], self.ssd_nw, writes=[cbf])
        S = ld([128, 2048], F32, "S")
        Sbf_e = ld([128, 16, 128], BF16, "Sbfe")
        Sbf_o = ld([128, 16, 128], BF16, "Sbfo")
        xdt_e = ld([128, 16, 128], BF16, "xdte")
        xdt_o = ld([128, 16, 128], BF16, "xdto")
        xdtw = ld([128, 2048], BF16, "xdtw")
        Btm = ld([128, 8, 128], BF16, "Btm")
        Sb, xdb, Btb = Buf(), Buf(), Buf()
        for t_ in (S, Sbf_e, Sbf_o, xdt_e, xdt_o):
            P.memset("pool", t_[:], 0.0, writes=[Sb])
        ach = ld([128, 2048], F32, "ach")
        achb = Buf()
        D2 = ld([128, 2048], F32, "D2")
        yraw = ld([128, 16, 128], F32, "yraw")
        yrb = Buf()
        sqy = ld([128, 2048], F32, "sqy")
        sqb = Buf()
        yab = ld([128, 16, 128], BF16, "yab")
        yabb = Buf()
        sms = [{k: ld([128, 32], F32, k + str(i)) for k in ("dttm", "cumc", "negc", "ds", "w2", "D1")} for i in range(2)]
        smbs = [Buf(), Buf()]
        CBm = [(ld([128, 128], F32, "CBm%d" % i), Buf()) for i in range(3)]
        NH = 4
        hb = [dict(arg=ld([128, 128], F32, "arg%d" % i), E=ld([128, 128], F32, "E%d" % i),
                   G=ld([128, 128], BF16, "G%d" % i), ec=ld([128, 128], F32, "ec%d" % i),
                   Cd=ld([128, 128], BF16, "Cd%d" % i), argb=Buf(), Eb=Buf(), Gb=Buf(), ecb=Buf(), Cdb=Buf())
              for i in range(NH)]
        yraws = [(yraw, yrb), (ld([128, 16, 128], F32, "yraw2"), Buf())]
        rstd = ld([128, 128], F32, "rstdy")
        lds = []
        for i in range(2):
            lds.append(dict(BT=ld([128, 8, 128], BF16, "BT%d" % i),
                            CT=ld([128, 8, 128], BF16, "CT%d" % i),
                            dt=ld([128, 128], F32, "dtc%d" % i), cum=ld([128, 128], F32, "cumc%d" % i), b=Buf()))
        eds = [dict(xsT=ld([128, 16, 128], BF16, "xsT%d" % i), zs=ld([128, 16, 128], F32, "zs%d" % i), b=Buf())
               for i in range(3)]
        xdt_e2 = ld([128, 16, 128], BF16, "xdte2")
        xdt_o2 = ld([128, 16, 128], BF16, "xdto2")
        xdtw2 = ld([128, 2048], BF16, "xdtw2")
        Btm2 = ld([128, 8, 128], BF16, "Btm2")
        ach2 = ld([128, 2048], F32, "ach2")
        xdb2, Btb2, achb2 = Buf(), Buf(), Buf()
        for t_ in (xdt_e2, xdt_o2):
            P.memset("pool", t_[:], 0.0, writes=[xdb2])
        pro = [dict(xe=xdt_e, xo=xdt_o, xw=xdtw, xb=xdb, Bt=Btm, Btb=Btb, ach=ach, achb=achb),
               dict(xe=xdt_e2, xo=xdt_o2, xw=xdtw2, xb=xdb2, Bt=Btm2, Btb=Btb2, ach=ach2, achb=achb2)]
        D2b = Buf()
        iy0, (yp0, yp0b) = P.ps_reserve()
        iy1, (yp1, yp1b) = P.ps_reserve()
        yps = [(yp0, yp0b), (yp1, yp1b)]
        mgen = None
        if self.overlap_mods:
            imods, mods_bank = P.ps_reserve()
            mgen = self.mods_gen(1, mods_bank)
        ident = self.cs["ident"]
        ones = self.cs["ones"]
        cnt = dict(h=0, g=0)

        def prologue(c):
            L = lds[c % 2]
            lb = L["b"]
            sm = sms[c % 2]
            smb = smbs[c % 2]
            Q = pro[c % 2]
            sl = slice(c * 128, (c + 1) * 128)
            E = eds[c % 3]
            eb = E["b"]
            P.dma("sp", E["xsT"][:, :, :], self.xs_d[:, sl].rearrange("(k p) t -> p k t", p=128), reads=[xsb], writes=[eb])
            P.dma("sp", L["BT"][:, :, :], self.B_d[:, sl].rearrange("(k p) t -> p k t", p=128), reads=[Bb, lb], writes=[lb])
            P.dma("sp", L["CT"][:, :, :], self.C_d[:, sl].rearrange("(k p) t -> p k t", p=128), reads=[Cb, lb], writes=[lb])
            P.dma("sp", E["zs"][:, :, :], self.zs_d[:, sl].rearrange("(k p) t -> p k t", p=128), reads=[zsb, eb], writes=[eb])
            P.dma("sp", L["dt"][:, :], self.dt_d[:, sl], reads=[dtb, lb], writes=[lb])
            P.dma("sp", L["cum"][:, :], self.cum_d[:, sl], reads=[cumb, lb], writes=[lb])
            pt, pb = P.ps()
            P.tr(pt[:, 0:128], L["dt"][:, :], ident[:], reads=[lb, self.cbuf], writes=[pb], inc=False)
            P.tr(pt[:, 128:256], L["cum"][:, :], ident[:], reads=[lb, self.cbuf], writes=[pb])
            P.ts("dve", sm["D1"][:], ident[:, 0:32], L["cum"][:, 127:128], ALU.mult, reads=[lb, self.cbuf, smb],
                 writes=[smb])
            pt1, pb1 = P.ps()
            P.mm(pt1[:, 0:32], lhsT=ones[:], rhs=sm["D1"][:], start=True, stop=True, reads=[smb, self.cbuf], writes=[pb1])
            P.copy("dve", sm["dttm"][:], pt[:, 0:32], reads=[pb], writes=[smb])
            P.copy("dve", sm["cumc"][:], pt[:, 128:160], reads=[pb, smb], writes=[smb])
            P.ts("dve", sm["negc"][:], sm["cumc"][:], -1.0, ALU.mult, reads=[smb], writes=[smb])
            P.tt("dve", sm["ds"][:], pt1[:, 0:32], sm["cumc"][:], ALU.subtract, reads=[pb1, smb], writes=[smb])
            P.act(sm["ds"][:], sm["ds"][:], AF.Exp, reads=[smb], writes=[smb])
            P.tt("dve", sm["w2"][:], sm["ds"][:], sm["dttm"][:], ALU.mult, reads=[smb], writes=[smb])
            P.ts("dve", D2[:], expd[:], L["cum"][:, 127:128], ALU.mult, reads=[lb, cbf], writes=[D2b])
            pds = []
            for q4 in range(4):
                ptd, pbd = P.ps()
                P.mm(ptd[:, :], lhsT=ones[:], rhs=D2[:, q4 * 512:(q4 + 1) * 512], start=True, stop=True,
                     reads=[D2b, self.cbuf], writes=[pbd])
                pds.append((ptd, pbd))
            for q4 in range(4):
                ptd, pbd = pds[q4]
                P.act(Q["ach"][:, q4 * 512:(q4 + 1) * 512], ptd[:, :], AF.Exp, reads=[pbd], writes=[Q["achb"]])
            for q4 in range(4):
                ptx, pbx = P.ps()
                for i4 in range(4):
                    P.tr(ptx[:, i4 * 128:(i4 + 1) * 128], E["xsT"][:, q4 * 4 + i4, :], self.ident_bf[:],
                         reads=[eb, self.cbuf], writes=[pbx], inc=(i4 == 3))
                src = ptx[:, 0:512].rearrange("p (a b d) -> p a b d", a=4, b=2)
                dtv = sm["dttm"][:, q4 * 8:(q4 + 1) * 8].rearrange("p (a b) -> p a b", b=2)
                w2v = sm["w2"][:, q4 * 8:(q4 + 1) * 8].rearrange("p (a b) -> p a b", b=2)
                xwv = Q["xw"][:, q4 * 512:(q4 + 1) * 512].rearrange("p (a b d) -> p a b d", a=4, b=2)
                P.tt("dve", Q["xe"][:, q4 * 4:(q4 + 1) * 4, 0:64], src[:, :, 0, :],
                     dtv[:, :, 0].unsqueeze(2).broadcast_to([128, 4, 64]), ALU.mult, reads=[pbx, smb], writes=[Q["xb"]])
                P.tt("dve", Q["xo"][:, q4 * 4:(q4 + 1) * 4, 64:128], src[:, :, 1, :],
                     dtv[:, :, 1].unsqueeze(2).broadcast_to([128, 4, 64]), ALU.mult, reads=[pbx, smb], writes=[Q["xb"]])
                for par in range(2):
                    P.tt("dve", xwv[:, :, par, :], src[:, :, par, :],
                         w2v[:, :, par].unsqueeze(2).broadcast_to([128, 4, 64]), ALU.mult, reads=[pbx, smb],
                         writes=[Q["xb"]])
            for q2 in range(2):
                ptx, pbx = P.ps()
                for i4 in range(4):
                    P.tr(ptx[:, i4 * 128:(i4 + 1) * 128], L["BT"][:, q2 * 4 + i4, :], self.ident_bf[:],
                         reads=[lb, self.cbuf], writes=[pbx], inc=(i4 == 3))
                P.copy("act", Q["Bt"][:, q2 * 4:(q2 + 1) * 4, :], ptx[:, 0:512].rearrange("p (a d) -> p a d", a=4),
                       reads=[pbx], writes=[Q["Btb"]])

        def heads(c, gens=()):
            L = lds[c % 2]
            lb = L["b"]
            sm = sms[c % 2]
            smb = smbs[c % 2]
            Q = pro[c % 2]
            live = {}
            cms = {}

            def front(h):
                g = h // 4
                if h % 4 == 0:
                    cm, cmb = CBm[cnt["g"] % 3]
                    cnt["g"] += 1
                    pt, pb = P.ps()
                    P.mm(pt[:, 0:128], lhsT=L["BT"][:, g, :], rhs=L["CT"][:, g, :], start=True, stop=True, reads=[lb],
                         writes=[pb])
                    P.tt("dve", cm[:], pt[:, 0:128], self.cs["triT"][:], ALU.mult, reads=[pb, self.cbuf], writes=[cmb])
                    cms[g] = (cm, cmb)
                cm, cmb = cms[g]
                H = hb[cnt["h"] % NH]
                cnt["h"] += 1
                pt, pb = P.ps()
                P.mm(pt[:, 0:128], lhsT=selh[:, h * 128:(h + 1) * 128], rhs=L["cum"][:, :], start=True, stop=True,
                     reads=[lb, cbf], writes=[pb])
                P.ts("dve", H["arg"][:], pt[:, 0:128], sm["negc"][:, h:h + 1], ALU.add, 0.0, ALU.min,
                     reads=[pb, smb], writes=[H["argb"]])
                P.act(H["ec"][:], pt[:, 0:128], AF.Exp, reads=[pb, H["argb"]], writes=[H["ecb"]])
                P.act(H["E"][:], H["arg"][:], AF.Exp, reads=[H["argb"]], writes=[H["Eb"]])
                P.tt("pool", H["G"][:], H["E"][:], cm[:], ALU.mult, reads=[H["Eb"], cmb], writes=[H["Gb"]])
                P.tt("dve", H["Cd"][:], L["CT"][:, g, :], H["ec"][:], ALU.mult, reads=[lb, H["ecb"]], writes=[H["Cdb"]])
                live[h] = H

            def back(h):
                g = h // 4
                H = live.pop(h)
                yp, ypb = yps[g % 2]
                xd = Q["xe"] if h % 2 == 0 else Q["xo"]
                sb_ = Sbf_e if h % 2 == 0 else Sbf_o
                col = ((h // 2) % 2) * 128
                P.mm(yp[:, col:col + 128], lhsT=xd[:, h // 2, :], rhs=H["G"][:], start=(h % 2 == 0), stop=False,
                     reads=[Q["xb"], H["Gb"]], writes=[ypb], inc=False)
                P.mm(yp[:, col:col + 128], lhsT=sb_[:, h // 2, :], rhs=H["Cd"][:], start=False, stop=(h % 2 == 1),
                     reads=[Sb, H["Cdb"]], writes=[ypb], inc=True)
                if h % 4 == 3:
                    yr_, yrb_ = yraws[c % 2]
                    P.copy("act", yr_[:, 2 * g:2 * g + 2, :], yp[:, 0:256].rearrange("p (a d) -> p a d", a=2),
                           reads=[ypb], writes=[yrb_])
                for gen in gens:
                    next(gen, None)

            LA = NH - 2
            for i in range(32 + LA):
                if i < 32:
                    front(i)
                if i - LA >= 0:
                    back(i - LA)

        def update(c):
            Q = pro[c % 2]
            pts = []
            for q4 in range(4):
                pt, pb = P.ps()
                for gg in range(2):
                    g = q4 * 2 + gg
                    P.mm(pt[:, gg * 256:(gg + 1) * 256], lhsT=Q["Bt"][:, g, :], rhs=Q["xw"][:, g * 256:(g + 1) * 256],
                         start=True, stop=True, reads=[Q["Btb"], Q["xb"]], writes=[pb], inc=(gg == 1))
                pts.append((pt, pb))
            for q4 in range(4):
                pt, pb = pts[q4]
                sl4 = slice(q4 * 512, (q4 + 1) * 512)
                P.tt("dve", S[:, sl4], S[:, sl4], Q["ach"][:, sl4], ALU.mult, reads=[Q["achb"], Sb], writes=[Sb])
                P.tt("dve", S[:, sl4], S[:, sl4], pt[:, :], ALU.add, reads=[pb, Sb], writes=[Sb])
            Sv = S[:, :].rearrange("p (a b d) -> p a b d", a=16, b=2)
            P.copy("pool", Sbf_e[:, :, 0:64], Sv[:, :, 0, :], reads=[Sb], writes=[Sb])
            P.copy("pool", Sbf_o[:, :, 64:128], Sv[:, :, 1, :], reads=[Sb], writes=[Sb])

        def epilogue(c):
            E = eds[c % 3]
            eb = E["b"]
            yraw, yrb = yraws[c % 2]
            sl = slice(c * 128, (c + 1) * 128)
            for k in range(16):
                P.stt(yraw[:, k, :], E["xsT"][:, k, :], Dc[:, k:k + 1], yraw[:, k, :], ALU.mult, ALU.add,
                      reads=[eb, cbf, yrb], writes=[yrb])
                if k % 4 == 3:
                    yield
            yflat = yraw[:, :, :].rearrange("p a d -> p (a d)")
            P.tt("dve", yflat, yflat, E["zs"][:, :, :].rearrange("p a d -> p (a d)"), ALU.mult, reads=[eb, yrb],
                 writes=[yrb])
            yield
            P.act(sqy[:], yflat, AF.Square, reads=[yrb], writes=[sqb])
            yield
            pt, pb = P.ps()
            for k in range(16):
                P.mm(pt[:, 0:128], lhsT=ones[:], rhs=sqy[:, k * 128:(k + 1) * 128], start=(k == 0), stop=(k == 15),
                     reads=[sqb, self.cbuf], writes=[pb])
            rb_ = Buf()
            self.rstd_from_ss(pt[:, 0:128], pb, 2048, 128, rstd[:], rb_)
            yield
            P.tt("dve", yraw[:, :, :], yraw[:, :, :], rstd[:, :].unsqueeze(1).broadcast_to([128, 16, 128]), ALU.mult,
                 reads=[rb_, yrb], writes=[yrb])
            yield
            for k in range(16):
                P.act(yab[:, k, :], yraw[:, k, :], AF.Copy, reads=[yrb, cbf], writes=[yabb], scale=snw[:, k:k + 1])
                if k % 4 == 3:
                    yield
            P.dma("sp", self.ym_d[0:2048, sl].rearrange("(k p) t -> p k t", p=128), yab[:, :, :], reads=[yabb],
                  writes=[ymb])

        prologue(0)
        prev_ep = None
        for c in range(16):
            if c + 1 < 16:
                prologue(c + 1)
            gl = ([mgen] if mgen is not None else []) + ([prev_ep] if prev_ep is not None else [])
            heads(c, gens=gl)
            if prev_ep is not None:
                for _ in prev_ep:
                    pass
            update(c)
            prev_ep = epilogue(c)
        for _ in prev_ep:
            pass
        if mgen is not None:
            for _ in mgen:
                pass
            P.ps_unreserve(imods)
        P.ps_unreserve(iy0)
        P.ps_unreserve(iy1)
        P.release(m)

        dm = ld([128, 8, 128], F32, "dm")
        qdt = ld([128, 8, 128], F32, "qdt")
        rnw = ld([128, 16], F32, "rnw")
        cbf = Buf()
        P.dma("sp", dm[:, :, :].rearrange("p a d -> p (a d)"), self.cst_d["ret_dm"], writes=[cbf])
        P.dma("sp", qdt[:, :, :].rearrange("p a d -> p (a d)"), self.cst_d["ret_qd"], writes=[cbf])
        P.dma("sp", rnw[:], self.ret_nw, writes=[cbf])
        kd = self.cs["ret_kd"]
        cdv = host_consts()["_ret_cd"]
        R = ld([128, 8, 512], F32, "R")
        Rbf = ld([128, 8, 512], BF16, "Rbf")
        Rb = Buf()
        P.memset("pool", R[:], 0.0, writes=[Rb])
        P.memset("pool", Rbf[:], 0.0, writes=[Rb])
        yraw = ld([128, 16, 128], F32, "yrawr")
        yrb = Buf()
        sqy = ld([128, 2048], F32, "sqyr")
        sqb = Buf()
        yab = ld([128, 16, 128], BF16, "yabr")
        yabb = Buf()
        rstd8 = ld([128, 8, 128], F32, "rstd8")
        hb = [dict(G=ld([128, 128], BF16, "rG%d" % i), qd=ld([128, 2, 128], BF16, "rqd%d" % i),
                   kw=ld([128, 256], BF16, "rkw%d" % i), Gb=Buf(), qdb=Buf(), kwb=Buf()) for i in range(4)]
        cnt = dict(h=0)
        lds = []
        for i in range(3):
            lds.append(dict(qT=ld([128, 16, 128], BF16, "rqT%d" % i), kT=ld([128, 16, 128], BF16, "rkT%d" % i),
                            v=ld([128, 2048], BF16, "rv%d" % i), gs=ld([128, 16, 128], F32, "rgs%d" % i), b=Buf()))
        yraws = [(yraw, yrb), (ld([128, 16, 128], F32, "yrawr2"), Buf())]
        iy0, (yp0, yp0b) = P.ps_reserve()
        iy1, (yp1, yp1b) = P.ps_reserve()
        yps = [(yp0, yp0b), (yp1, yp1b)]

        def loads(c):
            L = lds[c % 3]
            lb = L["b"]
            sl = slice(c * 128, (c + 1) * 128)
            P.dma("sp", L["qT"][:, :, :], self.rq_d[:, sl].rearrange("(k p) t -> p k t", p=128), reads=[rqb], writes=[lb])
            P.dma("sp", L["kT"][:, :, :], self.rk_d[:, sl].rearrange("(k p) t -> p k t", p=128), reads=[rkb, lb], writes=[lb])
            P.dma("sp", L["v"][:, :], self.rv_d[sl, :], reads=[rvb, lb], writes=[lb])
            P.dma("sp", L["gs"][:, :, :], self.rg_d[:, sl].rearrange("(k p) t -> p k t", p=128), reads=[rgb, lb], writes=[lb])

        def rheads(c, gens=()):
            L = lds[c % 3]
            lb = L["b"]
            yraw_, yrb_ = yraws[c % 2]
            live = {}

            def front(h):
                H = hb[cnt["h"] % 4]
                cnt["h"] += 1
                pt, pb = P.ps()
                P.mm(pt[:, 0:128], lhsT=L["kT"][:, 2 * h, :], rhs=L["qT"][:, 2 * h, :], start=True, stop=False,
                     reads=[lb], writes=[pb], inc=False)
                P.mm(pt[:, 0:128], lhsT=L["kT"][:, 2 * h + 1, :], rhs=L["qT"][:, 2 * h + 1, :], start=False, stop=True,
                     reads=[lb], writes=[pb])
                pt2, pb2 = P.ps()
                P.tr(pt2[:, 0:128], L["kT"][:, 2 * h, :], self.ident_bf[:], reads=[lb, self.cbuf], writes=[pb2], inc=False)
                P.tr(pt2[:, 128:256], L["kT"][:, 2 * h + 1, :], self.ident_bf[:], reads=[lb, self.cbuf], writes=[pb2])
                P.tt("dve", H["G"][:], pt[:, 0:128], dm[:, h, :], ALU.mult, reads=[pb, cbf], writes=[H["Gb"]])
                P.tt("pool", H["qd"][:, :, :], L["qT"][:, 2 * h:2 * h + 2, :],
                     qdt[:, h, :].unsqueeze(1).broadcast_to([128, 2, 128]), ALU.mult, reads=[lb, cbf], writes=[H["qdb"]])
                P.ts("dve", H["kw"][:], pt2[:, 0:256], kd[:, h:h + 1], ALU.mult, reads=[pb2, self.cbuf], writes=[H["kwb"]])
                live[h] = H

            def back(h):
                H = live.pop(h)
                yp, ypb = yps[h % 2]
                for vc in range(2):
                    col = vc * 128
                    P.mm(yp[:, col:col + 128], lhsT=L["v"][:, h * 256 + vc * 128:h * 256 + (vc + 1) * 128], rhs=H["G"][:],
                         start=True, stop=False, reads=[lb, H["Gb"]], writes=[ypb], inc=False)
                    P.mm(yp[:, col:col + 128], lhsT=Rbf[:, h, vc * 128:(vc + 1) * 128], rhs=H["qd"][:, 0, :],
                         start=False, stop=False, reads=[Rb, H["qdb"]], writes=[ypb], inc=False)
                    P.mm(yp[:, col:col + 128], lhsT=Rbf[:, h, 256 + vc * 128:256 + (vc + 1) * 128], rhs=H["qd"][:, 1, :],
                         start=False, stop=True, reads=[Rb, H["qdb"]], writes=[ypb], inc=True)
                pt3, pb3 = P.ps()
                for dc in range(2):
                    P.mm(pt3[:, dc * 256:(dc + 1) * 256], lhsT=H["kw"][:, dc * 128:(dc + 1) * 128],
                         rhs=L["v"][:, h * 256:(h + 1) * 256], start=True, stop=True, reads=[lb, H["kwb"]], writes=[pb3],
                         inc=(dc == 1))
                P.copy("act", yraw_[:, 2 * h:2 * h + 2, :], yp[:, 0:256].rearrange("p (a d) -> p a d", a=2),
                       reads=[ypb], writes=[yrb_])
                P.stt(R[:, h, :], R[:, h, :], float(cdv[h]), pt3[:, :], ALU.mult, ALU.add, reads=[pb3, Rb], writes=[Rb])
                P.copy("pool", Rbf[:, h, :], R[:, h, :], reads=[Rb], writes=[Rb])
                for gen in gens:
                    next(gen, None)
                    next(gen, None)

            LA = 2
            for i in range(8 + LA):
                if i < 8:
                    front(i)
                if i - LA >= 0:
                    back(i - LA)

        def repilogue(c):
            L = lds[c % 3]
            lb = L["b"]
            yraw_, yrb_ = yraws[c % 2]
            sl = slice(c * 128, (c + 1) * 128)
            yflat = yraw_[:, :, :].rearrange("p a d -> p (a d)")
            P.act(sqy[:], yflat, AF.Square, reads=[yrb_], writes=[sqb])
            yield
            for q2 in range(2):
                pt, pb = P.ps()
                for hh in range(4):
                    h = q2 * 4 + hh
                    for vc in range(2):
                        P.mm(pt[:, hh * 128:(hh + 1) * 128], lhsT=ones[:],
                             rhs=sqy[:, (2 * h + vc) * 128:(2 * h + vc + 1) * 128], start=(vc == 0), stop=(vc == 1),
                             reads=[sqb, self.cbuf], writes=[pb], inc=(vc == 1 and hh == 3))
                rb_ = Buf()
                self.rstd_from_ss(pt[:, :], pb, 256, 512, rstd8[:, q2 * 4:(q2 + 1) * 4, :].rearrange("p a d -> p (a d)"),
                                  rb_)
                yv = yraw_[:, q2 * 8:(q2 + 1) * 8, :].rearrange("p (a b) d -> p a b d", b=2)
                for vc in range(2):
                    P.tt("dve", yv[:, :, vc, :], yv[:, :, vc, :], rstd8[:, q2 * 4:(q2 + 1) * 4, :], ALU.mult,
                         reads=[rb_, yrb_], writes=[yrb_])
                yield
            P.tt("dve", yflat, yflat, L["gs"][:, :, :].rearrange("p a d -> p (a d)"), ALU.mult, reads=[lb, yrb_],
                 writes=[yrb_])
            yield
            for k in range(16):
                P.act(yab[:, k, :], yraw_[:, k, :], AF.Copy, reads=[yrb_, cbf], writes=[yabb], scale=rnw[:, k:k + 1])
                if k % 4 == 3:
                    yield
            P.dma("sp", self.ym_d[2048:4096, sl].rearrange("(k p) t -> p k t", p=128), yab[:, :, :], reads=[yabb],
                  writes=[ymb])

        loads(0)
        prev = None
        for c in range(16):
            if c + 1 < 16:
                loads(c + 1)
            rheads(c, gens=[prev] if prev is not None else [])
            if prev is not None:
                for _ in prev:
                    pass
            prev = repilogue(c)
        for _ in prev:
            pass
        P.ps_unreserve(iy0)
        P.ps_unreserve(iy1)
        P.release(m)
        self.stage_down(self.ym_d, ymb, self.hy_out, 32)

    def load_pos(self):
        P = self.P
        pb = Buf()
        pos_i = P.sb([128, T], I32, "pos_i")
        pos_f = P.sb([128, T], F32, "pos_f")
        P.dma("sp", pos_i[:], self.posin.partition_broadcast(128), writes=[pb])
        P.copy("dve", pos_f[:], pos_i[:], reads=[pb], writes=[pb])
        return pos_f, pb

    def trig(self, out_t, out_buf, pos_f, pb, invcol, phase, tmp):
        P = self.P
        v, vi, vf, tb = tmp
        INV2PI = float(1.0 / (2 * np.pi))
        P.ts("dve", v[:], pos_f[:], invcol, ALU.mult, reads=[pb, self.cbuf], writes=[tb])
        P.ts("dve", v[:], v[:], INV2PI, ALU.mult, float(phase), ALU.add, reads=[tb], writes=[tb])
        P.copy("dve", vi[:], v[:], reads=[tb], writes=[tb])
        P.copy("dve", vf[:], vi[:], reads=[tb], writes=[tb])
        P.tt("dve", v[:], v[:], vf[:], ALU.subtract, reads=[tb], writes=[tb])
        P.stt(v[:], v[:], 0.5, v[:], ALU.is_gt, ALU.subtract, reads=[tb], writes=[tb])
        P.stt(v[:], v[:], 0.5, v[:], ALU.is_gt, ALU.subtract, reads=[tb], writes=[tb])
        P.act(out_t[:], v[:], AF.Sin, reads=[tb], writes=[out_buf], scale=float(2 * np.pi))

    def proj_fm(self, hT, hbuf, W, col0, ncols, handler, dup=False):
        P = self.P
        w, wb = self.pw[self.pw_i % 2]
        self.pw_i += 1
        if ncols < 128:
            P.memset("pool", w[:, :, :], 0.0, writes=[wb])
        self.wload(w[:, :, 0:ncols], wb, W[:, col0:col0 + ncols].rearrange("(k p) n -> p k n", p=128))
        if dup:
            wb2 = Buf()
            P.dma("pool", w[:, :, ncols:2 * ncols], W[:, col0:col0 + ncols].rearrange("(k p) n -> p k n", p=128),
                  writes=[wb2])
        prev = None
        for tt in range(T // 512):
            pt, pb = P.ps()
            for k in range(KC):
                P.mm(pt[:, :], lhsT=w[:, k, :], rhs=hT[:, k, tt * 512:(tt + 1) * 512], start=(k == 0),
                     stop=(k == KC - 1), reads=[wb, hbuf[tt]] + ([wb2] if dup else []), writes=[pb])
            if prev is not None:
                handler(*prev)
            prev = (tt, pt, pb)
        handler(*prev)

    def rope_fm(self, xf, xfb, pm, cos_t, sin_t, tbuf, tt, out_ap, out_buf, tmp2):
        P = self.P
        pt, pb = P.ps()
        P.mm(pt[:, :], lhsT=pm[:], rhs=xf[:], start=True, stop=True, reads=[xfb, self.cbuf], writes=[pb])
        t1, t2, t12b = tmp2
        sl = slice(tt * 512, (tt + 1) * 512)
        P.tt("pool", t1[:], xf[:], cos_t[:, sl], ALU.mult, reads=[xfb, tbuf], writes=[t12b])
        P.tt("dve", t2[:], pt[:, :], sin_t[:, sl], ALU.mult, reads=[pb, tbuf], writes=[t12b])
        P.tt("dve", out_ap, t1[:], t2[:], ALU.add, reads=[t12b], writes=[out_buf])

    def stage_dsa(self, hT, hbuf, m):
        P = self.P
        W = self.dsa_in
        if not hasattr(self, "q_d"):
            self.q_d = self.dscr("q_d", [D, T], BF16)
            self.qi_d = self.dscr("qi_d", [1024, T], F32)
            self.ao_d = self.dscr("ao_d", [D, T], BF16)
        qdb, qidb, aob = Buf(), Buf(), Buf()
        kT = P.sb([128, T], BF16, "kT")
        vtm = P.sb([128, 16, 128], BF16, "vtm")
        kiA = P.sb([128, T], F32, "kiA")
        kiB = P.sb([128, T], F32, "kiB")
        witm = P.sb([128, 16, 16], F32, "witm")
        resb = Buf()
        idxnw = P.sb([128, 1], F32, "idxnw")
        P.dma("sp", idxnw[:], self.idx_nw, writes=[resb])
        P.memset("pool", kiA[:], 0.0, writes=[resb])
        P.memset("pool", kiB[:], 0.0, writes=[resb])
        mp = P.mark()
        cosq = P.sb([128, T], F32, "cosq")
        sinq = P.sb([128, T], F32, "sinq")
        cosi = P.sb([128, T], F32, "cosi")
        sini = P.sb([128, T], F32, "sini")
        trb = Buf()
        m2 = P.mark()
        pos_f, pb_ = self.load_pos()
        tmp = (P.sb([128, T], F32, "tv"), P.sb([128, T], I32, "tvi"), P.sb([128, T], F32, "tvf"), Buf())
        inv = self.cs["inv"]
        self.trig(cosq, trb, pos_f, pb_, inv[:, 1:2], 0.25, tmp)
        self.trig(sinq, trb, pos_f, pb_, inv[:, 1:2], 0.0, tmp)
        self.trig(cosi, trb, pos_f, pb_, inv[:, 2:3], 0.25, tmp)
        self.trig(sini, trb, pos_f, pb_, inv[:, 2:3], 0.0, tmp)
        P.release(m2)
        self.pw = [(P.sb([128, KC, 128], BF16, "pw%d" % i), Buf()) for i in range(2)]
        self.pw_i = 0
        xfs = [(P.sb([128, 512], F32, "xf%d" % i), Buf()) for i in range(2)]
        tmp2s = [(P.sb([128, 512], F32, "t1_%d" % i), P.sb([128, 512], F32, "t2_%d" % i), Buf()) for i in range(2)]
        rowb = [(P.sb([128, T], BF16, "rowb%d" % i), Buf()) for i in range(2)]
        rowf = [(P.sb([128, T], F32, "rowf%d" % i), Buf()) for i in range(2)]
        cnt = [0]

        def roped(pm, cos_t, sin_t, out_tile, out_buf, scale_in=None):
            def h(tt, pt, pb):
                xf, xfb = xfs[cnt[0] % 2]
                t2 = tmp2s[cnt[0] % 2]
                cnt[0] += 1
                P.copy("act", xf[:], pt[:, :], reads=[pb], writes=[xfb])
                self.rope_fm(xf, xfb, pm, cos_t, sin_t, trb, tt, out_tile[:, tt * 512:(tt + 1) * 512], out_buf, t2)
            return h

        for hh in range(16):
            rt, rb = rowb[hh % 2]
            self.proj_fm(hT, hbuf, W, IN1_OFF["q"] + hh * 128, 128, roped(self.cs["pm_q"], cosq, sinq, rt, rb))
            P.dma("sp", self.q_d[hh * 128:(hh + 1) * 128, :], rt[:, :], reads=[rb], writes=[qdb])
        self.proj_fm(hT, hbuf, W, IN1_OFF["k"], 128, roped(self.cs["pm_q"], cosq, sinq, kT, resb))
        vT, vTb = rowb[0]

        def hv(tt, pt, pb):
            P.copy("act", vT[:, tt * 512:(tt + 1) * 512], pt[:, :], reads=[pb], writes=[vTb])
        self.proj_fm(hT, hbuf, W, IN1_OFF["v"], 128, hv)
        for kt in range(16):
            pt, pb = P.ps()
            ptb = pt
            P.tr(ptb[:, 0:128], vT[:, kt * 128:(kt + 1) * 128], self.ident_bf[:], reads=[vTb, self.cbuf], writes=[pb])
            P.copy("dve", vtm[:, kt, :], ptb[:, 0:128], reads=[pb], writes=[resb])
        for ch in range(8):
            rt, rb = rowf[ch % 2]
            self.proj_fm(hT, hbuf, W, IN1_OFF["qi"] + ch * 128, 128, roped(self.cs["pm_i"], cosi, sini, rt, rb))
            P.dma("sp", self.qi_d[ch * 128:(ch + 1) * 128, :], rt[:, :], reads=[rb], writes=[qidb])
        kir, kirb = rowf[0]

        def hki(tt, pt, pb):
            xf, xfb = xfs[cnt[0] % 2]
            t1, t2, t12b = tmp2s[cnt[0] % 2]
            cnt[0] += 1
            P.copy("act", xf[:], pt[:, :], reads=[pb], writes=[xfb])
            P.act(t1[:], pt[:, :], AF.Square, reads=[pb], writes=[t12b])
            p2, p2b = P.ps()
            P.mm(p2[:, :], lhsT=self.cs["bones"][:], rhs=t1[:], start=True, stop=True, reads=[t12b, self.cbuf],
                 writes=[p2b])
            self.rstd_from_ss(p2[:, :], p2b, 64, 512, t2[:], t12b)
            P.stt(xf[:], xf[:], idxnw[:, 0:1], t2[:], ALU.mult, ALU.mult, reads=[xfb, t12b, resb], writes=[xfb])
            t3 = tmp2s[cnt[0] % 2]
            self.rope_fm(xf, xfb, self.cs["pm_i"], cosi, sini, trb, tt, kir[:, tt * 512:(tt + 1) * 512], kirb, t3)
        self.proj_fm(hT, hbuf, W, IN1_OFF["ki"], 64, hki, dup=True)
        P.copy("dve", kiA[0:64, :], kir[0:64, :], reads=[kirb], writes=[resb])
        P.copy("dve", kiB[64:128, :], kir[64:128, :], reads=[kirb], writes=[resb])
        wiT, wiTb = rowf[1]

        def hwi(tt, pt, pb):
            P.act(wiT[:, tt * 512:(tt + 1) * 512], pt[:, :], AF.Copy, reads=[pb], writes=[wiTb], scale=1.0 / 32.0)
        self.proj_fm(hT, hbuf, W, IN1_OFF["wi"], 16, hwi)
        for j in range(16):
            pt, pb = P.ps()
            P.tr(pt[:, 0:128], wiT[:, j * 128:(j + 1) * 128], self.cs["ident"][:], reads=[wiTb, self.cbuf],
                 writes=[pb])
            P.copy("dve", witm[:, j, :], pt[:, 0:16], reads=[pb], writes=[resb])
        P.release(mp)

        accs = [(P.sb([128, T], F32, "acc%d" % i), Buf()) for i in range(2)]
        work = P.sb([128, T], F32, "work")
        workb = Buf()
        m8 = P.sb([128, 8], F32, "m8")
        thr = P.sb([128, 1], F32, "thr")
        sels = [(P.sb([128, T], BF16, "sel%d" % i), Buf()) for i in range(2)]
        selTs = [(P.sb([128, 16, 128], BF16, "selT%d" % i), Buf()) for i in range(2)]
        qTs = [(P.sb([128, 16 * 128], BF16, "qT%d" % i), Buf()) for i in range(2)]
        qiTs = [(P.sb([128, 8, 128], F32, "qiT%d" % i), Buf()) for i in range(2)]
        rl = [(P.sb([128, 512], F32, "rl%d" % i), Buf()) for i in range(3)]
        rw = [(P.sb([128, 512], F32, "rw%d" % i), Buf()) for i in range(2)]
        ef = [(P.sb([128, 512], BF16, "ef%d" % i), Buf()) for i in range(4)]
        pTs = [(P.sb([128, 512], BF16, "pT%d" % i), Buf()) for i in range(4)]
        rsum = P.sb([128, 512], F32, "rsum")
        rsb = Buf()
        aos = [(P.sb([128, 512], BF16, "ao%d" % i), Buf()) for i in range(2)]
        iO, (accO, accOb) = P.ps_reserve()
        iS, (accS, accSb) = P.ps_reserve()
        iO2, (accO2, accOb2) = P.ps_reserve()
        iS2, (accS2, accSb2) = P.ps_reserve()
        accsets = [(accO, accOb, accS, accSb), (accO2, accOb2, accS2, accSb2)]
        SC = float(128.0 ** -0.5)
        cn = dict(c1=0, c2=0, c3=0)

        def score_part(j):
            Wj = 128 * (j + 1)
            nseg = (Wj + 511) // 512
            qT, qTb = qTs[j % 2]
            qiT, qiTb = qiTs[j % 2]
            acc, accb = accs[j % 2]
            sel, selb = sels[j % 2]
            P.dma("sp", qT[:, :].rearrange("p (h t) -> p h t", h=16),
                  self.q_d[:, j * 128:(j + 1) * 128].rearrange("(h p) t -> p h t", p=128), reads=[qdb], writes=[qTb])
            P.dma("sp", qiT[:, :, :],
                  self.qi_d[:, j * 128:(j + 1) * 128].rearrange("(h p) t -> p h t", p=128), reads=[qidb],
                  writes=[qiTb])
            if j > 0:
                P.memset("pool", acc[:, 0:Wj - 128], 0.0, writes=[accb])
            P.copy("pool", acc[:, Wj - 128:Wj], self.cs["cbias"][:], reads=[self.cbuf], writes=[accb])
            for hi in range(16):
                rk = kiA if hi % 2 == 0 else kiB
                for sg in range(nseg):
                    s0 = sg * 512
                    s1 = min(Wj, s0 + 512)
                    pt, pb = P.ps()
                    P.mm(pt[:, 0:s1 - s0], lhsT=qiT[:, hi // 2, :], rhs=rk[:, s0:s1], start=True, stop=True,
                         reads=[qiTb, resb], writes=[pb])
                    r, rb_ = rl[cn["c1"] % 3]
                    r2, rb2 = rw[cn["c1"] % 2]
                    cn["c1"] += 1
                    P.act(r[:, 0:s1 - s0], pt[:, 0:s1 - s0], AF.Relu, reads=[pb], writes=[rb_])
                    P.stt(acc[:, s0:s1], r[:, 0:s1 - s0], witm[:, j, hi:hi + 1], acc[:, s0:s1], ALU.mult, ALU.add,
                          reads=[rb_, resb, accb], writes=[accb])
            if j >= 2:
                P.copy("dve", work[:, 0:Wj], acc[:, 0:Wj], reads=[accb], writes=[workb])
                for r_ in range(32):
                    P.op("dve", lambda e: e.max(out=m8[:], in_=work[:, 0:Wj]), reads=[workb], writes=[workb])
                    if r_ < 31:
                        P.op("dve", lambda e: e.match_replace(out=work[:, 0:Wj], in_to_replace=m8[:],
                                                               in_values=work[:, 0:Wj], imm_value=NEG),
                             reads=[workb], writes=[workb])
                P.ts("dve", thr[:], m8[:, 7:8], -1.0e29, ALU.max, reads=[workb], writes=[workb])
            else:
                P.memset("dve", thr[:], -1.0e29, writes=[workb])
            P.ts("dve", sel[:, 0:Wj], acc[:, 0:Wj], thr[:, 0:1], ALU.is_ge, reads=[accb, workb], writes=[selb])

        def attn_part(j):
            qT, qTb = qTs[j % 2]
            selT, selTb = selTs[j % 2]
            sel, selb = sels[j % 2]
            for kt in range(j + 1):
                pt, pb = P.ps()
                P.tr(pt[:, 0:128], sel[:, kt * 128:(kt + 1) * 128], self.ident_bf[:], reads=[selb, self.cbuf],
                     writes=[pb])
                P.copy("act", selT[:, kt, :], pt[:, 0:128], reads=[pb], writes=[selTb])
            steps = [(g, kt) for g in range(4) for kt in range(j + 1)]
            LA = 2
            live = {}

            def front(i):
                g, kt = steps[i]
                pt, pb = P.ps()
                P.mm(pt[:, :], lhsT=kT[:, kt * 128:(kt + 1) * 128], rhs=qT[:, g * 512:(g + 1) * 512],
                     start=True, stop=True, reads=[resb, qTb], writes=[pb])
                e_, eb = ef[cn["c2"] % 4]
                p_, pb2 = pTs[cn["c2"] % 4]
                cn["c2"] += 1
                P.act(e_[:], pt[:, :], AF.Exp, reads=[pb], writes=[eb], scale=SC)
                P.tt("pool", p_[:, :].rearrange("p (h q) -> p h q", h=4),
                     e_[:, :].rearrange("p (h q) -> p h q", h=4),
                     selT[:, kt, :].unsqueeze(1).broadcast_to([128, 4, 128]), ALU.mult,
                     reads=[eb, selTb], writes=[pb2])
                live[i] = (p_, pb2)

            def back(i):
                g, kt = steps[i]
                p_, pb2 = live.pop(i)
                accO, accOb, accS, accSb = accsets[g % 2]
                P.mm(accO[:, :], lhsT=vtm[:, kt, :], rhs=p_[:, :], start=(kt == 0), stop=(kt == j),
                     reads=[resb, pb2], writes=[accOb], inc=False)
                P.mm(accS[:, :], lhsT=self.ones_bf[:], rhs=p_[:, :], start=(kt == 0), stop=(kt == j),
                     reads=[self.cbuf, pb2], writes=[accSb], inc=True)
                if kt == j:
                    P.op("dve", lambda e: e.reciprocal(out=rsum[:], in_=accS[:, :]), reads=[accSb], writes=[rsb])
                    ao, aob_ = aos[cn["c3"] % 2]
                    cn["c3"] += 1
                    P.tt("dve", ao[:], accO[:, :], rsum[:], ALU.mult, reads=[accOb, rsb], writes=[aob_])
                    P.dma("sp", self.ao_d[g * 512:(g + 1) * 512, j * 128:(j + 1) * 128].rearrange(
                        "(h p) t -> p h t", p=128), ao[:, :].rearrange("p (h q) -> p h q", h=4), reads=[aob_],
                        writes=[aob])

            n = len(steps)
            for i in range(n + LA):
                if i < n:
                    front(i)
                if i - LA >= 0:
                    back(i - LA)

        score_part(0)
        for j in range(16):
            if j + 1 < 16:
                score_part(j + 1)
            attn_part(j)
        for i_ in (iO, iS, iO2, iS2):
            P.ps_unreserve(i_)
        P.release(m)
        self.stage_down(self.ao_d, aob, self.dsa_out, 16)

_CACHE = {}


def col_layout(v, nchunk):
    return np.ascontiguousarray(np.asarray(v).reshape(nchunk, 128).T)


def make_inmaps(inputs, ncores=4):
    g = lambda k: np.asarray(inputs[k])
    hc = host_consts()
    shared = {}
    shared["mod_w"] = np.ascontiguousarray(g("mod_w"), dtype=np.float32)
    shared["mod_b"] = np.stack([col_layout(g("mod_b")[i], 144) for i in range(2)]).astype(np.float32)
    nw = g("norm_w")
    shared["norm_w"] = np.stack(
        [np.concatenate([col_layout(nw[i, s], KC) for s in range(6)], axis=1) for i in range(2)]).astype(np.float32)
    shared["ffn_w_gate"] = np.ascontiguousarray(g("ffn_w_gate"), dtype=np.float32)
    shared["ffn_w_up"] = np.ascontiguousarray(g("ffn_w_up"), dtype=np.float32)
    shared["ffn_w_down"] = np.ascontiguousarray(g("ffn_w_down"), dtype=np.float32)
    shared["hy_w_in"] = np.ascontiguousarray(g("hy_w_in")[0], dtype=np.float32)
    cw = g("hy_conv_w")[0]
    shared["conv_w"] = np.ascontiguousarray(
        cw.reshape(4, 32, 128).transpose(2, 1, 0).reshape(128, 128)).astype(np.float32)
    shared["conv_b"] = col_layout(g("hy_conv_b")[0], 32).astype(np.float32)
    sm = np.zeros((128, 4), np.float32)
    sm[:32, 0] = g("ssd_A_log")[0]
    sm[:32, 1] = g("ssd_dt_bias")[0]
    shared["ssd_small"] = sm
    shared["ssd_Dc"] = col_layout(np.repeat(g("ssd_D")[0], 64), 16).astype(np.float32)
    shared["ssd_nw"] = col_layout(g("ssd_norm_w")[0], 16).astype(np.float32)
    shared["ret_nw"] = col_layout(g("ret_norm_w")[0], 16).astype(np.float32)
    shared["hy_w_out"] = np.ascontiguousarray(g("hy_w_out")[0], dtype=np.float32)
    shared["dsa_w_in"] = np.ascontiguousarray(g("dsa_w_in")[0], dtype=np.float32)
    shared["idx_nw"] = np.concatenate([g("idx_k_norm_w")[0]] * 2).reshape(128, 1).astype(np.float32)
    shared["dsa_w_out"] = np.ascontiguousarray(g("dsa_w_out")[0], dtype=np.float32)
    for k in CONST_SHAPES:
        shared["c_" + k] = hc[k]
    maps = []
    x = g("x")
    c = g("c")
    pos = g("positions")
    for b in range(ncores):
        mp = dict(shared)
        mp["xT"] = np.ascontiguousarray(x[b].T, dtype=np.float32)
        mp["c_col"] = col_layout(c[b], KC).astype(np.float32)
        mp["pos"] = np.ascontiguousarray(pos[b], dtype=np.int32)
        maps.append(mp)
    return maps


def run(inputs, n_sub=6, trace=False, ncores=4):
    key = str(n_sub)
    if key not in _CACHE:
        _CACHE[key] = Builder(n_sub)
    bld = _CACHE[key]
    maps = make_inmaps(inputs, ncores)
    used = set(bld.inp.keys())
    maps = [{k: v for k, v in mp.items() if k in used} for mp in maps]
    res = run_bass_kernel_spmd(bld.nc, maps, core_ids=list(range(ncores)), trace=trace)
    out = np.stack([np.ascontiguousarray(res.results[b]["yT"].T) for b in range(ncores)])
    return out.astype(np.float32), res


def kernel(**inputs):
    out, _ = run(inputs, 6)
    return out
```
